# Optimizing a Trainium2 kernel written in Bass

```python
import jax, jax.numpy as jnp
from jax import lax
import numpy as np

D_MODEL = 1024
BATCH = 16
SEQ = 4096
DEPTH = 2

CHUNK = 64
ML_HEADS = 4
ML_HEAD_DIM = D_MODEL // 8
ML_WIDTH = ML_HEADS * ML_HEAD_DIM
CONV_WIDTH = 4
SB_HEADS = 8
SB_HEAD_DIM = D_MODEL // 16
SB_WIDTH = SB_HEADS * SB_HEAD_DIM
SB_BLOCK = 128
N_BRANCH = 2
D_FF = -(-8 * D_MODEL // (3 * 256)) * 256
EPS = 1e-6
IN_COLS = 2 * ML_WIDTH + ML_WIDTH + ML_WIDTH + 2 * ML_HEADS + 3 * SB_WIDTH + N_BRANCH * D_MODEL

kernel_name = "hybrid_mlstm_stickbreaking_gated_block"


def rmsnorm(x, g):
    xf = x.astype(jnp.float32)
    y = xf * lax.rsqrt(jnp.mean(xf * xf, axis=-1, keepdims=True) + EPS)
    return (y * g.astype(jnp.float32)).astype(x.dtype)


def causal_dwconv(x, w, b):
    C = x.shape[-1]
    y = lax.conv_general_dilated(
        x, w[:, None, :], window_strides=(1,), padding=[(CONV_WIDTH - 1, 0)],
        dimension_numbers=("NWC", "WIO", "NWC"), feature_group_count=C)
    return y + b


def mlstm_chunkwise(q, k, v, ig, fg):
    B, S, H, Dh = q.shape
    nc = S // CHUNK
    k = k * (Dh ** -0.5)
    lf = jax.nn.log_sigmoid(fg)

    def chunks4(a):
        return a.reshape(B, nc, CHUNK, H, Dh).transpose(1, 0, 3, 2, 4)

    def chunks3(a):
        return a.reshape(B, nc, CHUNK, H).transpose(1, 0, 3, 2)

    causal = jnp.tril(jnp.ones((CHUNK, CHUNK), dtype=bool))

    def step(carry, xs):
        C, n, m = carry
        qc, kc, vc, ic, fc = xs
        b = jnp.cumsum(fc, axis=-1)
        log_d = b[..., :, None] - b[..., None, :] + ic[..., None, :]
        log_d = jnp.where(causal, log_d, -jnp.inf)
        log_inter = b + m[..., None]
        m_t = jnp.maximum(log_inter, jnp.max(log_d, axis=-1))
        w_intra = jnp.exp(log_d - m_t[..., None])
        w_inter = jnp.exp(log_inter - m_t)
        s = jnp.einsum("bhld,bhmd->bhlm", qc, kc) * w_intra
        num = (w_inter[..., None] * jnp.einsum("bhld,bhde->bhle", qc, C)
               + jnp.einsum("bhlm,bhme->bhle", s, vc))
        den = w_inter * jnp.einsum("bhld,bhd->bhl", qc, n) + jnp.sum(s, axis=-1)
        h = num / jnp.maximum(jnp.abs(den), jnp.exp(-m_t))[..., None]
        b_last = b[..., -1]
        log_w = b_last[..., None] - b + ic
        m_new = jnp.maximum(b_last + m, jnp.max(log_w, axis=-1))
        w = jnp.exp(log_w - m_new[..., None])
        decay = jnp.exp(b_last + m - m_new)
        C_new = decay[..., None, None] * C + jnp.einsum("bhld,bhle->bhde", kc * w[..., None], vc)
        n_new = decay[..., None] * n + jnp.einsum("bhl,bhld->bhd", w, kc)
        return (C_new, n_new, m_new), h

    init = (jnp.zeros((B, H, Dh, Dh), jnp.float32),
            jnp.zeros((B, H, Dh), jnp.float32),
            jnp.zeros((B, H), jnp.float32))
    xs = (chunks4(q), chunks4(k), chunks4(v), chunks3(ig), chunks3(lf))
    _, h = lax.scan(step, init, xs)
    return h.transpose(1, 0, 3, 2, 4).reshape(B, S, H * Dh)


def stick_breaking(q, k, v):
    B, H, S, Dh = q.shape
    scale = Dh ** -0.5
    outs = []
    for start in range(0, S, SB_BLOCK):
        end = start + SB_BLOCK
        qb = q[:, :, start:end]
        kb = k[:, :, :end]
        vb = v[:, :, :end]
        z = jnp.einsum("bhtd,bhsd->bhts", qb, kb) * scale
        t_idx = start + jnp.arange(SB_BLOCK)
        s_idx = jnp.arange(end)
        mask = s_idx[None, :] < t_idx[:, None]
        u = jnp.where(mask, jax.nn.log_sigmoid(-z), 0.0)
        tail = lax.cumsum(u, axis=3, reverse=True) - u
        a = jnp.where(mask, jnp.exp(jax.nn.log_sigmoid(z) + tail), 0.0)
        outs.append(jnp.einsum("bhts,bhsd->bhtd", a, vb))
    o = jnp.concatenate(outs, axis=2)
    return o.transpose(0, 2, 1, 3).reshape(B, S, H * Dh)


def head_rms(x, g):
    xf = x.astype(jnp.float32)
    return xf * lax.rsqrt(jnp.mean(xf * xf, axis=-1, keepdims=True) + EPS) * g.astype(jnp.float32)


def setup_inputs(seed: int = 0) -> dict:
    key = jax.random.key(seed)
    ks = jax.random.split(key, 16)
    nrm = jax.random.normal
    f32 = jnp.float32
    x = nrm(ks[0], (BATCH, SEQ, D_MODEL), f32)
    g_mix = 1.0 + 0.02 * nrm(ks[1], (DEPTH, D_MODEL), f32)
    w_in = nrm(ks[2], (DEPTH, D_MODEL, IN_COLS), f32) * D_MODEL ** -0.5
    conv_w = nrm(ks[3], (DEPTH, CONV_WIDTH, 2 * ML_WIDTH), f32) * CONV_WIDTH ** -0.5
    conv_b = 0.02 * nrm(ks[4], (DEPTH, 2 * ML_WIDTH), f32)
    i_bias = 0.1 * nrm(ks[5], (DEPTH, ML_HEADS), f32)
    f_bias = jnp.linspace(3.0, 6.0, ML_HEADS, dtype=f32)[None, :] + 0.1 * nrm(ks[6], (DEPTH, ML_HEADS), f32)
    b_gates = jnp.concatenate([i_bias, f_bias], axis=-1)
    g_q = 1.0 + 0.02 * nrm(ks[7], (DEPTH, SB_HEAD_DIM), f32)
    g_k = 1.0 + 0.02 * nrm(ks[8], (DEPTH, SB_HEAD_DIM), f32)
    w_br_a = nrm(ks[9], (DEPTH, ML_WIDTH, D_MODEL), f32) * ML_WIDTH ** -0.5
    w_br_b = nrm(ks[10], (DEPTH, SB_WIDTH, D_MODEL), f32) * SB_WIDTH ** -0.5
    w_out = nrm(ks[11], (DEPTH, D_MODEL, D_MODEL), f32) * D_MODEL ** -0.5
    g_ffn = 1.0 + 0.02 * nrm(ks[12], (DEPTH, D_MODEL), f32)
    w_gu = nrm(ks[13], (DEPTH, D_MODEL, 2 * D_FF), f32) * D_MODEL ** -0.5
    w_down = nrm(ks[14], (DEPTH, D_FF, D_MODEL), f32) * D_FF ** -0.5
    return {"x": x, "g_mix": g_mix, "w_in": w_in, "conv_w": conv_w, "conv_b": conv_b,
            "b_gates": b_gates, "g_q": g_q, "g_k": g_k, "w_br_a": w_br_a, "w_br_b": w_br_b,
            "w_out": w_out, "g_ffn": g_ffn, "w_gu": w_gu, "w_down": w_down}


def reference(x, g_mix, w_in, conv_w, conv_b, b_gates, g_q, g_k, w_br_a, w_br_b,
              w_out, g_ffn, w_gu, w_down):
    B, S, _ = x.shape
    sizes = [2 * ML_WIDTH, ML_WIDTH, ML_WIDTH, 2 * ML_HEADS, SB_WIDTH, SB_WIDTH, SB_WIDTH]
    cuts = list(np.cumsum(sizes))
    for l in range(DEPTH):
        h = rmsnorm(x, g_mix[l])
        proj = h @ w_in[l]
        qk_pre, v_m, o_m, gates, q_s, k_s, v_s, gate_pre = jnp.split(proj, cuts, axis=-1)

        qk_m = jax.nn.silu(causal_dwconv(qk_pre, conv_w[l], conv_b[l]))
        q_m, k_m = jnp.split(qk_m.astype(jnp.float32), 2, axis=-1)
        gates = gates.astype(jnp.float32) + b_gates[l].astype(jnp.float32)
        ig, fg = gates[..., :ML_HEADS], gates[..., ML_HEADS:]
        hd = (B, S, ML_HEADS, ML_HEAD_DIM)
        h_m = mlstm_chunkwise(q_m.reshape(hd), k_m.reshape(hd),
                              v_m.astype(jnp.float32).reshape(hd), ig, fg)
        y_a = (jax.nn.sigmoid(o_m.astype(jnp.float32)) * h_m).astype(x.dtype)

        sd = (B, S, SB_HEADS, SB_HEAD_DIM)
        qs = head_rms(q_s.reshape(sd), g_q[l]).transpose(0, 2, 1, 3)
        kss = head_rms(k_s.reshape(sd), g_k[l]).transpose(0, 2, 1, 3)
        vs = v_s.astype(jnp.float32).reshape(sd).transpose(0, 2, 1, 3)
        y_b = stick_breaking(qs, kss, vs).astype(x.dtype)

        g = jax.nn.sigmoid(gate_pre).reshape(B, S, N_BRANCH, D_MODEL)
        mix = g[..., 0, :] * (y_a @ w_br_a[l]) + g[..., 1, :] * (y_b @ w_br_b[l])
        x = x + mix @ w_out[l]

        h2 = rmsnorm(x, g_ffn[l])
        gt, up = jnp.split(h2 @ w_gu[l], 2, axis=-1)
        x = x + (jax.nn.silu(gt) * up) @ w_down[l]
    return x
```

```python
import math
from contextlib import ExitStack

import numpy as np
import concourse.bass as bass
import concourse.mybir as mybir
from concourse.bass_utils import run_bass_kernel_spmd

F32 = mybir.dt.float32
BF16 = mybir.dt.bfloat16
AF = mybir.ActivationFunctionType
ALU = mybir.AluOpType

D = 1024
DEPTH = 2
NCORES = 8
ML_H = 4
SB_H = 8
D_FF = 2816
IN_COLS = 5640
EPS = 1e-6
T = 512
C_QK, C_VM, C_OM, C_G, C_QS, C_KS, C_VS, C_GP = 0, 1024, 1536, 2048, 2056, 2568, 3080, 3592
NEG = -30000.0


class Cfg:
    def __init__(self, nseq=2, S=4096):
        self.nseq = nseq
        self.S = S
        self.ntok = nseq * S
        self.ntile = self.ntok // T
        self.tps = S // T


class Sem:
    def __init__(self, h):
        self.h = h
        self.v = 0


class Phase:
    def __init__(self, nc, name):
        self.nc = nc
        self.name = name
        self.es = ExitStack()
        self.waited = {}
        self.engs = {"pe": nc.tensor, "act": nc.scalar, "dve": nc.vector, "pool": nc.gpsimd, "sp": nc.sync}
        self.esem = {}
        self.nsem = 0
        self.final = []

    def __enter__(self):
        self.es.__enter__()
        for e in ("pe", "act", "dve", "pool"):
            self.esem[e] = self.sem(e)
        return self

    def __exit__(self, *a):
        if a[0] is None:
            self.wait("sp", *self.final)
            self.nc.all_engine_barrier()
        return self.es.__exit__(*a)

    def sem(self, name):
        self.nsem += 1
        return Sem(self.es.enter_context(self.nc.semaphore(f"{self.name}_{name}_{self.nsem}")))

    def sb(self, name, shape, dt):
        return self.es.enter_context(self.nc.sbuf_tensor(f"{self.name}_{name}", list(shape), dt))

    def ps(self, name, shape, dt):
        return self.es.enter_context(self.nc.psum_tensor(f"{self.name}_{name}", list(shape), dt))

    def done(self, e, ins):
        s = self.esem[e]
        if s.v >= 30000:
            s = self.sem(e)
            self.esem[e] = s
        ins.then_inc(s.h, 1)
        s.v += 1
        return (s, s.v)

    def wait(self, e, *toks):
        eng = self.engs[e]
        for tok in toks:
            if tok is None:
                continue
            if isinstance(tok, (list,)):
                self.wait(e, *tok)
                continue
            s, v = tok
            key = (e, id(s))
            if self.waited.get(key, 0) >= v:
                continue
            eng.wait_ge(s.h, v)
            self.waited[key] = v

    def dma(self, q, out, in_, sem, **kw):
        ins = self.engs[q].dma_start(out=out, in_=in_, **kw)
        ins.then_inc(sem.h, 16)
        sem.v += 16
        return (sem, sem.v)


def make_consts(ph):
    P = ph.engs["pool"]
    c = {}
    f1 = ph.sb("c_f1", [128, 128], F32)
    c["identb"] = ph.sb("c_identb", [128, 128], BF16)
    t = ph.done("pool", P.memset(f1[:], 1.0))
    ph.wait("pool", t)
    t = ph.done("pool", P.affine_select(out=f1[:], in_=f1[:], pattern=[[-1, 128]], compare_op=ALU.is_equal,
                                        fill=0.0, base=0, channel_multiplier=1))
    ph.wait("pool", t)
    c["t_identb"] = ph.done("pool", P.tensor_copy(out=c["identb"][:], in_=f1[:]))
    c["_f1"] = f1
    return c


def tri_f32(ph, name, val, kind):
    P = ph.engs["pool"]
    t_ = ph.sb(name, [128, 128], F32)
    t = ph.done("pool", P.memset(t_[:], val))
    if kind != "all":
        ph.wait("pool", t)
        if kind == "le":
            pat, cm = [[1, 128]], -1
        else:
            pat, cm = [[-1, 128]], 1
        t = ph.done("pool", P.affine_select(out=t_[:], in_=t_[:], pattern=pat, compare_op=ALU.is_ge,
                                            fill=0.0, base=0, channel_multiplier=cm))
    return t_, t


def tri_bf16(ph, name, val, kind):
    P = ph.engs["pool"]
    f, t = tri_f32(ph, name + "_f", val, kind)
    b = ph.sb(name, [128, 128], BF16)
    ph.wait("pool", t)
    t = ph.done("pool", P.tensor_copy(out=b[:], in_=f[:]))
    return b, t


def phase1(nc, cfg, L, x_in, W, scr):
    with Phase(nc, f"p1l{L}") as ph:
        PE, ACT, DVE, POOL, SP = (ph.engs[k] for k in ("pe", "act", "dve", "pool", "sp"))
        Win = ph.sb("win", [128, 8, IN_COLS], BF16)
        gmix = ph.sb("gmix", [128, D], F32)
        cw = ph.sb("cw", [128, 32], F32)
        cb = ph.sb("cb", [128, 8], F32)
        bg = ph.sb("bg", [128, 8], F32)
        gqk = ph.sb("gqk", [128, 2], F32)
        xt = [ph.sb("xt0", [128, 4, D], F32)] * 2
        stat = [ph.sb(f"stat{i}", [128, 16], F32) for i in range(2)]
        junk = ph.sb("junk", [128, D], BF16)
        hbf = [ph.sb(f"hbf{i}", [128, D], BF16) for i in range(2)]
        hT = [ph.sb(f"hT{i}", [128, 8, T], BF16) for i in range(2)]
        pre = ph.sb("pre", [128, 8, T + 3], F32)
        acc = [ph.sb(f"acc{i}", [128, T], F32) for i in range(2)]
        sq = [ph.sb(f"sq{i}", [128, T], BF16) for i in range(2)]
        rstd = [ph.sb(f"rstd{i}", [128, T], F32) for i in range(2)]
        gsb = [ph.sb(f"gsb{i}", [128, 16], F32) for i in range(2)]
        qk_st = ph.sb("qk_st", [128, 8, T], BF16)
        km_st = ph.sb("km_st", [128, 4, 512], BF16)
        vm_st = ph.sb("vm_st", [128, 4, 512], BF16)
        om_st = ph.sb("om_st", [128, 4, 512], BF16)
        vs_st = ph.sb("vs_st", [128, 4, 512], BF16)
        qs_st = ph.sb("qs_st", [128, 4, T], BF16)
        ks_st = ph.sb("ks_st", [128, 4, T], BF16)
        g_st = ph.sb("g_st", [128, 16, T], BF16)
        g12_st = ph.sb("g12_st", [128, 4, 12], F32)
        pf = ph.ps("pf", [128, 6, 512], F32)
        pb = ph.ps("pb", [128, 2, 1024], BF16)

        cst = make_consts(ph)
        negU, t_negU = tri_f32(ph, "negU", -1.0, "le")
        negO, t_negO = tri_f32(ph, "negO", -1.0, "all")
        bones = ph.sb("bones", [128, 128], BF16)
        t = ph.done("pool", POOL.memset(bones[:], 0.0))
        ph.wait("pool", t)
        POOL.memset(bones[0:64, 0:64], 1.0 / 64)
        t_bones = ph.done("pool", POOL.memset(bones[64:128, 64:128], 1.0 / 64))

        s_w = ph.sem("w")
        s_p = ph.sem("par")
        t_par = []
        t_par.append(ph.dma("sp", gmix[:], W["g_mix"][L:L + 1, :].partition_broadcast(128), s_p))
        t_par.append(ph.dma("sp", cw[:], W["cwl"][L], s_p))
        t_par.append(ph.dma("sp", cb[:], W["cbl"][L], s_p))
        t_par.append(ph.dma("sp", bg[:], W["b_gates"][L:L + 1, :].partition_broadcast(128), s_p))
        t_par.append(ph.dma("sp", gqk[:], W["gqk"][L], s_p))
        t_par = t_par[-1]
        t_w = None
        wv = W["w_in"][L].rearrange("(k p) n -> p k n", p=128)
        for k in range(8):
            t_w = ph.dma("pool", Win[:, k, :], wv[:, k, :], s_w)
        ph.wait("dve", t_par)
        t_gq = ph.done("dve", DVE.tensor_scalar_mul(out=gqk[:, 0:1], in0=gqk[:, 0:1], scalar1=0.125))

        s_x = [ph.sem("x0")] * 2
        st_sems = {k: ph.sem("st_" + k) for k in ("qk", "km", "vm", "om", "vs", "qs", "ks", "g", "g12")}
        st_tok = {k: None for k in st_sems}
        xt_free = [None]
        hT_free = [None, None]
        hbf_free = [None, None]
        stat_free = [None, None]
        tp_free = None
        kmT_free = None
        mm_free = [None] * 4
        ss_free = None
        small_free = None
        acc_free = [None, None]
        sq_free = [None, None]
        rstd_free = [None, None]
        gsb_free = [None, None]
        pre_free = [None] * 8
        state = {"mm": 0, "acc": 0, "sq": 0, "gsb": 0}
        LOGS = -0.5 * math.log(128.0)

        def mm_bank():
            b = state["mm"] % 4
            state["mm"] += 1
            return b

        for i in range(cfg.ntile):
            slot = i % 2
            seq_start = (i % cfg.tps == 0)
            r0 = i * T
            ph.wait("sp", xt_free[0])
            t_x = ph.dma("sp", xt[slot][:], x_in[r0:r0 + T, :].rearrange("(b p) d -> p b d", p=128), s_x[slot])
            st = stat[slot]
            hT_ready = []
            for b in range(4):
                ph.wait("act", t_x, stat_free[slot] if b == 0 else None)
                t = ph.done("act", ACT.activation(out=junk[:], in_=xt[slot][:, b, :], func=AF.Square,
                                                  accum_out=st[:, b:b + 1]))
                ph.wait("act", t)
                t = ph.done("act", ACT.activation(out=st[:, 4 + b:5 + b], in_=st[:, b:b + 1], func=AF.Ln,
                                                  bias=EPS, scale=1.0 / D))
                ph.wait("act", t)
                t_r = ph.done("act", ACT.activation(out=st[:, 8 + b:9 + b], in_=st[:, 4 + b:5 + b], func=AF.Exp,
                                                    scale=-0.5))
                hb = hbf[b % 2]
                ph.wait("dve", t_r, t_par, hbf_free[b % 2])
                t_h = ph.done("dve", DVE.scalar_tensor_tensor(out=hb[:], in0=xt[slot][:, b, :],
                                                              scalar=st[:, 8 + b:9 + b], in1=gmix[:],
                                                              op0=ALU.mult, op1=ALU.mult))
                if b == 3:
                    xt_free[0] = t_h
                    stat_free[slot] = t_h
                ph.wait("pe", t_h, cst["t_identb"], tp_free)
                for c in range(8):
                    ins = PE.transpose(out=pb[:, 0, c * 128:(c + 1) * 128], in_=hb[:, c * 128:(c + 1) * 128],
                                       identity=cst["identb"][:])
                t_tp = ph.done("pe", ins)
                hbf_free[b % 2] = t_tp
                ph.wait("act", t_tp, hT_free[slot] if b == 0 else None)
                t_e = ph.done("act", ACT.activation(out=hT[slot][:, :, b * 128:(b + 1) * 128],
                                                    in_=pb[:, 0, :].rearrange("p (c t) -> p c t", c=8),
                                                    func=AF.Copy))
                tp_free = t_e
                hT_ready.append(t_e)
            hTs = hT[slot]

            def fm_group(col0):
                bk = mm_bank()
                ph.wait("pe", hT_ready, t_w, mm_free[bk])
                for k in range(8):
                    ins = PE.matmul(pf[:, bk, :], lhsT=Win[:, k, col0:col0 + 128], rhs=hTs[:, k, :],
                                    start=(k == 0), stop=(k == 7))
                return bk, ph.done("pe", ins)

            def tm_group(b, col0, n):
                bk = mm_bank()
                ph.wait("pe", hT_ready, t_w, mm_free[bk])
                for k in range(8):
                    ins = PE.matmul(pf[:, bk, 0:n], lhsT=hTs[:, k, b * 128:(b + 1) * 128], rhs=Win[:, k, col0:col0 + n],
                                    start=(k == 0), stop=(k == 7))
                return bk, ph.done("pe", ins)

            for b in range(4):
                ph.wait("pe", small_free)
                ph.wait("pe", hT_ready, t_w)
                for k in range(8):
                    ins = PE.matmul(pf[:, 5, 0:8], lhsT=hTs[:, k, b * 128:(b + 1) * 128], rhs=Win[:, k, C_G:C_G + 8],
                                    start=(k == 0), stop=(k == 7))
                t_g = ph.done("pe", ins)
                gi = state["gsb"] % 2
                state["gsb"] += 1
                gs = gsb[gi]
                ph.wait("dve", t_g, t_par, gsb_free[gi])
                t = ph.done("dve", DVE.tensor_tensor(out=gs[:, 0:8], in0=pf[:, 5, 0:8], in1=bg[:], op=ALU.add))
                ph.wait("act", t)
                t = ph.done("act", ACT.activation(out=gs[:, 8:12], in_=gs[:, 4:8], func=AF.Exp, scale=-1.0))
                ph.wait("act", t)
                t_sp = ph.done("act", ACT.activation(out=gs[:, 12:16], in_=gs[:, 8:12], func=AF.Ln, bias=1.0))
                ph.wait("pe", t_sp, t_negU, t_negO)
                PE.matmul(pf[:, 5, 8:12], lhsT=negU[:], rhs=gs[:, 12:16], start=True, stop=True)
                t_b = ph.done("pe", PE.matmul(pf[:, 5, 12:16], lhsT=negO[:], rhs=gs[:, 12:16], start=True, stop=True))
                ph.wait("act", t_b, st_tok["g12"] if b == 0 else None)
                t_e1 = ph.done("act", ACT.activation(out=g12_st[:, b, 0:8], in_=pf[:, 5, 8:16], func=AF.Exp))
                ph.wait("dve", t_b)
                t = ph.done("dve", DVE.tensor_tensor(out=gs[:, 8:12], in0=gs[:, 0:4], in1=pf[:, 5, 8:12], op=ALU.subtract))
                ph.wait("act", t)
                t_e2 = ph.done("act", ACT.activation(out=g12_st[:, b, 8:12], in_=gs[:, 8:12], func=AF.Exp, bias=LOGS))
                small_free = [t_e1, t]
                gsb_free[gi] = t_e2
                g12_last = t_e2
                bk, t_m = tm_group(b, C_VM, 512)
                ph.wait("dve", t_m, st_tok["vm"] if b == 0 else None)
                t_vm = ph.done("dve", DVE.tensor_copy(out=vm_st[:, b, :], in_=pf[:, bk, :]))
                mm_free[bk] = t_vm
                bk, t_m = tm_group(b, C_VS, 512)
                ph.wait("dve", t_m, st_tok["vs"] if b == 0 else None)
                t_vs = ph.done("dve", DVE.tensor_copy(out=vs_st[:, b, :], in_=pf[:, bk, :]))
                mm_free[bk] = t_vs
            ph.wait("pool", g12_last)
            st_tok["g12"] = ph.dma("pool", scr["g12"][r0:r0 + T, :].rearrange("(b p) n -> p b n", p=128), g12_st[:], st_sems["g12"])
            ph.wait("pool", t_vm)
            st_tok["vm"] = ph.dma("pool", scr["vm"][r0:r0 + T, :].rearrange("(b p) n -> p b n", p=128), vm_st[:], st_sems["vm"])
            ph.wait("pool", t_vs)
            st_tok["vs"] = ph.dma("pool", scr["vs"][r0:r0 + T, :].rearrange("(b p) n -> p b n", p=128), vs_st[:], st_sems["vs"])

            for which, col, stg, key, gcol in ((0, C_QS, qs_st, "qs", 0), (1, C_KS, ks_st, "ks", 1)):
                for cc in range(4):
                    bk, t_m = fm_group(col + cc * 128)
                    si = state["sq"] % 2
                    state["sq"] += 1
                    ph.wait("act", t_m, sq_free[si])
                    t_sq = ph.done("act", ACT.activation(out=sq[si][:], in_=pf[:, bk, :], func=AF.Square))
                    ph.wait("pe", t_sq, t_bones, ss_free)
                    t_ss = ph.done("pe", PE.matmul(pf[:, 4, :], lhsT=bones[:], rhs=sq[si][:], start=True, stop=True))
                    sq_free[si] = t_ss
                    ph.wait("act", t_ss, rstd_free[si])
                    t_ln = ph.done("act", ACT.activation(out=rstd[si][:], in_=pf[:, 4, :], func=AF.Ln, bias=EPS))
                    ss_free = t_ln
                    ph.wait("act", t_ln)
                    t_rs = ph.done("act", ACT.activation(out=rstd[si][:], in_=rstd[si][:], func=AF.Exp, scale=-0.5))
                    ph.wait("dve", t_rs, t_gq, st_tok[key] if cc == 0 else None)
                    t_o = ph.done("dve", DVE.scalar_tensor_tensor(out=stg[:, cc, :], in0=pf[:, bk, :],
                                                                  scalar=gqk[:, gcol:gcol + 1], in1=rstd[si][:],
                                                                  op0=ALU.mult, op1=ALU.mult))
                    mm_free[bk] = t_o
                    rstd_free[si] = t_o
                ph.wait("pool", t_o)
                st_tok[key] = ph.dma("pool", scr[key + "T"][:, :, r0:r0 + T].rearrange("c p t -> p c t"), stg[:], st_sems[key])

            for c in range(16):
                bk, t_m = fm_group(C_GP + c * 128)
                ph.wait("act", t_m, st_tok["g"] if c == 0 else None)
                t_o = ph.done("act", ACT.activation(out=g_st[:, c, :], in_=pf[:, bk, :], func=AF.Sigmoid))
                mm_free[bk] = t_o
            ph.wait("pool", t_o)
            st_tok["g"] = ph.dma("pool", scr["gT"][:, :, r0:r0 + T].rearrange("c p t -> p c t"), g_st[:], st_sems["g"])
            for b in range(4):
                bk, t_m = tm_group(b, C_OM, 512)
                ph.wait("act", t_m, st_tok["om"] if b == 0 else None)
                t_o = ph.done("act", ACT.activation(out=om_st[:, b, :], in_=pf[:, bk, :], func=AF.Sigmoid))
                mm_free[bk] = t_o
            ph.wait("pool", t_o)
            st_tok["om"] = ph.dma("pool", scr["om"][r0:r0 + T, :].rearrange("(b p) n -> p b n", p=128), om_st[:], st_sems["om"])

            if seq_start:
                ph.wait("dve", [pre_free[c] for c in range(8)])
                t_z = ph.done("dve", DVE.memset(pre[:, :, 0:3], 0.0))
            else:
                t_z = None
            t_halo_prev = None
            for c in range(8):
                bk, t_m = fm_group(C_QK + c * 128)
                ph.wait("act", t_m, pre_free[c], t_z)
                t_cp = ph.done("act", ACT.activation(out=pre[:, c, 3:T + 3], in_=pf[:, bk, :], func=AF.Copy))
                mm_free[bk] = t_cp
                ai = state["acc"] % 2
                state["acc"] += 1
                a_ = acc[ai]
                ph.wait("dve", t_cp, t_par, acc_free[ai], t_z)
                t = ph.done("dve", DVE.tensor_scalar_mul(out=a_[:], in0=pre[:, c, 0:T], scalar1=cw[:, c * 4:c * 4 + 1]))
                for j in range(1, 4):
                    ph.wait("dve", t)
                    t = ph.done("dve", DVE.scalar_tensor_tensor(out=a_[:], in0=pre[:, c, j:j + T],
                                                                scalar=cw[:, c * 4 + j:c * 4 + j + 1], in1=a_[:],
                                                                op0=ALU.mult, op1=ALU.add))
                t_acc = t
                ph.wait("dve", t_acc)
                t_halo = ph.done("dve", DVE.tensor_copy(out=pre[:, c, 0:3], in_=pre[:, c, T:T + 3]))
                pre_free[c] = t_halo
                ph.wait("act", t_acc, st_tok["qk"] if c == 0 else None, st_tok["km"] if c == 4 else None)
                t_si = ph.done("act", ACT.activation(out=qk_st[:, c, :], in_=a_[:], func=AF.Silu, bias=cb[:, c:c + 1]))
                acc_free[ai] = t_si
                if c >= 4:
                    h = c - 4
                    for b in range(4):
                        ph.wait("pe", t_si, kmT_free)
                        t_t = ph.done("pe", PE.transpose(out=pb[:, 1, 0:128], in_=qk_st[:, c, b * 128:(b + 1) * 128],
                                                         identity=cst["identb"][:]))
                        ph.wait("dve", t_t, st_tok["km"])
                        t_k = ph.done("dve", DVE.tensor_copy(out=km_st[:, b, h * 128:(h + 1) * 128], in_=pb[:, 1, 0:128]))
                        kmT_free = t_k
            ph.wait("pool", t_si, t_k)
            st_tok["qk"] = ph.dma("pool", scr["qkT"][:, :, r0:r0 + T].rearrange("c p t -> p c t"), qk_st[:], st_sems["qk"])
            st_tok["km"] = ph.dma("pool", scr["km"][r0:r0 + T, :].rearrange("(b p) n -> p b n", p=128), km_st[:], st_sems["km"])
            hT_free[slot] = t_m
        ph.final = [v for v in st_tok.values() if v is not None]


def scratch_specs(cfg):
    n = cfg.ntok
    return {
        "qkT": ([8, 128, n], BF16), "km": ([n, 512], BF16), "vm": ([n, 512], BF16), "om": ([n, 512], BF16),
        "g12": ([n, 12], F32), "qsT": ([4, 128, n], BF16), "ksT": ([4, 128, n], BF16), "vs": ([n, 512], BF16),
        "gT": ([16, 128, n], BF16), "yaT": ([4, 128, n], BF16), "ybT": ([4, 128, n], BF16),
        "x1": ([n, D], F32), "xmid": ([n, D], F32),
    }


WEIGHT_SPECS = {
    "g_mix": [DEPTH, D], "w_in": [DEPTH, D, IN_COLS], "cwl": [DEPTH, 128, 32], "cbl": [DEPTH, 128, 8],
    "b_gates": [DEPTH, 8], "gqk": [DEPTH, 128, 2], "w_br_a": [DEPTH, 512, D], "w_br_b": [DEPTH, 512, D],
    "w_out": [DEPTH, D, D], "g_ffn": [DEPTH, D], "w_gu": [DEPTH, D, 2 * D_FF], "w_down": [DEPTH, D_FF, D],
}


def build_program(cfg, plan, ext_in=(), ext_out=()):
    nc = bass.Bass("TRN2", target_bir_lowering=False)
    W = {k: nc.dram_tensor(k, s, F32, kind="ExternalInput").ap() for k, s in WEIGHT_SPECS.items()}
    tens = {}
    tens["x"] = nc.dram_tensor("x", [cfg.ntok, D], F32, kind="ExternalInput").ap()
    tens["out"] = nc.dram_tensor("out", [cfg.ntok, D], F32, kind="ExternalOutput").ap()
    for k, (s, dt) in scratch_specs(cfg).items():
        kind = "ExternalInput" if k in ext_in else ("ExternalOutput" if k in ext_out else "Internal")
        tens[k] = nc.dram_tensor(k, s, dt, kind=kind).ap()
    for (pname, L, xin, xout) in plan:
        PHASES[pname](nc, cfg, L, tens.get(xin), W, tens, tens.get(xout))
    return nc


def host_layout_weights(inp):
    f = lambda a: np.ascontiguousarray(np.asarray(a, dtype=np.float32))
    cw = f(inp["conv_w"])
    cwl = f(cw.reshape(DEPTH, 4, 8, 128).transpose(0, 3, 2, 1).reshape(DEPTH, 128, 32))
    cbl = f(f(inp["conv_b"]).reshape(DEPTH, 8, 128).transpose(0, 2, 1))
    gq = f(inp["g_q"])
    gk = f(inp["g_k"])
    gqk = f(np.stack([np.concatenate([gq, gq], 1), np.concatenate([gk, gk], 1)], axis=2))
    return {
        "g_mix": f(inp["g_mix"]), "w_in": f(inp["w_in"]), "cwl": cwl, "cbl": cbl, "b_gates": f(inp["b_gates"]),
        "gqk": gqk, "w_br_a": f(inp["w_br_a"]), "w_br_b": f(inp["w_br_b"]), "w_out": f(inp["w_out"]),
        "g_ffn": f(inp["g_ffn"]), "w_gu": f(inp["w_gu"]), "w_down": f(inp["w_down"]),
    }


PHASES = {"p1": lambda nc, cfg, L, xin, W, tens, xout: phase1(nc, cfg, L, xin, W, tens)}


def phase2(nc, cfg, L, scr):
    with Phase(nc, f"p2l{L}") as ph:
        PE, ACT, DVE, POOL, SP = (ph.engs[k] for k in ("pe", "act", "dve", "pool", "sp"))
        NS = cfg.nseq
        qk_sb = [[ph.sb(f"qk{s}{p}", [128, 8, T], BF16) for p in range(2)] for s in range(NS)]
        km_sb = [[ph.sb(f"km{s}{p}", [128, 4, 512], BF16) for p in range(2)] for s in range(NS)]
        va_sb = [[ph.sb(f"va{s}{p}", [128, 4, 4, 129], BF16) for p in range(2)] for s in range(NS)]
        om_sb = [[ph.sb(f"om{s}{p}", [128, 4, 512], BF16) for p in range(2)] for s in range(NS)]
        g_sb = [[ph.sb(f"g{s}{p}", [128, 4, 12], F32) for p in range(2)] for s in range(NS)]
        C32 = [ph.sb(f"C32_{s}", [128, 4, 129], F32) for s in range(NS)]
        Cbf = [ph.sb(f"Cbf_{s}", [128, 4, 129], BF16) for s in range(NS)]
        ya_sb = [ph.sb(f"ya{i}", [128, 512], BF16) for i in range(2)]
        yaT_st = [ph.sb(f"yaT{s}", [128, 4, T], BF16) for s in range(NS)]
        NR = 4
        sT_sb = [ph.sb(f"sT{i}", [128, 128], BF16) for i in range(NR)]
        kw_sb = [ph.sb(f"kw{i}", [128, 128], BF16) for i in range(NR)]
        wk2 = [ph.sb(f"wk2_{i}", [128, 4], F32) for i in range(NR)]
        dtmp = [ph.sb(f"dtmp{i}", [128, 4], F32) for i in range(NR)]
        pf = ph.ps("pf", [128, 6, 512], F32)
        pb = ph.ps("pb", [128, 2, 1024], BF16)
        cst = make_consts(ph)
        maskLE, t_mask = tri_f32(ph, "maskLE", 1.0, "le")
        t_init = []
        for s in range(NS):
            t_init.append(ph.done("pool", POOL.memset(C32[s][:], 0.0)))
            t_init.append(ph.done("pool", POOL.memset(Cbf[s][:], 0.0)))
            for p in range(2):
                t_init.append(ph.done("pool", POOL.memset(va_sb[s][p][:, :, :, 128:129], 1.0)))
        t_init = t_init[-1]

        s_ld = [[ph.sem(f"ld{s}{p}") for p in range(2)] for s in range(NS)]
        s_st = [ph.sem(f"st{s}") for s in range(NS)]
        st_tok = [None] * NS
        slot_free = [[[] for p in range(2)] for s in range(NS)]
        C_tok = [[t_init] * 4 for s in range(NS)]
        Cbf_tok = [[t_init] * 4 for s in range(NS)]
        Cbf_read = [[None] * 4 for s in range(NS)]
        st_free = [None, None]
        acc_free = [None, None]
        cps_free = [None, None]
        sT_free = [None] * NR
        kw_free = [None] * NR
        wk2_free = [None] * NR
        dtmp_free = [None] * NR
        ya_free = [None, None]
        pb_free = [None, None]
        cnt = {"u": 0, "c": 0}

        ld_toks = {}

        def emit_loads(t):
            par = t % 2
            for s in range(NS):
                r0 = s * cfg.S + t * T
                ph.wait("sp", slot_free[s][par])
                sem = s_ld[s][par]
                ph.dma("sp", qk_sb[s][par][:], scr["qkT"][:, :, r0:r0 + T].rearrange("c p t -> p c t"), sem)
                ph.dma("sp", km_sb[s][par][:], scr["km"][r0:r0 + T, :].rearrange("(b p) n -> p b n", p=128), sem)
                for b in range(4):
                    ph.dma("sp", va_sb[s][par][:, b, :, 0:128],
                           scr["vm"][r0 + b * 128:r0 + (b + 1) * 128, :].rearrange("p (h e) -> p h e", h=4), sem)
                ph.dma("sp", om_sb[s][par][:], scr["om"][r0:r0 + T, :].rearrange("(b p) n -> p b n", p=128), sem)
                ld_toks[(t, s)] = ph.dma("sp", g_sb[s][par][:], scr["g12"][r0:r0 + T, :].rearrange("(b p) n -> p b n", p=128), sem)

        emit_loads(0)
        for t in range(cfg.tps):
            par = t % 2
            if t + 1 < cfg.tps:
                emit_loads(t + 1)
            ld_tok = [ld_toks[(t, s)] for s in range(NS)]
            last = [None] * NS
            for j in range(4):
                jr = slice(j * 128, (j + 1) * 128)
                for s in range(NS):
                    qk, km, va, om, g = qk_sb[s][par], km_sb[s][par], va_sb[s][par], om_sb[s][par], g_sb[s][par]
                    ci = cnt["c"] % NR
                    cnt["c"] += 1
                    ph.wait("dve", ld_tok[s], wk2_free[ci])
                    t_wk2 = ph.done("dve", DVE.tensor_tensor(out=wk2[ci][:], in0=g[:, j, 8:12], in1=g[:, j, 4:8], op=ALU.mult))
                    yi = cnt["c"] % 2
                    y_toks = []
                    for h in range(4):
                        u = cnt["u"]
                        cnt["u"] += 1
                        r = u % NR
                        b2 = u % 2
                        ph.wait("pe", ld_tok[s], st_free[b2])
                        t_st = ph.done("pe", PE.matmul(pf[:, b2, 0:128], lhsT=qk[:, 4 + h, jr], rhs=qk[:, h, jr], start=True, stop=True))
                        ph.wait("dve", t_st, t_mask, sT_free[r])
                        t_sT = ph.done("dve", DVE.scalar_tensor_tensor(out=sT_sb[r][:], in0=pf[:, b2, 0:128], scalar=g[:, j, 8 + h:9 + h],
                                                                       in1=maskLE[:], op0=ALU.mult, op1=ALU.mult))
                        st_free[b2] = t_sT
                        ph.wait("pool", ld_tok[s], t_wk2, kw_free[r])
                        t_kw = ph.done("pool", POOL.tensor_scalar_mul(out=kw_sb[r][:], in0=km[:, j, h * 128:(h + 1) * 128],
                                                                      scalar1=wk2[ci][:, h:h + 1]))
                        ph.wait("pe", Cbf_tok[s][h], t_sT, acc_free[b2])
                        PE.matmul(pf[:, 2 + b2, 0:129], lhsT=qk[:, h, jr], rhs=Cbf[s][:, h, :], start=True, stop=False)
                        t_acc = ph.done("pe", PE.matmul(pf[:, 2 + b2, 0:129], lhsT=sT_sb[r][:], rhs=va[:, j, h, :], start=False, stop=True))
                        sT_free[r] = t_acc
                        ph.wait("pe", t_kw, cps_free[b2])
                        t_cps = ph.done("pe", PE.matmul(pf[:, 4 + b2, 0:129], lhsT=kw_sb[r][:], rhs=va[:, j, h, :], start=True, stop=True))
                        kw_free[r] = t_cps
                        ph.wait("dve", t_cps, C_tok[s][h], Cbf_tok[s][h])
                        t_c = ph.done("dve", DVE.scalar_tensor_tensor(out=C32[s][:, h, :], in0=C32[s][:, h, :], scalar=g[:, j, 4 + h:5 + h],
                                                                      in1=pf[:, 4 + b2, 0:129], op0=ALU.mult, op1=ALU.add))
                        C_tok[s][h] = t_c
                        cps_free[b2] = t_c
                        ph.wait("act", t_c, t_acc)
                        Cbf_tok[s][h] = ph.done("act", ACT.activation(out=Cbf[s][:, h, :], in_=C32[s][:, h, :], func=AF.Copy))
                        d = dtmp[r]
                        ph.wait("dve", t_acc, dtmp_free[r])
                        t1 = ph.done("dve", DVE.tensor_tensor(out=d[:, 0:1], in0=pf[:, 2 + b2, 128:129], in1=g[:, j, h:h + 1], op=ALU.mult))
                        ph.wait("dve", t1)
                        t1 = ph.done("dve", DVE.scalar_tensor_tensor(out=d[:, 1:2], in0=d[:, 0:1], scalar=-1.0, in1=d[:, 0:1],
                                                                     op0=ALU.mult, op1=ALU.max))
                        ph.wait("dve", t1)
                        t1 = ph.done("dve", DVE.tensor_scalar_max(out=d[:, 1:2], in0=d[:, 1:2], scalar1=1.0))
                        ph.wait("dve", t1)
                        t1 = ph.done("dve", DVE.reciprocal(out=d[:, 2:3], in_=d[:, 1:2]))
                        ph.wait("dve", t1)
                        t1 = ph.done("dve", DVE.tensor_tensor(out=d[:, 3:4], in0=d[:, 2:3], in1=g[:, j, h:h + 1], op=ALU.mult))
                        ph.wait("dve", t1, ya_free[yi] if h == 0 else None)
                        t_y = ph.done("dve", DVE.scalar_tensor_tensor(out=ya_sb[yi][:, h * 128:(h + 1) * 128], in0=pf[:, 2 + b2, 0:128],
                                                                      scalar=d[:, 3:4], in1=om[:, j, h * 128:(h + 1) * 128],
                                                                      op0=ALU.mult, op1=ALU.mult))
                        acc_free[b2] = t_y
                        dtmp_free[r] = t_y
                        y_toks.append(t_y)
                        last[s] = [t_y, t_cps, t_acc, t_kw]
                    wk2_free[ci] = t_kw
                    tb = cnt["c"] % 2
                    ph.wait("pe", y_toks, cst["t_identb"], pb_free[tb])
                    for h in range(4):
                        ins = PE.transpose(out=pb[:, tb, h * 128:(h + 1) * 128], in_=ya_sb[yi][:, h * 128:(h + 1) * 128],
                                           identity=cst["identb"][:])
                    t_tp = ph.done("pe", ins)
                    ya_free[yi] = t_tp
                    ph.wait("act", t_tp, st_tok[s] if j == 0 else None)
                    t_ev = ph.done("act", ACT.activation(out=yaT_st[s][:, :, jr], in_=pb[:, tb, 0:512].rearrange("p (h l) -> p h l", h=4),
                                                         func=AF.Copy))
                    pb_free[tb] = t_ev
                    last[s].append(t_ev)
            for s in range(NS):
                r0 = s * cfg.S + t * T
                ph.wait("act", last[s][-1])
                st_tok[s] = ph.dma("act", scr["yaT"][:, :, r0:r0 + T].rearrange("c p t -> p c t"), yaT_st[s][:], s_st[s])
                slot_free[s][par] = list(last[s])
        ph.final = [x for x in st_tok if x is not None]


PHASES["p2"] = lambda nc, cfg, L, xin, W, tens, xout: phase2(nc, cfg, L, tens)


def phase3(nc, cfg, L, scr):
    with Phase(nc, f"p3l{L}") as ph:
        PE, ACT, DVE, POOL, SP = (ph.engs[k] for k in ("pe", "act", "dve", "pool", "sp"))
        S = cfg.S
        NB = S // 128
        NQT = S // T
        kT = ph.sb("kT", [128, 4, S], BF16)
        qT = ph.sb("qT", [128, 4, S], BF16)
        vv = ph.sb("vv", [128, NB, 512], BF16)
        e_sb = [ph.sb(f"e{i}", [128, T], F32) for i in range(2)]
        sp_sb = [ph.sb(f"sp{i}", [128, T], BF16) for i in range(2)]
        Ss = [ph.sb(f"Ss{i}", [128, T], BF16) for i in range(2)]
        aT_sb = [ph.sb(f"aT{i}", [128, T], BF16) for i in range(2)]
        yb_st = [ph.sb(f"yb{i}", [64, T], BF16) for i in range(2)]
        pf = ph.ps("pf", [128, 6, 512], F32)
        cst = make_consts(ph)
        identb = cst["identb"]
        negTri, t_tri = tri_bf16(ph, "negTri", -1.0, "ge")
        negOne, t_one = tri_bf16(ph, "negOne", -1.0, "all")
        nm_f = ph.sb("nm_f", [128, T], F32)
        negmask = []
        t_nm = None
        for i in range(4):
            m = ph.sb(f"negmask{i}", [128, T], BF16)
            ph.wait("pool", t_nm)
            t = ph.done("pool", POOL.memset(nm_f[:], NEG))
            ph.wait("pool", t)
            t = ph.done("pool", POOL.affine_select(out=nm_f[:], in_=nm_f[:], pattern=[[-1, T]], compare_op=ALU.is_ge,
                                                   fill=0.0, base=i * 128, channel_multiplier=1))
            ph.wait("pool", t)
            t_nm = ph.done("pool", POOL.tensor_copy(out=m[:], in_=nm_f[:]))
            negmask.append(m)
        t_consts = [cst["t_identb"], t_tri, t_one, t_nm]

        s_ld = ph.sem("ld")
        s_yb = [ph.sem("yb0"), ph.sem("yb1")]
        zfree = [None, None]
        spfree = [None, None]
        Ssfree = [None, None]
        Afree = [None, None]
        aTfree = [None, None]
        ofree = [None, None]
        ybfree = [None, None]
        prev_done = []
        gk = {"k": 0, "grp": 0}

        for s in range(cfg.nseq):
            c0 = s * S
            ph.wait("sp", prev_done)
            ph.dma("sp", kT[:], scr["ksT"][:, :, c0:c0 + S].rearrange("c p t -> p c t"), s_ld)
            ph.dma("sp", qT[:], scr["qsT"][:, :, c0:c0 + S].rearrange("c p t -> p c t"), s_ld)
            ld_tok = ph.dma("sp", vv[:], scr["vs"][c0:c0 + S, :].rearrange("(b p) n -> p b n", p=128), s_ld)
            units = []
            for h in range(SB_H):
                for qt in range(NQT):
                    kbs = list(range(4 * qt + 3, -1, -1))
                    for n, kb in enumerate(kbs):
                        units.append(dict(h=h, qt=qt, n=n, kb=kb, last=(n == len(kbs) - 1), diag=(kb >= 4 * qt),
                                          i=kb - 4 * qt, grp=gk["grp"]))
                    gk["grp"] += 1
            NU = len(units)
            k0 = gk["k"]

            def operands(U):
                hc = U["h"] // 2
                p0 = (U["h"] % 2) * 64
                lk = kT[p0:p0 + 64, hc, U["kb"] * 128:(U["kb"] + 1) * 128]
                rq = qT[p0:p0 + 64, hc, U["qt"] * T:(U["qt"] + 1) * T]
                return lk, rq

            def stage0(U, k):
                b = k % 2
                lk, rq = operands(U)
                ph.wait("pe", ld_tok, t_consts, zfree[b])
                ins = PE.matmul(pf[:, b, :], lhsT=lk, rhs=rq, start=True, stop=not U["diag"])
                if U["diag"]:
                    ins = PE.matmul(pf[:, b, :], lhsT=identb[:], rhs=negmask[U["i"]][:], start=False, stop=True)
                U["t_z"] = ph.done("pe", ins)
                ph.wait("act", U["t_z"])
                U["t_e"] = ph.done("act", ACT.activation(out=e_sb[b][:], in_=pf[:, b, :], func=AF.Exp))
                zfree[b] = U["t_e"]
                ph.wait("act", U["t_e"], spfree[b])
                U["t_sp"] = ph.done("act", ACT.activation(out=sp_sb[b][:], in_=e_sb[b][:], func=AF.Ln, bias=1.0))
                U["t_ss"] = None
                if not U["last"]:
                    n = U["n"]
                    dst = Ss[(n + 1) % 2]
                    ph.wait("dve", U["t_sp"], Ssfree[(n + 1) % 2])
                    if n == 0:
                        U["t_ss"] = ph.done("dve", DVE.tensor_copy(out=dst[:], in_=sp_sb[b][:]))
                    else:
                        ph.wait("dve", U["t_ssprev"])
                        U["t_ss"] = ph.done("dve", DVE.tensor_tensor(out=dst[:], in0=Ss[n % 2][:], in1=sp_sb[b][:], op=ALU.add))

            def stage1(U, k):
                b = k % 2
                n = U["n"]
                lk, rq = operands(U)
                ph.wait("pe", U["t_sp"], Afree[b], U.get("t_ssprev"))
                mms = [(lk, rq), (negTri[:], sp_sb[b][:])]
                if n > 0:
                    mms.append((negOne[:], Ss[n % 2][:]))
                if U["diag"]:
                    mms.append((identb[:], negmask[U["i"]][:]))
                for j, (l_, r_) in enumerate(mms):
                    ins = PE.matmul(pf[:, 2 + b, :], lhsT=l_, rhs=r_, start=(j == 0), stop=(j == len(mms) - 1))
                U["t_A"] = ph.done("pe", ins)
                if n > 0:
                    Ssfree[n % 2] = U["t_A"]
                spfree[b] = [U["t_A"], U["t_ss"]]
                ph.wait("act", U["t_A"], aTfree[b])
                U["t_a"] = ph.done("act", ACT.activation(out=aT_sb[b][:], in_=pf[:, 2 + b, :], func=AF.Exp))
                Afree[b] = U["t_a"]

            def stage2(U, k):
                b = k % 2
                g2 = U["grp"] % 2
                h = U["h"]
                ph.wait("pe", U["t_a"], ofree[g2] if U["n"] == 0 else None)
                U["t_av"] = ph.done("pe", PE.matmul(pf[0:64, 4 + g2, :], lhsT=vv[:, U["kb"], h * 64:(h + 1) * 64], rhs=aT_sb[b][:],
                                                    start=(U["n"] == 0), stop=U["last"]))
                aTfree[b] = U["t_av"]
                if U["last"]:
                    ph.wait("dve", U["t_av"], ybfree[g2])
                    t_ev = ph.done("dve", DVE.tensor_copy(out=yb_st[g2][:], in_=pf[0:64, 4 + g2, :]))
                    ofree[g2] = t_ev
                    r0 = c0 + U["qt"] * T
                    p0 = (h % 2) * 64
                    ph.wait("sp", t_ev)
                    ybfree[g2] = ph.dma("sp", scr["ybT"][h // 2, p0:p0 + 64, r0:r0 + T], yb_st[g2][:], s_yb[g2])
                    U["t_ev"] = t_ev

            for step in range(NU + 2):
                if step < NU:
                    U = units[step]
                    if U["n"] > 0:
                        U["t_ssprev"] = units[step - 1]["t_ss"]
                    stage0(U, k0 + step)
                if 0 <= step - 1 < NU:
                    stage1(units[step - 1], k0 + step - 1)
                if 0 <= step - 2 < NU:
                    stage2(units[step - 2], k0 + step - 2)
            gk["k"] = k0 + NU
            lastU = units[-1]
            prev_done = [lastU["t_av"], lastU["t_a"], lastU["t_ev"]]
        ph.final = [x for x in ybfree if x is not None]


PHASES["p3"] = lambda nc, cfg, L, xin, W, tens, xout: phase3(nc, cfg, L, tens)


def phase4(nc, cfg, L, x_in, W, scr):
    with Phase(nc, f"p4l{L}") as ph:
        PE, ACT, DVE, POOL, SP = (ph.engs[k] for k in ("pe", "act", "dve", "pool", "sp"))
        Wa = ph.sb("wa", [128, 4, D], BF16)
        Wb = ph.sb("wb", [128, 4, D], BF16)
        Wo = ph.sb("wo", [128, 8, D], BF16)
        ya = [ph.sb(f"ya{i}", [128, 4, T], BF16) for i in range(2)]
        yb = [ph.sb(f"yb{i}", [128, 4, T], BF16) for i in range(2)]
        gT = [ph.sb(f"gT{i}", [128, 16, T], BF16) for i in range(2)]
        xs = [ph.sb(f"xs{i}", [128, 4, D], F32) for i in range(2)]
        mixT = ph.sb("mixT", [128, 8, T], BF16)
        tmpa = [ph.sb(f"tmpa{i}", [128, T], F32) for i in range(2)]
        tmpb = [ph.sb(f"tmpb{i}", [128, T], F32) for i in range(2)]
        pf = ph.ps("pf", [128, 8, 512], F32)
        s_w = ph.sem("w")
        ph.dma("pool", Wa[:], W["w_br_a"][L].rearrange("(k p) n -> p k n", p=128), s_w)
        ph.dma("pool", Wb[:], W["w_br_b"][L].rearrange("(k p) n -> p k n", p=128), s_w)
        wo_v = W["w_out"][L].rearrange("(k p) n -> p k n", p=128)
        ph.dma("pool", Wo[:, 0:4, :], wo_v[:, 0:4, :], s_w)
        t_w = ph.dma("pool", Wo[:, 4:8, :], wo_v[:, 4:8, :], s_w)
        s_ld = [ph.sem("ld0"), ph.sem("ld1")]
        s_st = [ph.sem("st0"), ph.sem("st1")]
        slot_free = [[], []]
        st_tok = [None, None]
        ld_toks = {}

        def emit_loads(i):
            p = i % 2
            r0 = i * T
            ph.wait("sp", slot_free[p], st_tok[p])
            ph.dma("sp", ya[p][:], scr["yaT"][:, :, r0:r0 + T].rearrange("c p t -> p c t"), s_ld[p])
            ph.dma("sp", yb[p][:], scr["ybT"][:, :, r0:r0 + T].rearrange("c p t -> p c t"), s_ld[p])
            ph.dma("sp", gT[p][:, 0:8, :], scr["gT"][0:8, :, r0:r0 + T].rearrange("c p t -> p c t"), s_ld[p])
            ph.dma("sp", gT[p][:, 8:16, :], scr["gT"][8:16, :, r0:r0 + T].rearrange("c p t -> p c t"), s_ld[p])
            ld_toks[i] = ph.dma("sp", xs[p][:], x_in[r0:r0 + T, :].rearrange("(b p) d -> p b d", p=128), s_ld[p])

        bank_free = [None] * 8
        tmpa_free = [None, None]
        tmpb_free = [None, None]
        mix_free = None
        emit_loads(0)
        cnt = 0
        for i in range(cfg.ntile):
            p = i % 2
            if i + 1 < cfg.ntile:
                emit_loads(i + 1)
            ld = ld_toks[i]
            mix_toks = []
            for c in range(8):
                ba, bb = (2 * c) % 4, (2 * c + 1) % 4
                ph.wait("pe", ld, t_w, bank_free[ba])
                for k in range(4):
                    ins = PE.matmul(pf[:, ba, :], lhsT=Wa[:, k, c * 128:(c + 1) * 128], rhs=ya[p][:, k, :], start=(k == 0), stop=(k == 3))
                t_pa = ph.done("pe", ins)
                ph.wait("pe", bank_free[bb])
                for k in range(4):
                    ins = PE.matmul(pf[:, bb, :], lhsT=Wb[:, k, c * 128:(c + 1) * 128], rhs=yb[p][:, k, :], start=(k == 0), stop=(k == 3))
                t_pb = ph.done("pe", ins)
                ti = c % 2
                ph.wait("dve", t_pa, ld, tmpa_free[ti])
                t_a = ph.done("dve", DVE.tensor_tensor(out=tmpa[ti][:], in0=pf[:, ba, :], in1=gT[p][:, c, :], op=ALU.mult))
                bank_free[ba] = t_a
                ph.wait("dve", t_pb, tmpb_free[ti])
                t_b = ph.done("dve", DVE.tensor_tensor(out=tmpb[ti][:], in0=pf[:, bb, :], in1=gT[p][:, 8 + c, :], op=ALU.mult))
                bank_free[bb] = t_b
                ph.wait("pool", t_a, t_b, mix_free if c == 0 else None)
                t_m = ph.done("pool", POOL.tensor_tensor(out=mixT[:, c, :], in0=tmpa[ti][:], in1=tmpb[ti][:], op=ALU.add))
                tmpa_free[ti] = t_m
                tmpb_free[ti] = t_m
                mix_toks.append(t_m)
            for b in range(4):
                for hf in range(2):
                    bk = 4 + (cnt % 4)
                    cnt += 1
                    ph.wait("pe", mix_toks, bank_free[bk])
                    for k in range(8):
                        ins = PE.matmul(pf[:, bk, :], lhsT=mixT[:, k, b * 128:(b + 1) * 128], rhs=Wo[:, k, hf * 512:(hf + 1) * 512],
                                        start=(k == 0), stop=(k == 7))
                    t_po = ph.done("pe", ins)
                    ph.wait("dve", t_po, ld)
                    t_x = ph.done("dve", DVE.tensor_tensor(out=xs[p][:, b, hf * 512:(hf + 1) * 512], in0=pf[:, bk, :],
                                                           in1=xs[p][:, b, hf * 512:(hf + 1) * 512], op=ALU.add))
                    bank_free[bk] = t_x
            mix_free = t_po
            r0 = i * T
            ph.wait("act", t_x)
            st_tok[p] = ph.dma("act", scr["x1"][r0:r0 + T, :].rearrange("(b p) d -> p b d", p=128), xs[p][:], s_st[p])
            slot_free[p] = [t_po, t_x, t_m]
        ph.final = [x for x in st_tok if x is not None]


def phase5(nc, cfg, L, W, scr, x_out):
    with Phase(nc, f"p5l{L}") as ph:
        PE, ACT, DVE, POOL, SP = (ph.engs[k] for k in ("pe", "act", "dve", "pool", "sp"))
        NC_FF = D_FF // 128
        Wgu = ph.sb("wgu", [128, 8, 2 * D_FF], BF16)
        Wd = ph.sb("wd", [128, NC_FF, D], BF16)
        gf = ph.sb("gf", [128, D], F32)
        xs = [ph.sb(f"xs{i}", [128, 4, D], F32) for i in range(2)]
        stat = [ph.sb(f"stat{i}", [128, 16], F32) for i in range(2)]
        hbf = [ph.sb(f"hbf{i}", [128, D], BF16) for i in range(4)]
        hT = ph.sb("hT", [128, 8, T], BF16)
        act = ph.sb("act", [128, NC_FF, T], BF16)
        pf = ph.ps("pf", [128, 6, 512], F32)
        pb = ph.ps("pb", [128, 2, 1024], BF16)
        cst = make_consts(ph)
        s_w = ph.sem("w")
        s_p = ph.sem("par")
        t_par = ph.dma("sp", gf[:], W["g_ffn"][L:L + 1, :].partition_broadcast(128), s_p)
        wg_v = W["w_gu"][L].rearrange("(k p) n -> p k n", p=128)
        for k in range(8):
            ph.dma("pool", Wgu[:, k, :], wg_v[:, k, :], s_w)
        wd_v = W["w_down"][L].rearrange("(k p) n -> p k n", p=128)
        for k0 in range(0, NC_FF, 6):
            k1 = min(NC_FF, k0 + 6)
            t_w = ph.dma("pool", Wd[:, k0:k1, :], wd_v[:, k0:k1, :], s_w)
        s_ld = [ph.sem("ld0"), ph.sem("ld1")]
        s_st = [ph.sem("st0"), ph.sem("st1")]
        slot_free = [[], []]
        st_tok = [None, None]
        ld_toks = {}

        def emit_loads(i):
            p = i % 2
            r0 = i * T
            ph.wait("sp", slot_free[p], st_tok[p])
            ld_toks[i] = ph.dma("sp", xs[p][:], scr["x1"][r0:r0 + T, :].rearrange("(b p) d -> p b d", p=128), s_ld[p])

        hbf_free = [None] * 4
        stat_free = [None, None]
        hT_free = None
        tp_free = [None, None]
        h_toks = {}

        def emit_norm(i):
            p = i % 2
            st = stat[p]
            toks = []
            for b in range(4):
                ph.wait("act", ld_toks[i], stat_free[p] if b == 0 else None, hbf_free[b])
                t = ph.done("act", ACT.activation(out=hbf[b][:], in_=xs[p][:, b, :], func=AF.Square, accum_out=st[:, b:b + 1]))
                ph.wait("act", t)
                t = ph.done("act", ACT.activation(out=st[:, 4 + b:5 + b], in_=st[:, b:b + 1], func=AF.Ln, bias=EPS, scale=1.0 / D))
                ph.wait("act", t)
                t_r = ph.done("act", ACT.activation(out=st[:, 8 + b:9 + b], in_=st[:, 4 + b:5 + b], func=AF.Exp, scale=-0.5))
                ph.wait("dve", t_r, t_par, hbf_free[b])
                t_h = ph.done("dve", DVE.scalar_tensor_tensor(out=hbf[b][:], in0=xs[p][:, b, :], scalar=st[:, 8 + b:9 + b], in1=gf[:],
                                                              op0=ALU.mult, op1=ALU.mult))
                toks.append(t_h)
            stat_free[p] = toks[-1]
            h_toks[i] = toks

        def emit_transposes(i):
            ready = []
            for b in range(4):
                tb = b % 2
                ph.wait("pe", h_toks[i][b], cst["t_identb"], tp_free[tb])
                for c in range(8):
                    ins = PE.transpose(out=pb[:, tb, c * 128:(c + 1) * 128], in_=hbf[b][:, c * 128:(c + 1) * 128], identity=cst["identb"][:])
                t_tp = ph.done("pe", ins)
                hbf_free[b] = t_tp
                ph.wait("act", t_tp, hT_free if b == 0 else None)
                t_e = ph.done("act", ACT.activation(out=hT[:, :, b * 128:(b + 1) * 128], in_=pb[:, tb, :].rearrange("p (c t) -> p c t", c=8),
                                                    func=AF.Copy))
                tp_free[tb] = t_e
                ready.append(t_e)
            return ready

        bank_free = [None] * 6
        sg_free = [None, None]
        act_free = None
        emit_loads(0)
        emit_norm(0)
        hT_ready = emit_transposes(0)
        cnt = 0
        for i in range(cfg.ntile):
            p = i % 2
            if i + 1 < cfg.ntile:
                emit_loads(i + 1)
            a_toks = []
            for c in range(NC_FF):
                bg_, bu_ = (2 * c) % 4, (2 * c + 1) % 4
                ph.wait("pe", hT_ready, t_w, bank_free[bg_])
                for k in range(8):
                    ins = PE.matmul(pf[:, bg_, :], lhsT=Wgu[:, k, c * 128:(c + 1) * 128], rhs=hT[:, k, :], start=(k == 0), stop=(k == 7))
                t_g = ph.done("pe", ins)
                ph.wait("pe", bank_free[bu_])
                for k in range(8):
                    ins = PE.matmul(pf[:, bu_, :], lhsT=Wgu[:, k, D_FF + c * 128:D_FF + (c + 1) * 128], rhs=hT[:, k, :],
                                    start=(k == 0), stop=(k == 7))
                t_u = ph.done("pe", ins)
                ph.wait("act", t_g, act_free if c == 0 else None)
                t_s = ph.done("act", ACT.activation(out=act[:, c, :], in_=pf[:, bg_, :], func=AF.Silu))
                bank_free[bg_] = t_s
                ph.wait("dve", t_s, t_u)
                t_a = ph.done("dve", DVE.tensor_tensor(out=act[:, c, :], in0=pf[:, bu_, :], in1=act[:, c, :], op=ALU.mult))
                bank_free[bu_] = t_a
                a_toks.append(t_a)
            hT_free = t_u
            if i + 1 < cfg.ntile:
                emit_norm(i + 1)
            for b in range(4):
                for hf in range(2):
                    bk = 4 + (cnt % 2)
                    cnt += 1
                    ph.wait("pe", a_toks, bank_free[bk])
                    for k in range(NC_FF):
                        ins = PE.matmul(pf[:, bk, :], lhsT=act[:, k, b * 128:(b + 1) * 128], rhs=Wd[:, k, hf * 512:(hf + 1) * 512],
                                        start=(k == 0), stop=(k == NC_FF - 1))
                    t_pd = ph.done("pe", ins)
                    ph.wait("dve", t_pd)
                    t_x = ph.done("dve", DVE.tensor_tensor(out=xs[p][:, b, hf * 512:(hf + 1) * 512], in0=pf[:, bk, :],
                                                           in1=xs[p][:, b, hf * 512:(hf + 1) * 512], op=ALU.add))
                    bank_free[bk] = t_x
            act_free = t_pd
            r0 = i * T
            ph.wait("act", t_x)
            st_tok[p] = ph.dma("act", x_out[r0:r0 + T, :].rearrange("(b p) d -> p b d", p=128), xs[p][:], s_st[p])
            slot_free[p] = [t_x]
            if i + 1 < cfg.ntile:
                hT_ready = emit_transposes(i + 1)
        ph.final = [x for x in st_tok if x is not None]


PHASES["p4"] = lambda nc, cfg, L, xin, W, tens, xout: phase4(nc, cfg, L, xin, W, tens)
PHASES["p5"] = lambda nc, cfg, L, xin, W, tens, xout: phase5(nc, cfg, L, W, tens, xout)


def full_plan():
    plan = []
    for L in range(DEPTH):
        xin = "x" if L == 0 else "xmid"
        xout = "xmid" if L == 0 else "out"
        plan += [("p1", L, xin, None), ("p2", L, None, None), ("p3", L, None, None), ("p4", L, xin, None), ("p5", L, None, xout)]
    return plan


_CACHE = {}


def kernel(x, g_mix, w_in, conv_w, conv_b, b_gates, g_q, g_k, w_br_a, w_br_b, w_out, g_ffn, w_gu, w_down):
    x = np.asarray(x, dtype=np.float32)
    B, S, _ = x.shape
    nseq = B // NCORES
    cfg = Cfg(nseq=nseq, S=S)
    key = (nseq, S)
    if key not in _CACHE:
        _CACHE[key] = build_program(cfg, full_plan())
    nc = _CACHE[key]
    Wd = host_layout_weights(dict(conv_w=conv_w, conv_b=conv_b, g_q=g_q, g_k=g_k, g_mix=g_mix, w_in=w_in, b_gates=b_gates,
                                  w_br_a=w_br_a, w_br_b=w_br_b, w_out=w_out, g_ffn=g_ffn, w_gu=w_gu, w_down=w_down))
    in_maps = []
    for c in range(NCORES):
        m = dict(Wd)
        m["x"] = np.ascontiguousarray(x[c * nseq:(c + 1) * nseq].reshape(nseq * S, D))
        in_maps.append(m)
    res = run_bass_kernel_spmd(nc, in_maps, core_ids=list(range(NCORES)))
    out = np.concatenate([np.asarray(r["out"], dtype=np.float32).reshape(nseq, S, D) for r in res.results], axis=0)
    return out
```

```python
import math
from contextlib import ExitStack

import numpy as np
import concourse.bass as bass
import concourse.mybir as mybir
from concourse.bass_utils import run_bass_kernel_spmd

F32 = mybir.dt.float32
BF16 = mybir.dt.bfloat16
AF = mybir.ActivationFunctionType
ALU = mybir.AluOpType

D = 1024
DEPTH = 2
NCORES = 8
ML_H = 4
SB_H = 8
D_FF = 2816
IN_COLS = 5640
EPS = 1e-6
T = 512
C_QK, C_VM, C_OM, C_G, C_QS, C_KS, C_VS, C_GP = 0, 1024, 1536, 2048, 2056, 2568, 3080, 3592
NEG = -30000.0


class Cfg:
    def __init__(self, nseq=2, S=4096):
        self.nseq = nseq
        self.S = S
        self.ntok = nseq * S
        self.ntile = self.ntok // T
        self.tps = S // T


class Sem:
    def __init__(self, h):
        self.h = h
        self.v = 0


class Phase:
    def __init__(self, nc, name):
        self.nc = nc
        self.name = name
        self.es = ExitStack()
        self.waited = {}
        self.engs = {"pe": nc.tensor, "act": nc.scalar, "dve": nc.vector, "pool": nc.gpsimd, "sp": nc.sync}
        self.esem = {}
        self.nsem = 0
        self.all_sems = []
        self.final = []

    def __enter__(self):
        self.es.__enter__()
        for e in ("pe", "act", "dve", "pool"):
            self.esem[e] = self.sem(e)
        return self

    def __exit__(self, *a):
        if a[0] is None:
            self.wait("sp", *self.final)
            self.nc.all_engine_barrier()
            for h in self.all_sems:
                self.nc.gpsimd.sem_clear(h)
            self.nc.all_engine_barrier()
        return self.es.__exit__(*a)

    def sem(self, name):
        self.nsem += 1
        h = self.es.enter_context(self.nc.semaphore(f"{self.name}_{name}_{self.nsem}"))
        self.all_sems.append(h)
        return Sem(h)

    def sb(self, name, shape, dt):
        return self.es.enter_context(self.nc.sbuf_tensor(f"{self.name}_{name}", list(shape), dt))

    def ps(self, name, shape, dt):
        return self.es.enter_context(self.nc.psum_tensor(f"{self.name}_{name}", list(shape), dt))

    def done(self, e, ins):
        s = self.esem[e]
        if s.v >= 30000:
            s = self.sem(e)
            self.esem[e] = s
        ins.then_inc(s.h, 1)
        s.v += 1
        return (s, s.v)

    def wait(self, e, *toks):
        eng = self.engs[e]
        for tok in toks:
            if tok is None:
                continue
            if isinstance(tok, (list,)):
                self.wait(e, *tok)
                continue
            s, v = tok
            key = (e, id(s))
            if self.waited.get(key, 0) >= v:
                continue
            eng.wait_ge(s.h, v)
            self.waited[key] = v

    def dma(self, q, out, in_, sem, **kw):
        ins = self.engs[q].dma_start(out=out, in_=in_, **kw)
        ins.then_inc(sem.h, 16)
        sem.v += 16
        return (sem, sem.v)


def make_consts(ph):
    P = ph.engs["pool"]
    c = {}
    f1 = ph.sb("c_f1", [128, 128], F32)
    c["identb"] = ph.sb("c_identb", [128, 128], BF16)
    t = ph.done("pool", P.memset(f1[:], 1.0))
    ph.wait("pool", t)
    t = ph.done("pool", P.affine_select(out=f1[:], in_=f1[:], pattern=[[-1, 128]], compare_op=ALU.is_equal,
                                        fill=0.0, base=0, channel_multiplier=1))
    ph.wait("pool", t)
    c["t_identb"] = ph.done("pool", P.tensor_copy(out=c["identb"][:], in_=f1[:]))
    c["_f1"] = f1
    return c


def tri_f32(ph, name, val, kind):
    P = ph.engs["pool"]
    t_ = ph.sb(name, [128, 128], F32)
    t = ph.done("pool", P.memset(t_[:], val))
    if kind != "all":
        ph.wait("pool", t)
        if kind == "le":
            pat, cm = [[1, 128]], -1
        else:
            pat, cm = [[-1, 128]], 1
        t = ph.done("pool", P.affine_select(out=t_[:], in_=t_[:], pattern=pat, compare_op=ALU.is_ge,
                                            fill=0.0, base=0, channel_multiplier=cm))
    return t_, t


def tri_bf16(ph, name, val, kind):
    P = ph.engs["pool"]
    f, t = tri_f32(ph, name + "_f", val, kind)
    b = ph.sb(name, [128, 128], BF16)
    ph.wait("pool", t)
    t = ph.done("pool", P.tensor_copy(out=b[:], in_=f[:]))
    return b, t


def phase1(nc, cfg, L, x_in, W, scr):
    with Phase(nc, f"p1l{L}") as ph:
        PE, ACT, DVE, POOL, SP = (ph.engs[k] for k in ("pe", "act", "dve", "pool", "sp"))
        Win = ph.sb("win", [128, 8, IN_COLS], BF16)
        gmix = ph.sb("gmix", [128, D], F32)
        cw = ph.sb("cw", [128, 32], F32)
        cb = ph.sb("cb", [128, 8], F32)
        bg = ph.sb("bg", [128, 8], F32)
        gqk = ph.sb("gqk", [128, 2], F32)
        xt = [ph.sb("xt0", [128, 4, D], F32)] * 2
        stat = [ph.sb(f"stat{i}", [128, 16], F32) for i in range(2)]
        hbf = [ph.sb(f"hbf{i}", [128, D], BF16) for i in range(4)]
        hT = [ph.sb("hT0", [128, 8, T], BF16)] * 2
        pre = ph.sb("pre", [128, 8, T + 3], F32)
        acc = [ph.sb(f"acc{i}", [128, T], F32) for i in range(2)]
        sq = [ph.sb(f"sq{i}", [128, T], BF16) for i in range(2)]
        rstd = [ph.sb(f"rstd{i}", [128, T], F32) for i in range(2)]
        gsb = [ph.sb(f"gsb{i}", [128, 16], F32) for i in range(2)]
        qk_st = ph.sb("qk_st", [128, 8, T], BF16)
        km_st = ph.sb("km_st", [128, 4, 512], BF16)
        vm_st = ph.sb("vm_st", [128, 4, 512], BF16)
        om_st = ph.sb("om_st", [128, 4, 512], BF16)
        vs_st = ph.sb("vs_st", [128, 4, 512], BF16)
        qs_st = ph.sb("qs_st", [128, 4, T], BF16)
        ks_st = ph.sb("ks_st", [128, 4, T], BF16)
        g_st = ph.sb("g_st", [128, 16, T], BF16)
        g12_st = ph.sb("g12_st", [128, 4, 12], F32)
        pf = ph.ps("pf", [128, 6, 512], F32)
        pb = ph.ps("pb", [128, 2, 1024], BF16)

        cst = make_consts(ph)
        negU, t_negU = tri_f32(ph, "negU", -1.0, "le")
        negO, t_negO = tri_f32(ph, "negO", -1.0, "all")
        bones = ph.sb("bones", [128, 128], BF16)
        t = ph.done("pool", POOL.memset(bones[:], 0.0))
        ph.wait("pool", t)
        POOL.memset(bones[0:64, 0:64], 1.0 / 64)
        t_bones = ph.done("pool", POOL.memset(bones[64:128, 64:128], 1.0 / 64))

        s_w = ph.sem("w")
        s_p = ph.sem("par")
        t_par = []
        t_par.append(ph.dma("sp", gmix[:], W["g_mix"][L:L + 1, :].partition_broadcast(128), s_p))
        t_par.append(ph.dma("sp", cw[:], W["cwl"][L], s_p))
        t_par.append(ph.dma("sp", cb[:], W["cbl"][L], s_p))
        t_par.append(ph.dma("sp", bg[:], W["b_gates"][L:L + 1, :].partition_broadcast(128), s_p))
        t_par.append(ph.dma("sp", gqk[:], W["gqk"][L], s_p))
        t_par = t_par[-1]
        t_w = None
        wv = W["w_in"][L].rearrange("(k p) n -> p k n", p=128)
        for k in range(8):
            t_w = ph.dma("pool", Win[:, k, :], wv[:, k, :], s_w)
        ph.wait("dve", t_par)
        t_gq = ph.done("dve", DVE.tensor_scalar_mul(out=gqk[:, 0:1], in0=gqk[:, 0:1], scalar1=0.125))

        s_x = [ph.sem("x0")] * 2
        st_sems = {k: ph.sem("st_" + k) for k in ("qk", "km", "vm", "om", "vs", "qs", "ks", "g", "g12")}
        st_tok = {k: None for k in st_sems}
        xt_free = [None]
        hT_free = [None, None]
        hbf_free = [None] * 4
        stat_free = [None, None]
        tp_free = None
        kmT_free = None
        mm_free = [None] * 4
        ss_box = [None]
        small_free = None
        acc_free = [None, None]
        sq_free = [None, None]
        rstd_free = [None, None]
        gsb_free = [None, None]
        pre_free = [None] * 8
        state = {"mm": 0, "acc": 0, "sq": 0, "gsb": 0}
        LOGS = -0.5 * math.log(128.0)

        def mm_bank():
            b = state["mm"] % 4
            state["mm"] += 1
            return b

        x_toks = {}
        h_toks = {}
        tpst = {"tp_free": None}

        def emit_xload(i):
            r0 = i * T
            ph.wait("sp", xt_free[0])
            x_toks[i] = ph.dma("sp", xt[0][:], x_in[r0:r0 + T, :].rearrange("(b p) d -> p b d", p=128), s_x[0])

        def emit_norm(i):
            slot = i % 2
            st = stat[slot]
            toks = []
            for b in range(4):
                ph.wait("act", x_toks[i], stat_free[slot] if b == 0 else None, hbf_free[b])
                t = ph.done("act", ACT.activation(out=hbf[b][:], in_=xt[0][:, b, :], func=AF.Square,
                                                  accum_out=st[:, b:b + 1]))
                ph.wait("act", t)
                t = ph.done("act", ACT.activation(out=st[:, 4 + b:5 + b], in_=st[:, b:b + 1], func=AF.Ln,
                                                  bias=EPS, scale=1.0 / D))
                ph.wait("act", t)
                t_r = ph.done("act", ACT.activation(out=st[:, 8 + b:9 + b], in_=st[:, 4 + b:5 + b], func=AF.Exp,
                                                    scale=-0.5))
                ph.wait("dve", t_r, t_par, hbf_free[b])
                t_h = ph.done("dve", DVE.scalar_tensor_tensor(out=hbf[b][:], in0=xt[0][:, b, :],
                                                              scalar=st[:, 8 + b:9 + b], in1=gmix[:],
                                                              op0=ALU.mult, op1=ALU.mult))
                toks.append(t_h)
            xt_free[0] = toks[-1]
            stat_free[slot] = toks[-1]
            h_toks[i] = toks

        def emit_transposes(i):
            slot = i % 2
            ready = []
            for b in range(4):
                ph.wait("pe", h_toks[i][b], cst["t_identb"], tpst["tp_free"])
                for c in range(8):
                    ins = PE.transpose(out=pb[:, 0, c * 128:(c + 1) * 128], in_=hbf[b][:, c * 128:(c + 1) * 128],
                                       identity=cst["identb"][:])
                t_tp = ph.done("pe", ins)
                hbf_free[b] = t_tp
                ph.wait("act", t_tp, hT_free[0] if b == 0 else None)
                t_e = ph.done("act", ACT.activation(out=hT[slot][:, :, b * 128:(b + 1) * 128],
                                                    in_=pb[:, 0, :].rearrange("p (c t) -> p c t", c=8),
                                                    func=AF.Copy))
                tpst["tp_free"] = t_e
                ready.append(t_e)
            return ready

        emit_xload(0)
        emit_norm(0)
        if cfg.ntile > 1:
            emit_xload(1)
        hT_ready_next = emit_transposes(0)
        for i in range(cfg.ntile):
            slot = i % 2
            seq_start = (i % cfg.tps == 0)
            r0 = i * T
            hT_ready = hT_ready_next
            hTs = hT[slot]

            def fm_group(col0):
                bk = mm_bank()
                ph.wait("pe", hT_ready, t_w, mm_free[bk])
                for k in range(8):
                    ins = PE.matmul(pf[:, bk, :], lhsT=Win[:, k, col0:col0 + 128], rhs=hTs[:, k, :],
                                    start=(k == 0), stop=(k == 7))
                return bk, ph.done("pe", ins)

            def tm_group(b, col0, n):
                bk = mm_bank()
                ph.wait("pe", hT_ready, t_w, mm_free[bk])
                for k in range(8):
                    ins = PE.matmul(pf[:, bk, 0:n], lhsT=hTs[:, k, b * 128:(b + 1) * 128], rhs=Win[:, k, col0:col0 + n],
                                    start=(k == 0), stop=(k == 7))
                return bk, ph.done("pe", ins)

            for b in range(4):
                ph.wait("pe", small_free)
                ph.wait("pe", hT_ready, t_w)
                for k in range(8):
                    ins = PE.matmul(pf[:, 5, 0:8], lhsT=hTs[:, k, b * 128:(b + 1) * 128], rhs=Win[:, k, C_G:C_G + 8],
                                    start=(k == 0), stop=(k == 7))
                t_g = ph.done("pe", ins)
                gi = state["gsb"] % 2
                state["gsb"] += 1
                gs = gsb[gi]
                ph.wait("dve", t_g, t_par, gsb_free[gi])
                t = ph.done("dve", DVE.tensor_tensor(out=gs[:, 0:8], in0=pf[:, 5, 0:8], in1=bg[:], op=ALU.add))
                ph.wait("act", t)
                t = ph.done("act", ACT.activation(out=gs[:, 8:12], in_=gs[:, 4:8], func=AF.Exp, scale=-1.0))
                ph.wait("act", t)
                t_sp = ph.done("act", ACT.activation(out=gs[:, 12:16], in_=gs[:, 8:12], func=AF.Ln, bias=1.0))
                bk, t_m = tm_group(b, C_VM, 512)
                ph.wait("dve", t_m, st_tok["vm"] if b == 0 else None)
                t_vm = ph.done("dve", DVE.tensor_copy(out=vm_st[:, b, :], in_=pf[:, bk, :]))
                mm_free[bk] = t_vm
                bk, t_m = tm_group(b, C_VS, 512)
                ph.wait("dve", t_m, st_tok["vs"] if b == 0 else None)
                t_vs = ph.done("dve", DVE.tensor_copy(out=vs_st[:, b, :], in_=pf[:, bk, :]))
                mm_free[bk] = t_vs
                ph.wait("pe", t_sp, t_negU, t_negO)
                PE.matmul(pf[:, 5, 8:12], lhsT=negU[:], rhs=gs[:, 12:16], start=True, stop=True)
                t_b = ph.done("pe", PE.matmul(pf[:, 5, 12:16], lhsT=negO[:], rhs=gs[:, 12:16], start=True, stop=True))
                ph.wait("act", t_b, st_tok["g12"] if b == 0 else None)
                t_e1 = ph.done("act", ACT.activation(out=g12_st[:, b, 0:8], in_=pf[:, 5, 8:16], func=AF.Exp))
                ph.wait("dve", t_b, t_e1)
                t = ph.done("dve", DVE.tensor_tensor(out=gs[:, 8:12], in0=gs[:, 0:4], in1=pf[:, 5, 8:12], op=ALU.subtract))
                ph.wait("act", t)
                t_e2 = ph.done("act", ACT.activation(out=g12_st[:, b, 8:12], in_=gs[:, 8:12], func=AF.Exp, bias=LOGS))
                small_free = [t_e1, t]
                gsb_free[gi] = t_e2
                g12_last = t_e2
            ph.wait("pool", g12_last)
            st_tok["g12"] = ph.dma("pool", scr["g12"][r0:r0 + T, :].rearrange("(b p) n -> p b n", p=128), g12_st[:], st_sems["g12"])
            ph.wait("pool", t_vm)
            st_tok["vm"] = ph.dma("pool", scr["vm"][r0:r0 + T, :].rearrange("(b p) n -> p b n", p=128), vm_st[:], st_sems["vm"])
            ph.wait("pool", t_vs)
            st_tok["vs"] = ph.dma("pool", scr["vs"][r0:r0 + T, :].rearrange("(b p) n -> p b n", p=128), vs_st[:], st_sems["vs"])

            qjobs = [(col, stg, key, gcol, cc) for (col, stg, key, gcol) in ((C_QS, qs_st, "qs", 0), (C_KS, ks_st, "ks", 1))
                     for cc in range(4)]
            pend = None

            def qk_tail(job):
                (col, stg, key, gcol, cc), bk, si, t_sq = job
                nonlocal_ss = ss_box
                ph.wait("pe", t_sq, t_bones, nonlocal_ss[0])
                t_ss = ph.done("pe", PE.matmul(pf[:, 4, :], lhsT=bones[:], rhs=sq[si][:], start=True, stop=True))
                sq_free[si] = t_ss
                ph.wait("act", t_ss, rstd_free[si])
                t_ln = ph.done("act", ACT.activation(out=rstd[si][:], in_=pf[:, 4, :], func=AF.Ln, bias=EPS))
                nonlocal_ss[0] = t_ln
                ph.wait("act", t_ln)
                t_rs = ph.done("act", ACT.activation(out=rstd[si][:], in_=rstd[si][:], func=AF.Exp, scale=-0.5))
                ph.wait("dve", t_rs, t_gq, st_tok[key] if cc == 0 else None)
                t_o = ph.done("dve", DVE.scalar_tensor_tensor(out=stg[:, cc, :], in0=pf[:, bk, :],
                                                              scalar=gqk[:, gcol:gcol + 1], in1=rstd[si][:],
                                                              op0=ALU.mult, op1=ALU.mult))
                mm_free[bk] = t_o
                rstd_free[si] = t_o
                if cc == 3:
                    ph.wait("pool", t_o)
                    st_tok[key] = ph.dma("pool", scr[key + "T"][:, :, r0:r0 + T].rearrange("c p t -> p c t"), stg[:], st_sems[key])

            for job in qjobs:
                col, stg, key, gcol, cc = job
                bk, t_m = fm_group(col + cc * 128)
                si = state["sq"] % 2
                state["sq"] += 1
                ph.wait("act", t_m, sq_free[si])
                t_sq = ph.done("act", ACT.activation(out=sq[si][:], in_=pf[:, bk, :], func=AF.Square))
                if pend is not None:
                    qk_tail(pend)
                pend = (job, bk, si, t_sq)
            qk_tail(pend)

            for c in range(16):
                bk, t_m = fm_group(C_GP + c * 128)
                ph.wait("act", t_m, st_tok["g"] if c == 0 else None)
                t_o = ph.done("act", ACT.activation(out=g_st[:, c, :], in_=pf[:, bk, :], func=AF.Sigmoid))
                mm_free[bk] = t_o
            ph.wait("pool", t_o)
            st_tok["g"] = ph.dma("pool", scr["gT"][:, :, r0:r0 + T].rearrange("c p t -> p c t"), g_st[:], st_sems["g"])
            for b in range(4):
                bk, t_m = tm_group(b, C_OM, 512)
                ph.wait("act", t_m, st_tok["om"] if b == 0 else None)
                t_o = ph.done("act", ACT.activation(out=om_st[:, b, :], in_=pf[:, bk, :], func=AF.Sigmoid))
                mm_free[bk] = t_o
            ph.wait("pool", t_o)
            st_tok["om"] = ph.dma("pool", scr["om"][r0:r0 + T, :].rearrange("(b p) n -> p b n", p=128), om_st[:], st_sems["om"])

            if i + 1 < cfg.ntile:
                emit_norm(i + 1)
                if i + 2 < cfg.ntile:
                    emit_xload(i + 2)
            if seq_start:
                ph.wait("dve", [pre_free[c] for c in range(8)])
                t_z = ph.done("dve", DVE.memset(pre[:, :, 0:3], 0.0))
            else:
                t_z = None
            t_halo_prev = None
            for c in range(8):
                bk, t_m = fm_group(C_QK + c * 128)
                ph.wait("act", t_m, pre_free[c], t_z)
                t_cp = ph.done("act", ACT.activation(out=pre[:, c, 3:T + 3], in_=pf[:, bk, :], func=AF.Copy))
                mm_free[bk] = t_cp
                ai = state["acc"] % 2
                state["acc"] += 1
                a_ = acc[ai]
                ph.wait("dve", t_cp, t_par, acc_free[ai], t_z)
                t = ph.done("dve", DVE.tensor_scalar_mul(out=a_[:], in0=pre[:, c, 0:T], scalar1=cw[:, c * 4:c * 4 + 1]))
                for j in range(1, 4):
                    ph.wait("dve", t)
                    t = ph.done("dve", DVE.scalar_tensor_tensor(out=a_[:], in0=pre[:, c, j:j + T],
                                                                scalar=cw[:, c * 4 + j:c * 4 + j + 1], in1=a_[:],
                                                                op0=ALU.mult, op1=ALU.add))
                t_acc = t
                ph.wait("dve", t_acc)
                t_halo = ph.done("dve", DVE.tensor_copy(out=pre[:, c, 0:3], in_=pre[:, c, T:T + 3]))
                pre_free[c] = t_halo
                ph.wait("act", t_acc, st_tok["qk"] if c == 0 else None, st_tok["km"] if c == 4 else None)
                t_si = ph.done("act", ACT.activation(out=qk_st[:, c, :], in_=a_[:], func=AF.Silu, bias=cb[:, c:c + 1]))
                acc_free[ai] = t_si
                if c >= 4:
                    h = c - 4
                    for b in range(4):
                        ph.wait("pe", t_si, kmT_free)
                        t_t = ph.done("pe", PE.transpose(out=pb[:, 1, 0:128], in_=qk_st[:, c, b * 128:(b + 1) * 128],
                                                         identity=cst["identb"][:]))
                        ph.wait("dve", t_t, st_tok["km"])
                        t_k = ph.done("dve", DVE.tensor_copy(out=km_st[:, b, h * 128:(h + 1) * 128], in_=pb[:, 1, 0:128]))
                        kmT_free = t_k
            ph.wait("pool", t_si, t_k)
            st_tok["qk"] = ph.dma("pool", scr["qkT"][:, :, r0:r0 + T].rearrange("c p t -> p c t"), qk_st[:], st_sems["qk"])
            st_tok["km"] = ph.dma("pool", scr["km"][r0:r0 + T, :].rearrange("(b p) n -> p b n", p=128), km_st[:], st_sems["km"])
            hT_free[0] = t_m
            if i + 1 < cfg.ntile:
                hT_ready_next = emit_transposes(i + 1)
        ph.final = [v for v in st_tok.values() if v is not None]


def scratch_specs(cfg):
    n = cfg.ntok
    return {
        "qkT": ([8, 128, n], BF16), "km": ([n, 512], BF16), "vm": ([n, 512], BF16), "om": ([n, 512], BF16),
        "g12": ([n, 12], F32), "qsT": ([4, 128, n], BF16), "ksT": ([4, 128, n], BF16), "vs": ([n, 512], BF16),
        "gT": ([16, 128, n], BF16), "yaT": ([4, 128, n], BF16), "ybT": ([4, 128, n], BF16),
        "x1": ([n, D], F32), "xmid": ([n, D], F32),
    }


WEIGHT_SPECS = {
    "g_mix": [DEPTH, D], "w_in": [DEPTH, D, IN_COLS], "cwl": [DEPTH, 128, 32], "cbl": [DEPTH, 128, 8],
    "b_gates": [DEPTH, 8], "gqk": [DEPTH, 128, 2], "w_br_a": [DEPTH, 512, D], "w_br_b": [DEPTH, 512, D],
    "w_out": [DEPTH, D, D], "g_ffn": [DEPTH, D], "w_gu": [DEPTH, D, 2 * D_FF], "w_down": [DEPTH, D_FF, D],
}


def build_program(cfg, plan, ext_in=(), ext_out=()):
    nc = bass.Bass("TRN2", target_bir_lowering=False)
    W = {k: nc.dram_tensor(k, s, F32, kind="ExternalInput").ap() for k, s in WEIGHT_SPECS.items()}
    tens = {}
    tens["x"] = nc.dram_tensor("x", [cfg.ntok, D], F32, kind="ExternalInput").ap()
    tens["out"] = nc.dram_tensor("out", [cfg.ntok, D], F32, kind="ExternalOutput").ap()
    for k, (s, dt) in scratch_specs(cfg).items():
        kind = "ExternalInput" if k in ext_in else ("ExternalOutput" if k in ext_out else "Internal")
        tens[k] = nc.dram_tensor(k, s, dt, kind=kind).ap()
    for (pname, L, xin, xout) in plan:
        PHASES[pname](nc, cfg, L, tens.get(xin), W, tens, tens.get(xout))
    return nc


def host_layout_weights(inp):
    f = lambda a: np.ascontiguousarray(np.asarray(a, dtype=np.float32))
    cw = f(inp["conv_w"])
    cwl = f(cw.reshape(DEPTH, 4, 8, 128).transpose(0, 3, 2, 1).reshape(DEPTH, 128, 32))
    cbl = f(f(inp["conv_b"]).reshape(DEPTH, 8, 128).transpose(0, 2, 1))
    gq = f(inp["g_q"])
    gk = f(inp["g_k"])
    gqk = f(np.stack([np.concatenate([gq, gq], 1), np.concatenate([gk, gk], 1)], axis=2))
    return {
        "g_mix": f(inp["g_mix"]), "w_in": f(inp["w_in"]), "cwl": cwl, "cbl": cbl, "b_gates": f(inp["b_gates"]),
        "gqk": gqk, "w_br_a": f(inp["w_br_a"]), "w_br_b": f(inp["w_br_b"]), "w_out": f(inp["w_out"]),
        "g_ffn": f(inp["g_ffn"]), "w_gu": f(inp["w_gu"]), "w_down": f(inp["w_down"]),
    }


PHASES = {"p1": lambda nc, cfg, L, xin, W, tens, xout: phase1(nc, cfg, L, xin, W, tens)}


def phase2(nc, cfg, L, scr):
    with Phase(nc, f"p2l{L}") as ph:
        PE, ACT, DVE, POOL, SP = (ph.engs[k] for k in ("pe", "act", "dve", "pool", "sp"))
        NS = cfg.nseq
        qk_sb = [[ph.sb(f"qk{s}{p}", [128, 8, T], BF16) for p in range(2)] for s in range(NS)]
        km_sb = [[ph.sb(f"km{s}{p}", [128, 4, 512], BF16) for p in range(2)] for s in range(NS)]
        va_sb = [[ph.sb(f"va{s}{p}", [128, 4, 4, 129], BF16) for p in range(2)] for s in range(NS)]
        om_sb = [[ph.sb(f"om{s}{p}", [128, 4, 512], BF16) for p in range(2)] for s in range(NS)]
        g_sb = [[ph.sb(f"g{s}{p}", [128, 4, 12], F32) for p in range(2)] for s in range(NS)]
        C32 = [ph.sb(f"C32_{s}", [128, 4, 129], F32) for s in range(NS)]
        Cbf = [ph.sb(f"Cbf_{s}", [128, 4, 129], BF16) for s in range(NS)]
        ya_sb = [ph.sb(f"ya{i}", [128, 512], BF16) for i in range(2)]
        yaT_st = [ph.sb(f"yaT{s}", [128, 4, T], BF16) for s in range(NS)]
        NR = 4
        sT_sb = [ph.sb(f"sT{i}", [128, 128], BF16) for i in range(NR)]
        kw_sb = [ph.sb(f"kw{i}", [128, 128], BF16) for i in range(NR)]
        wk2 = [ph.sb(f"wk2_{i}", [128, 4], F32) for i in range(NR)]
        dtmp = [ph.sb(f"dtmp{i}", [128, 4], F32) for i in range(NR)]
        pf = ph.ps("pf", [128, 6, 512], F32)
        pb = ph.ps("pb", [128, 2, 1024], BF16)
        cst = make_consts(ph)
        maskLE, t_mask = tri_f32(ph, "maskLE", 1.0, "le")
        t_init = []
        for s in range(NS):
            t_init.append(ph.done("pool", POOL.memset(C32[s][:], 0.0)))
            t_init.append(ph.done("pool", POOL.memset(Cbf[s][:], 0.0)))
            for p in range(2):
                t_init.append(ph.done("pool", POOL.memset(va_sb[s][p][:, :, :, 128:129], 1.0)))
        t_init = t_init[-1]

        s_ld = [[ph.sem(f"ld{s}{p}") for p in range(2)] for s in range(NS)]
        s_st = [ph.sem(f"st{s}") for s in range(NS)]
        st_tok = [None] * NS
        slot_free = [[[] for p in range(2)] for s in range(NS)]
        C_tok = [[t_init] * 4 for s in range(NS)]
        Cbf_tok = [[t_init] * 4 for s in range(NS)]
        Cbf_read = [[None] * 4 for s in range(NS)]
        st_free = [None, None]
        acc_free = [None, None]
        cps_free = [None, None]
        sT_free = [None] * NR
        kw_free = [None] * NR
        wk2_free = [None] * NR
        dtmp_free = [None] * NR
        ya_free = [None, None]
        pb_free = [None, None]
        cnt = {"u": 0, "c": 0}

        ld_toks = {}

        def emit_loads(t):
            par = t % 2
            for s in range(NS):
                r0 = s * cfg.S + t * T
                ph.wait("sp", slot_free[s][par])
                sem = s_ld[s][par]
                ph.dma("sp", qk_sb[s][par][:], scr["qkT"][:, :, r0:r0 + T].rearrange("c p t -> p c t"), sem)
                ph.dma("sp", km_sb[s][par][:], scr["km"][r0:r0 + T, :].rearrange("(b p) n -> p b n", p=128), sem)
                for b in range(4):
                    ph.dma("sp", va_sb[s][par][:, b, :, 0:128],
                           scr["vm"][r0 + b * 128:r0 + (b + 1) * 128, :].rearrange("p (h e) -> p h e", h=4), sem)
                ph.dma("sp", om_sb[s][par][:], scr["om"][r0:r0 + T, :].rearrange("(b p) n -> p b n", p=128), sem)
                ld_toks[(t, s)] = ph.dma("sp", g_sb[s][par][:], scr["g12"][r0:r0 + T, :].rearrange("(b p) n -> p b n", p=128), sem)

        emit_loads(0)
        for t in range(cfg.tps):
            par = t % 2
            if t + 1 < cfg.tps:
                emit_loads(t + 1)
            ld_tok = [ld_toks[(t, s)] for s in range(NS)]
            last = [None] * NS
            for j in range(4):
                jr = slice(j * 128, (j + 1) * 128)
                for s in range(NS):
                    qk, km, va, om, g = qk_sb[s][par], km_sb[s][par], va_sb[s][par], om_sb[s][par], g_sb[s][par]
                    ci = cnt["c"] % NR
                    cnt["c"] += 1
                    ph.wait("dve", ld_tok[s], wk2_free[ci])
                    t_wk2 = ph.done("dve", DVE.tensor_tensor(out=wk2[ci][:], in0=g[:, j, 8:12], in1=g[:, j, 4:8], op=ALU.mult))
                    yi = cnt["c"] % 2
                    y_toks = []
                    for h in range(4):
                        u = cnt["u"]
                        cnt["u"] += 1
                        r = u % NR
                        b2 = u % 2
                        ph.wait("pe", ld_tok[s], st_free[b2])
                        t_st = ph.done("pe", PE.matmul(pf[:, b2, 0:128], lhsT=qk[:, 4 + h, jr], rhs=qk[:, h, jr], start=True, stop=True))
                        ph.wait("dve", t_st, t_mask, sT_free[r])
                        t_sT = ph.done("dve", DVE.scalar_tensor_tensor(out=sT_sb[r][:], in0=pf[:, b2, 0:128], scalar=g[:, j, 8 + h:9 + h],
                                                                       in1=maskLE[:], op0=ALU.mult, op1=ALU.mult))
                        st_free[b2] = t_sT
                        ph.wait("pool", ld_tok[s], t_wk2, kw_free[r])
                        t_kw = ph.done("pool", POOL.tensor_scalar_mul(out=kw_sb[r][:], in0=km[:, j, h * 128:(h + 1) * 128],
                                                                      scalar1=wk2[ci][:, h:h + 1]))
                        ph.wait("pe", Cbf_tok[s][h], t_sT, acc_free[b2])
                        PE.matmul(pf[:, 2 + b2, 0:129], lhsT=qk[:, h, jr], rhs=Cbf[s][:, h, :], start=True, stop=False)
                        t_acc = ph.done("pe", PE.matmul(pf[:, 2 + b2, 0:129], lhsT=sT_sb[r][:], rhs=va[:, j, h, :], start=False, stop=True))
                        sT_free[r] = t_acc
                        ph.wait("pe", t_kw, cps_free[b2])
                        t_cps = ph.done("pe", PE.matmul(pf[:, 4 + b2, 0:129], lhsT=kw_sb[r][:], rhs=va[:, j, h, :], start=True, stop=True))
                        kw_free[r] = t_cps
                        ph.wait("dve", t_cps, C_tok[s][h], Cbf_tok[s][h])
                        t_c = ph.done("dve", DVE.scalar_tensor_tensor(out=C32[s][:, h, :], in0=C32[s][:, h, :], scalar=g[:, j, 4 + h:5 + h],
                                                                      in1=pf[:, 4 + b2, 0:129], op0=ALU.mult, op1=ALU.add))
                        C_tok[s][h] = t_c
                        cps_free[b2] = t_c
                        ph.wait("act", t_c, t_acc)
                        Cbf_tok[s][h] = ph.done("act", ACT.activation(out=Cbf[s][:, h, :], in_=C32[s][:, h, :], func=AF.Copy))
                        d = dtmp[r]
                        ph.wait("dve", t_acc, dtmp_free[r])
                        t1 = ph.done("dve", DVE.tensor_tensor(out=d[:, 0:1], in0=pf[:, 2 + b2, 128:129], in1=g[:, j, h:h + 1], op=ALU.mult))
                        ph.wait("dve", t1)
                        t1 = ph.done("dve", DVE.scalar_tensor_tensor(out=d[:, 1:2], in0=d[:, 0:1], scalar=-1.0, in1=d[:, 0:1],
                                                                     op0=ALU.mult, op1=ALU.max))
                        ph.wait("dve", t1)
                        t1 = ph.done("dve", DVE.tensor_scalar_max(out=d[:, 1:2], in0=d[:, 1:2], scalar1=1.0))
                        ph.wait("dve", t1)
                        t1 = ph.done("dve", DVE.reciprocal(out=d[:, 2:3], in_=d[:, 1:2]))
                        ph.wait("dve", t1)
                        t1 = ph.done("dve", DVE.tensor_tensor(out=d[:, 3:4], in0=d[:, 2:3], in1=g[:, j, h:h + 1], op=ALU.mult))
                        ph.wait("dve", t1, ya_free[yi] if h == 0 else None)
                        t_y = ph.done("dve", DVE.scalar_tensor_tensor(out=ya_sb[yi][:, h * 128:(h + 1) * 128], in0=pf[:, 2 + b2, 0:128],
                                                                      scalar=d[:, 3:4], in1=om[:, j, h * 128:(h + 1) * 128],
                                                                      op0=ALU.mult, op1=ALU.mult))
                        acc_free[b2] = t_y
                        dtmp_free[r] = t_y
                        y_toks.append(t_y)
                        last[s] = [t_y, t_cps, t_acc, t_kw]
                    wk2_free[ci] = t_kw
                    tb = cnt["c"] % 2
                    ph.wait("pe", y_toks, cst["t_identb"], pb_free[tb])
                    for h in range(4):
                        ins = PE.transpose(out=pb[:, tb, h * 128:(h + 1) * 128], in_=ya_sb[yi][:, h * 128:(h + 1) * 128],
                                           identity=cst["identb"][:])
                    t_tp = ph.done("pe", ins)
                    ya_free[yi] = t_tp
                    ph.wait("act", t_tp, st_tok[s] if j == 0 else None)
                    t_ev = ph.done("act", ACT.activation(out=yaT_st[s][:, :, jr], in_=pb[:, tb, 0:512].rearrange("p (h l) -> p h l", h=4),
                                                         func=AF.Copy))
                    pb_free[tb] = t_ev
                    last[s].append(t_ev)
            for s in range(NS):
                r0 = s * cfg.S + t * T
                ph.wait("act", last[s][-1])
                st_tok[s] = ph.dma("act", scr["yaT"][:, :, r0:r0 + T].rearrange("c p t -> p c t"), yaT_st[s][:], s_st[s])
                slot_free[s][par] = list(last[s])
        ph.final = [x for x in st_tok if x is not None]


PHASES["p2"] = lambda nc, cfg, L, xin, W, tens, xout: phase2(nc, cfg, L, tens)


def phase3(nc, cfg, L, scr):
    with Phase(nc, f"p3l{L}") as ph:
        PE, ACT, DVE, POOL, SP = (ph.engs[k] for k in ("pe", "act", "dve", "pool", "sp"))
        S = cfg.S
        NB = S // 128
        NQT = S // T
        kT = ph.sb("kT", [128, 4, S], BF16)
        qT = ph.sb("qT", [128, 4, S], BF16)
        vv = ph.sb("vv", [128, NB, 512], BF16)
        e_sb = [ph.sb(f"e{i}", [128, 2, T], F32) for i in range(2)]
        sp_sb = [ph.sb(f"sp{i}", [128, 2, T], BF16) for i in range(2)]
        Ss = [ph.sb(f"Ss{i}", [128, T], BF16) for i in range(2)]
        Stmp = ph.sb("Stmp", [128, T], BF16)
        aT_sb = [ph.sb(f"aT{i}", [128, 2, T], BF16) for i in range(2)]
        yb_st = [ph.sb(f"yb{i}", [64, T], BF16) for i in range(2)]
        pf = ph.ps("pf", [128, 8, 512], F32)
        cst = make_consts(ph)
        identb = cst["identb"]
        negTri, t_tri = tri_bf16(ph, "negTri", -1.0, "ge")
        negOne, t_one = tri_bf16(ph, "negOne", -1.0, "all")
        nm_f = ph.sb("nm_f", [128, T], F32)
        negmask = []
        t_nm = None
        for i in range(4):
            m = ph.sb(f"negmask{i}", [128, T], BF16)
            ph.wait("pool", t_nm)
            t = ph.done("pool", POOL.memset(nm_f[:], NEG))
            ph.wait("pool", t)
            t = ph.done("pool", POOL.affine_select(out=nm_f[:], in_=nm_f[:], pattern=[[-1, T]], compare_op=ALU.is_ge,
                                                   fill=0.0, base=i * 128, channel_multiplier=1))
            ph.wait("pool", t)
            t_nm = ph.done("pool", POOL.tensor_copy(out=m[:], in_=nm_f[:]))
            negmask.append(m)
        t_consts = [cst["t_identb"], t_tri, t_one, t_nm]

        s_ld = ph.sem("ld")
        s_yb = [ph.sem("yb0"), ph.sem("yb1")]
        zfree = [None, None]
        spfree = [None, None]
        Ssfree = [None, None]
        Stmp_free = [None]
        Afree = [None]
        aTfree = [None, None]
        ofree = [None, None]
        ybfree = [None, None]
        prev_done = []
        gk = {"k": 0, "grp": 0}

        for s in range(cfg.nseq):
            c0 = s * S
            ph.wait("sp", prev_done)
            ph.dma("sp", kT[:], scr["ksT"][:, :, c0:c0 + S].rearrange("c p t -> p c t"), s_ld)
            ph.dma("sp", qT[:], scr["qsT"][:, :, c0:c0 + S].rearrange("c p t -> p c t"), s_ld)
            ld_tok = ph.dma("sp", vv[:], scr["vs"][c0:c0 + S, :].rearrange("(b p) n -> p b n", p=128), s_ld)
            units = []
            for h in range(SB_H):
                for qt in range(NQT):
                    npair = 2 * (qt + 1)
                    for m in range(npair):
                        kb = 4 * qt + 3 - 2 * m
                        units.append(dict(h=h, qt=qt, m=m, kb=kb, last=(m == npair - 1), diag=(m < 2),
                                          i=kb - 4 * qt, grp=gk["grp"]))
                    gk["grp"] += 1
            NU = len(units)
            k0 = gk["k"]

            def operands(U, j):
                hc = U["h"] // 2
                p0 = (U["h"] % 2) * 64
                kb = U["kb"] - j
                lk = kT[p0:p0 + 64, hc, kb * 128:(kb + 1) * 128]
                rq = qT[p0:p0 + 64, hc, U["qt"] * T:(U["qt"] + 1) * T]
                return lk, rq

            def stage0(U, k):
                b = k % 2
                ph.wait("pe", ld_tok, t_consts, zfree[b])
                for j in range(2):
                    lk, rq = operands(U, j)
                    ins = PE.matmul(pf[:, 2 * b + j, :], lhsT=lk, rhs=rq, start=True, stop=not U["diag"])
                    if U["diag"]:
                        ins = PE.matmul(pf[:, 2 * b + j, :], lhsT=identb[:], rhs=negmask[U["i"] - j][:], start=False, stop=True)
                U["t_z"] = ph.done("pe", ins)
                ph.wait("act", U["t_z"])
                U["t_e"] = ph.done("act", ACT.activation(out=e_sb[b][:], in_=pf[:, 2 * b:2 * b + 2, :], func=AF.Exp))
                zfree[b] = U["t_e"]
                ph.wait("act", U["t_e"], spfree[b])
                U["t_sp"] = ph.done("act", ACT.activation(out=sp_sb[b][:], in_=e_sb[b][:], func=AF.Ln, bias=1.0))
                U["t_ss"] = None
                if not U["last"]:
                    m = U["m"]
                    dst = Ss[(m + 1) % 2]
                    ph.wait("dve", U["t_sp"], Ssfree[(m + 1) % 2])
                    if m == 0:
                        U["t_ss"] = ph.done("dve", DVE.tensor_tensor(out=dst[:], in0=sp_sb[b][:, 0, :], in1=sp_sb[b][:, 1, :], op=ALU.add))
                    else:
                        ph.wait("dve", U["t_ssprev"], Stmp_free[0])
                        t = ph.done("dve", DVE.tensor_tensor(out=Stmp[:], in0=Ss[m % 2][:], in1=sp_sb[b][:, 0, :], op=ALU.add))
                        ph.wait("dve", t)
                        U["t_ss"] = ph.done("dve", DVE.tensor_tensor(out=dst[:], in0=Stmp[:], in1=sp_sb[b][:, 1, :], op=ALU.add))
                        Stmp_free[0] = U["t_ss"]

            def stage1(U, k):
                b = k % 2
                m = U["m"]
                ph.wait("pe", U["t_sp"], Afree[0], U.get("t_ssprev"))
                for j in range(2):
                    lk, rq = operands(U, j)
                    mms = [(lk, rq), (negTri[:], sp_sb[b][:, j, :])]
                    if j == 1:
                        mms.append((negOne[:], sp_sb[b][:, 0, :]))
                    if m > 0:
                        mms.append((negOne[:], Ss[m % 2][:]))
                    if U["diag"]:
                        mms.append((identb[:], negmask[U["i"] - j][:]))
                    for jj, (l_, r_) in enumerate(mms):
                        ins = PE.matmul(pf[:, 4 + j, :], lhsT=l_, rhs=r_, start=(jj == 0), stop=(jj == len(mms) - 1))
                U["t_A"] = ph.done("pe", ins)
                if m > 0:
                    Ssfree[m % 2] = U["t_A"]
                spfree[b] = [U["t_A"], U["t_ss"]]
                ph.wait("act", U["t_A"], aTfree[b])
                U["t_a"] = ph.done("act", ACT.activation(out=aT_sb[b][:], in_=pf[:, 4:6, :], func=AF.Exp))
                Afree[0] = U["t_a"]

            def stage2(U, k):
                b = k % 2
                g2 = U["grp"] % 2
                h = U["h"]
                ph.wait("pe", U["t_a"], ofree[g2] if U["m"] == 0 else None)
                for j in range(2):
                    ins = PE.matmul(pf[0:64, 6 + g2, :], lhsT=vv[:, U["kb"] - j, h * 64:(h + 1) * 64], rhs=aT_sb[b][:, j, :],
                                    start=(U["m"] == 0 and j == 0), stop=(U["last"] and j == 1))
                U["t_av"] = ph.done("pe", ins)
                aTfree[b] = U["t_av"]
                if U["last"]:
                    ph.wait("dve", U["t_av"], ybfree[g2])
                    t_ev = ph.done("dve", DVE.tensor_copy(out=yb_st[g2][:], in_=pf[0:64, 6 + g2, :]))
                    ofree[g2] = t_ev
                    r0 = c0 + U["qt"] * T
                    p0 = (h % 2) * 64
                    ph.wait("sp", t_ev)
                    ybfree[g2] = ph.dma("sp", scr["ybT"][h // 2, p0:p0 + 64, r0:r0 + T], yb_st[g2][:], s_yb[g2])
                    U["t_ev"] = t_ev

            for step in range(NU + 2):
                if step < NU:
                    U = units[step]
                    if U["m"] > 0:
                        U["t_ssprev"] = units[step - 1]["t_ss"]
                    stage0(U, k0 + step)
                if 0 <= step - 1 < NU:
                    stage1(units[step - 1], k0 + step - 1)
                if 0 <= step - 2 < NU:
                    stage2(units[step - 2], k0 + step - 2)
            gk["k"] = k0 + NU
            lastU = units[-1]
            prev_done = [lastU["t_av"], lastU["t_a"], lastU["t_ev"]]
        ph.final = [x for x in ybfree if x is not None]


PHASES["p3"] = lambda nc, cfg, L, xin, W, tens, xout: phase3(nc, cfg, L, tens)


def phase4(nc, cfg, L, x_in, W, scr):
    with Phase(nc, f"p4l{L}") as ph:
        PE, ACT, DVE, POOL, SP = (ph.engs[k] for k in ("pe", "act", "dve", "pool", "sp"))
        Wa = ph.sb("wa", [128, 4, D], BF16)
        Wb = ph.sb("wb", [128, 4, D], BF16)
        Wo = ph.sb("wo", [128, 8, D], BF16)
        ya = [ph.sb(f"ya{i}", [128, 4, T], BF16) for i in range(2)]
        yb = [ph.sb(f"yb{i}", [128, 4, T], BF16) for i in range(2)]
        gT = [ph.sb(f"gT{i}", [128, 16, T], BF16) for i in range(2)]
        xs = [ph.sb(f"xs{i}", [128, 4, D], F32) for i in range(2)]
        mixT = ph.sb("mixT", [128, 8, T], BF16)
        tmpa = [ph.sb(f"tmpa{i}", [128, T], F32) for i in range(2)]
        tmpb = [ph.sb(f"tmpb{i}", [128, T], F32) for i in range(2)]
        pf = ph.ps("pf", [128, 8, 512], F32)
        s_w = ph.sem("w")
        ph.dma("pool", Wa[:], W["w_br_a"][L].rearrange("(k p) n -> p k n", p=128), s_w)
        ph.dma("pool", Wb[:], W["w_br_b"][L].rearrange("(k p) n -> p k n", p=128), s_w)
        wo_v = W["w_out"][L].rearrange("(k p) n -> p k n", p=128)
        ph.dma("pool", Wo[:, 0:4, :], wo_v[:, 0:4, :], s_w)
        t_w = ph.dma("pool", Wo[:, 4:8, :], wo_v[:, 4:8, :], s_w)
        s_ld = [ph.sem("ld0"), ph.sem("ld1")]
        s_st = [ph.sem("st0"), ph.sem("st1")]
        slot_free = [[], []]
        st_tok = [None, None]
        ld_toks = {}

        def emit_loads(i):
            p = i % 2
            r0 = i * T
            ph.wait("sp", slot_free[p], st_tok[p])
            ph.dma("sp", ya[p][:], scr["yaT"][:, :, r0:r0 + T].rearrange("c p t -> p c t"), s_ld[p])
            ph.dma("sp", yb[p][:], scr["ybT"][:, :, r0:r0 + T].rearrange("c p t -> p c t"), s_ld[p])
            ph.dma("sp", gT[p][:, 0:8, :], scr["gT"][0:8, :, r0:r0 + T].rearrange("c p t -> p c t"), s_ld[p])
            ph.dma("sp", gT[p][:, 8:16, :], scr["gT"][8:16, :, r0:r0 + T].rearrange("c p t -> p c t"), s_ld[p])
            ld_toks[i] = ph.dma("sp", xs[p][:], x_in[r0:r0 + T, :].rearrange("(b p) d -> p b d", p=128), s_ld[p])

        bank_free = [None] * 8
        tmpa_free = [None, None]
        tmpb_free = [None, None]
        mix_free = None
        emit_loads(0)
        cnt = 0
        for i in range(cfg.ntile):
            p = i % 2
            if i + 1 < cfg.ntile:
                emit_loads(i + 1)
            ld = ld_toks[i]
            mix_toks = []
            for c in range(8):
                ba, bb = (2 * c) % 4, (2 * c + 1) % 4
                ph.wait("pe", ld, t_w, bank_free[ba])
                for k in range(4):
                    ins = PE.matmul(pf[:, ba, :], lhsT=Wa[:, k, c * 128:(c + 1) * 128], rhs=ya[p][:, k, :], start=(k == 0), stop=(k == 3))
                t_pa = ph.done("pe", ins)
                ph.wait("pe", bank_free[bb])
                for k in range(4):
                    ins = PE.matmul(pf[:, bb, :], lhsT=Wb[:, k, c * 128:(c + 1) * 128], rhs=yb[p][:, k, :], start=(k == 0), stop=(k == 3))
                t_pb = ph.done("pe", ins)
                ti = c % 2
                ph.wait("dve", t_pa, ld, tmpa_free[ti])
                t_a = ph.done("dve", DVE.tensor_tensor(out=tmpa[ti][:], in0=pf[:, ba, :], in1=gT[p][:, c, :], op=ALU.mult))
                bank_free[ba] = t_a
                ph.wait("dve", t_pb, tmpb_free[ti])
                t_b = ph.done("dve", DVE.tensor_tensor(out=tmpb[ti][:], in0=pf[:, bb, :], in1=gT[p][:, 8 + c, :], op=ALU.mult))
                bank_free[bb] = t_b
                ph.wait("pool", t_a, t_b, mix_free if c == 0 else None)
                t_m = ph.done("pool", POOL.tensor_tensor(out=mixT[:, c, :], in0=tmpa[ti][:], in1=tmpb[ti][:], op=ALU.add))
                tmpa_free[ti] = t_m
                tmpb_free[ti] = t_m
                mix_toks.append(t_m)
            for b in range(4):
                for hf in range(2):
                    bk = 4 + (cnt % 4)
                    cnt += 1
                    ph.wait("pe", mix_toks, bank_free[bk])
                    for k in range(8):
                        ins = PE.matmul(pf[:, bk, :], lhsT=mixT[:, k, b * 128:(b + 1) * 128], rhs=Wo[:, k, hf * 512:(hf + 1) * 512],
                                        start=(k == 0), stop=(k == 7))
                    t_po = ph.done("pe", ins)
                    ph.wait("dve", t_po, ld)
                    t_x = ph.done("dve", DVE.tensor_tensor(out=xs[p][:, b, hf * 512:(hf + 1) * 512], in0=pf[:, bk, :],
                                                           in1=xs[p][:, b, hf * 512:(hf + 1) * 512], op=ALU.add))
                    bank_free[bk] = t_x
            mix_free = t_po
            r0 = i * T
            ph.wait("act", t_x)
            st_tok[p] = ph.dma("act", scr["x1"][r0:r0 + T, :].rearrange("(b p) d -> p b d", p=128), xs[p][:], s_st[p])
            slot_free[p] = [t_po, t_x, t_m]
        ph.final = [x for x in st_tok if x is not None]


def phase5(nc, cfg, L, W, scr, x_out):
    with Phase(nc, f"p5l{L}") as ph:
        PE, ACT, DVE, POOL, SP = (ph.engs[k] for k in ("pe", "act", "dve", "pool", "sp"))
        NC_FF = D_FF // 128
        Wgu = ph.sb("wgu", [128, 8, 2 * D_FF], BF16)
        Wd = ph.sb("wd", [128, NC_FF, D], BF16)
        gf = ph.sb("gf", [128, D], F32)
        xs = [ph.sb(f"xs{i}", [128, 4, D], F32) for i in range(2)]
        stat = [ph.sb(f"stat{i}", [128, 16], F32) for i in range(2)]
        hbf = [ph.sb(f"hbf{i}", [128, D], BF16) for i in range(4)]
        hT = ph.sb("hT", [128, 8, T], BF16)
        act = ph.sb("act", [128, NC_FF, T], BF16)
        pf = ph.ps("pf", [128, 6, 512], F32)
        pb = ph.ps("pb", [128, 2, 1024], BF16)
        cst = make_consts(ph)
        s_w = ph.sem("w")
        s_p = ph.sem("par")
        t_par = ph.dma("sp", gf[:], W["g_ffn"][L:L + 1, :].partition_broadcast(128), s_p)
        wg_v = W["w_gu"][L].rearrange("(k p) n -> p k n", p=128)
        for k in range(8):
            ph.dma("pool", Wgu[:, k, :], wg_v[:, k, :], s_w)
        wd_v = W["w_down"][L].rearrange("(k p) n -> p k n", p=128)
        for k0 in range(0, NC_FF, 6):
            k1 = min(NC_FF, k0 + 6)
            t_w = ph.dma("pool", Wd[:, k0:k1, :], wd_v[:, k0:k1, :], s_w)
        s_ld = [ph.sem("ld0"), ph.sem("ld1")]
        s_st = [ph.sem("st0"), ph.sem("st1")]
        slot_free = [[], []]
        st_tok = [None, None]
        ld_toks = {}

        def emit_loads(i):
            p = i % 2
            r0 = i * T
            ph.wait("sp", slot_free[p], st_tok[p])
            ld_toks[i] = ph.dma("sp", xs[p][:], scr["x1"][r0:r0 + T, :].rearrange("(b p) d -> p b d", p=128), s_ld[p])

        hbf_free = [None] * 4
        stat_free = [None, None]
        hT_free = None
        tp_free = [None, None]
        h_toks = {}

        def emit_norm(i):
            p = i % 2
            st = stat[p]
            toks = []
            for b in range(4):
                ph.wait("act", ld_toks[i], stat_free[p] if b == 0 else None, hbf_free[b])
                t = ph.done("act", ACT.activation(out=hbf[b][:], in_=xs[p][:, b, :], func=AF.Square, accum_out=st[:, b:b + 1]))
                ph.wait("act", t)
                t = ph.done("act", ACT.activation(out=st[:, 4 + b:5 + b], in_=st[:, b:b + 1], func=AF.Ln, bias=EPS, scale=1.0 / D))
                ph.wait("act", t)
                t_r = ph.done("act", ACT.activation(out=st[:, 8 + b:9 + b], in_=st[:, 4 + b:5 + b], func=AF.Exp, scale=-0.5))
                ph.wait("dve", t_r, t_par, hbf_free[b])
                t_h = ph.done("dve", DVE.scalar_tensor_tensor(out=hbf[b][:], in0=xs[p][:, b, :], scalar=st[:, 8 + b:9 + b], in1=gf[:],
                                                              op0=ALU.mult, op1=ALU.mult))
                toks.append(t_h)
            stat_free[p] = toks[-1]
            h_toks[i] = toks

        def emit_transposes(i):
            ready = []
            for b in range(4):
                tb = b % 2
                ph.wait("pe", h_toks[i][b], cst["t_identb"], tp_free[tb])
                for c in range(8):
                    ins = PE.transpose(out=pb[:, tb, c * 128:(c + 1) * 128], in_=hbf[b][:, c * 128:(c + 1) * 128], identity=cst["identb"][:])
                t_tp = ph.done("pe", ins)
                hbf_free[b] = t_tp
                ph.wait("act", t_tp, hT_free if b == 0 else None)
                t_e = ph.done("act", ACT.activation(out=hT[:, :, b * 128:(b + 1) * 128], in_=pb[:, tb, :].rearrange("p (c t) -> p c t", c=8),
                                                    func=AF.Copy))
                tp_free[tb] = t_e
                ready.append(t_e)
            return ready

        bank_free = [None] * 6
        sg_free = [None, None]
        act_free = None
        emit_loads(0)
        emit_norm(0)
        hT_ready = emit_transposes(0)
        cnt = 0
        for i in range(cfg.ntile):
            p = i % 2
            if i + 1 < cfg.ntile:
                emit_loads(i + 1)
            a_toks = []
            for c in range(NC_FF):
                bg_, bu_ = (2 * c) % 4, (2 * c + 1) % 4
                ph.wait("pe", hT_ready, t_w, bank_free[bg_])
                for k in range(8):
                    ins = PE.matmul(pf[:, bg_, :], lhsT=Wgu[:, k, c * 128:(c + 1) * 128], rhs=hT[:, k, :], start=(k == 0), stop=(k == 7))
                t_g = ph.done("pe", ins)
                ph.wait("pe", bank_free[bu_])
                for k in range(8):
                    ins = PE.matmul(pf[:, bu_, :], lhsT=Wgu[:, k, D_FF + c * 128:D_FF + (c + 1) * 128], rhs=hT[:, k, :],
                                    start=(k == 0), stop=(k == 7))
                t_u = ph.done("pe", ins)
                ph.wait("act", t_g, act_free if c == 0 else None)
                t_s = ph.done("act", ACT.activation(out=act[:, c, :], in_=pf[:, bg_, :], func=AF.Silu))
                bank_free[bg_] = t_s
                ph.wait("dve", t_s, t_u)
                t_a = ph.done("dve", DVE.tensor_tensor(out=act[:, c, :], in0=pf[:, bu_, :], in1=act[:, c, :], op=ALU.mult))
                bank_free[bu_] = t_a
                a_toks.append(t_a)
            hT_free = t_u
            if i + 1 < cfg.ntile:
                emit_norm(i + 1)
            for b in range(4):
                for hf in range(2):
                    bk = 4 + (cnt % 2)
                    cnt += 1
                    ph.wait("pe", a_toks, bank_free[bk])
                    for k in range(NC_FF):
                        ins = PE.matmul(pf[:, bk, :], lhsT=act[:, k, b * 128:(b + 1) * 128], rhs=Wd[:, k, hf * 512:(hf + 1) * 512],
                                        start=(k == 0), stop=(k == NC_FF - 1))
                    t_pd = ph.done("pe", ins)
                    ph.wait("dve", t_pd)
                    t_x = ph.done("dve", DVE.tensor_tensor(out=xs[p][:, b, hf * 512:(hf + 1) * 512], in0=pf[:, bk, :],
                                                           in1=xs[p][:, b, hf * 512:(hf + 1) * 512], op=ALU.add))
                    bank_free[bk] = t_x
            act_free = t_pd
            r0 = i * T
            ph.wait("act", t_x)
            st_tok[p] = ph.dma("act", x_out[r0:r0 + T, :].rearrange("(b p) d -> p b d", p=128), xs[p][:], s_st[p])
            slot_free[p] = [t_x]
            if i + 1 < cfg.ntile:
                hT_ready = emit_transposes(i + 1)
        ph.final = [x for x in st_tok if x is not None]


PHASES["p4"] = lambda nc, cfg, L, xin, W, tens, xout: phase4(nc, cfg, L, xin, W, tens)
PHASES["p5"] = lambda nc, cfg, L, xin, W, tens, xout: phase5(nc, cfg, L, W, tens, xout)


def full_plan():
    plan = []
    for L in range(DEPTH):
        xin = "x" if L == 0 else "xmid"
        xout = "xmid" if L == 0 else "out"
        plan += [("p1", L, xin, None), ("p2", L, None, None), ("p3", L, None, None), ("p4", L, xin, None), ("p5", L, None, xout)]
    return plan


_CACHE = {}


def kernel(x, g_mix, w_in, conv_w, conv_b, b_gates, g_q, g_k, w_br_a, w_br_b, w_out, g_ffn, w_gu, w_down):
    x = np.asarray(x, dtype=np.float32)
    B, S, _ = x.shape
    nseq = B // NCORES
    cfg = Cfg(nseq=nseq, S=S)
    key = (nseq, S)
    if key not in _CACHE:
        _CACHE[key] = build_program(cfg, full_plan())
    nc = _CACHE[key]
    Wd = host_layout_weights(dict(conv_w=conv_w, conv_b=conv_b, g_q=g_q, g_k=g_k, g_mix=g_mix, w_in=w_in, b_gates=b_gates,
                                  w_br_a=w_br_a, w_br_b=w_br_b, w_out=w_out, g_ffn=g_ffn, w_gu=w_gu, w_down=w_down))
    in_maps = []
    for c in range(NCORES):
        m = dict(Wd)
        m["x"] = np.ascontiguousarray(x[c * nseq:(c + 1) * nseq].reshape(nseq * S, D))
        in_maps.append(m)
    res = run_bass_kernel_spmd(nc, in_maps, core_ids=list(range(NCORES)))
    out = np.concatenate([np.asarray(r["out"], dtype=np.float32).reshape(nseq, S, D) for r in res.results], axis=0)
    return out
```

```python
import math
from contextlib import ExitStack

import numpy as np
import concourse.bass as bass
import concourse.mybir as mybir
from concourse.bass_utils import run_bass_kernel_spmd

F32 = mybir.dt.float32
BF16 = mybir.dt.bfloat16
AF = mybir.ActivationFunctionType
ALU = mybir.AluOpType

D = 1024
DEPTH = 2
NCORES = 8
ML_H = 4
SB_H = 8
D_FF = 2816
IN_COLS = 5640
EPS = 1e-6
T = 512
C_QK, C_VM, C_OM, C_G, C_QS, C_KS, C_VS, C_GP = 0, 1024, 1536, 2048, 2056, 2568, 3080, 3592
NEG = -30000.0


class Cfg:
    def __init__(self, nseq=2, S=4096):
        self.nseq = nseq
        self.S = S
        self.ntok = nseq * S
        self.ntile = self.ntok // T
        self.tps = S // T


class Sem:
    def __init__(self, h):
        self.h = h
        self.v = 0


class Phase:
    def __init__(self, nc, name):
        self.nc = nc
        self.name = name
        self.es = ExitStack()
        self.waited = {}
        self.engs = {"pe": nc.tensor, "act": nc.scalar, "dve": nc.vector, "pool": nc.gpsimd, "sp": nc.sync}
        self.esem = {}
        self.nsem = 0
        self.all_sems = []
        self.final = []

    def __enter__(self):
        self.es.__enter__()
        for e in ("pe", "act", "dve", "pool"):
            self.esem[e] = self.sem(e)
        return self

    def __exit__(self, *a):
        if a[0] is None:
            self.wait("sp", *self.final)
            self.nc.all_engine_barrier()
            for h in self.all_sems:
                self.nc.gpsimd.sem_clear(h)
            self.nc.all_engine_barrier()
        return self.es.__exit__(*a)

    def sem(self, name):
        self.nsem += 1
        h = self.es.enter_context(self.nc.semaphore(f"{self.name}_{name}_{self.nsem}"))
        self.all_sems.append(h)
        return Sem(h)

    def sb(self, name, shape, dt):
        return self.es.enter_context(self.nc.sbuf_tensor(f"{self.name}_{name}", list(shape), dt))

    def ps(self, name, shape, dt):
        return self.es.enter_context(self.nc.psum_tensor(f"{self.name}_{name}", list(shape), dt))

    def done(self, e, ins):
        s = self.esem[e]
        if s.v >= 30000:
            s = self.sem(e)
            self.esem[e] = s
        ins.then_inc(s.h, 1)
        s.v += 1
        return (s, s.v)

    def wait(self, e, *toks):
        eng = self.engs[e]
        for tok in toks:
            if tok is None:
                continue
            if isinstance(tok, (list,)):
                self.wait(e, *tok)
                continue
            s, v = tok
            key = (e, id(s))
            if self.waited.get(key, 0) >= v:
                continue
            eng.wait_ge(s.h, v)
            self.waited[key] = v

    def dma(self, q, out, in_, sem, **kw):
        ins = self.engs[q].dma_start(out=out, in_=in_, **kw)
        ins.then_inc(sem.h, 16)
        sem.v += 16
        return (sem, sem.v)


def make_consts(ph):
    P = ph.engs["pool"]
    c = {}
    f1 = ph.sb("c_f1", [128, 128], F32)
    c["identb"] = ph.sb("c_identb", [128, 128], BF16)
    t = ph.done("pool", P.memset(f1[:], 1.0))
    ph.wait("pool", t)
    t = ph.done("pool", P.affine_select(out=f1[:], in_=f1[:], pattern=[[-1, 128]], compare_op=ALU.is_equal,
                                        fill=0.0, base=0, channel_multiplier=1))
    ph.wait("pool", t)
    c["t_identb"] = ph.done("pool", P.tensor_copy(out=c["identb"][:], in_=f1[:]))
    c["_f1"] = f1
    return c


def tri_f32(ph, name, val, kind):
    P = ph.engs["pool"]
    t_ = ph.sb(name, [128, 128], F32)
    t = ph.done("pool", P.memset(t_[:], val))
    if kind != "all":
        ph.wait("pool", t)
        if kind == "le":
            pat, cm = [[1, 128]], -1
        else:
            pat, cm = [[-1, 128]], 1
        t = ph.done("pool", P.affine_select(out=t_[:], in_=t_[:], pattern=pat, compare_op=ALU.is_ge,
                                            fill=0.0, base=0, channel_multiplier=cm))
    return t_, t


def tri_bf16(ph, name, val, kind):
    P = ph.engs["pool"]
    f, t = tri_f32(ph, name + "_f", val, kind)
    b = ph.sb(name, [128, 128], BF16)
    ph.wait("pool", t)
    t = ph.done("pool", P.tensor_copy(out=b[:], in_=f[:]))
    return b, t


def phase1(nc, cfg, L, x_in, W, scr):
    with Phase(nc, f"p1l{L}") as ph:
        PE, ACT, DVE, POOL, SP = (ph.engs[k] for k in ("pe", "act", "dve", "pool", "sp"))
        Win = ph.sb("win", [128, 8, IN_COLS], BF16)
        gmix = ph.sb("gmix", [128, D], F32)
        cw = ph.sb("cw", [128, 32], F32)
        cb = ph.sb("cb", [128, 8], F32)
        bg = ph.sb("bg", [128, 8], F32)
        gqk = ph.sb("gqk", [128, 2], F32)
        xt = [ph.sb("xt0", [128, 4, D], F32)] * 2
        stat = [ph.sb(f"stat{i}", [128, 16], F32) for i in range(2)]
        hbf = [ph.sb(f"hbf{i}", [128, D], BF16) for i in range(4)]
        hT = [ph.sb("hT0", [128, 8, T], BF16)] * 2
        pre = ph.sb("pre", [128, 8, T + 3], F32)
        acc = [ph.sb(f"acc{i}", [128, T], F32) for i in range(2)]
        sq = [ph.sb(f"sq{i}", [128, T], BF16) for i in range(2)]
        rstd = [ph.sb(f"rstd{i}", [128, T], F32) for i in range(2)]
        gsb = [ph.sb(f"gsb{i}", [128, 64], F32) for i in range(2)]
        bg4 = ph.sb("bg4", [128, 32], F32)
        qk_st = ph.sb("qk_st", [128, 8, T], BF16)
        km_st = ph.sb("km_st", [128, 4, 512], BF16)
        vm_st = ph.sb("vm_st", [128, 4, 512], BF16)
        om_st = ph.sb("om_st", [128, 4, 512], BF16)
        vs_st = ph.sb("vs_st", [128, 4, 512], BF16)
        qs_st = ph.sb("qs_st", [128, 4, T], BF16)
        ks_st = ph.sb("ks_st", [128, 4, T], BF16)
        g_st = ph.sb("g_st", [128, 16, T], BF16)
        g12_st = ph.sb("g12_st", [128, 4, 12], F32)
        pf = ph.ps("pf", [128, 6, 512], F32)
        pb = ph.ps("pb", [128, 2, 1024], BF16)

        cst = make_consts(ph)
        negU, t_negU = tri_f32(ph, "negU", -1.0, "le")
        negO, t_negO = tri_f32(ph, "negO", -1.0, "all")
        bones = ph.sb("bones", [128, 128], BF16)
        t = ph.done("pool", POOL.memset(bones[:], 0.0))
        ph.wait("pool", t)
        POOL.memset(bones[0:64, 0:64], 1.0 / 64)
        t_bones = ph.done("pool", POOL.memset(bones[64:128, 64:128], 1.0 / 64))

        s_w = ph.sem("w")
        s_p = ph.sem("par")
        t_par = []
        t_par.append(ph.dma("sp", gmix[:], W["g_mix"][L:L + 1, :].partition_broadcast(128), s_p))
        t_par.append(ph.dma("sp", cw[:], W["cwl"][L], s_p))
        t_par.append(ph.dma("sp", cb[:], W["cbl"][L], s_p))
        t_par.append(ph.dma("sp", bg[:], W["b_gates"][L:L + 1, :].partition_broadcast(128), s_p))
        for b in range(4):
            t_par.append(ph.dma("sp", bg4[:, b * 8:(b + 1) * 8], W["b_gates"][L:L + 1, :].partition_broadcast(128), s_p))
        t_par.append(ph.dma("sp", gqk[:], W["gqk"][L], s_p))
        t_par = t_par[-1]
        t_w = None
        wv = W["w_in"][L].rearrange("(k p) n -> p k n", p=128)
        for k in range(8):
            t_w = ph.dma("pool", Win[:, k, :], wv[:, k, :], s_w)
        ph.wait("dve", t_par)
        t_gq = ph.done("dve", DVE.tensor_scalar_mul(out=gqk[:, 0:1], in0=gqk[:, 0:1], scalar1=0.125))

        s_x = [ph.sem("x0")] * 2
        st_sems = {k: ph.sem("st_" + k) for k in ("qk", "km", "vm", "om", "vs", "qs", "ks", "g", "g12")}
        st_tok = {k: None for k in st_sems}
        xt_free = [None]
        hT_free = [None, None]
        hbf_free = [None] * 4
        stat_free = [None, None]
        tp_free = None
        kmT_free = None
        mm_free = [None] * 4
        ss_box = [None]
        small_free = None
        acc_free = [None, None]
        sq_free = [None, None]
        rstd_free = [None, None]
        gsb_free = [None, None]
        pre_free = [None] * 8
        state = {"mm": 0, "acc": 0, "sq": 0, "gsb": 0}
        LOGS = -0.5 * math.log(128.0)

        def mm_bank():
            b = state["mm"] % 4
            state["mm"] += 1
            return b

        x_toks = {}
        h_toks = {}
        tpst = {"tp_free": None, 0: None, 1: None}

        def emit_xload(i):
            r0 = i * T
            ph.wait("sp", xt_free[0])
            x_toks[i] = ph.dma("sp", xt[0][:], x_in[r0:r0 + T, :].rearrange("(b p) d -> p b d", p=128), s_x[0])

        def emit_norm(i):
            slot = i % 2
            st = stat[slot]
            toks = []
            for b in range(4):
                ph.wait("act", x_toks[i], stat_free[slot] if b == 0 else None, hbf_free[b])
                t = ph.done("act", ACT.activation(out=hbf[b][:], in_=xt[0][:, b, :], func=AF.Square,
                                                  accum_out=st[:, b:b + 1]))
                ph.wait("act", t)
                t = ph.done("act", ACT.activation(out=st[:, 4 + b:5 + b], in_=st[:, b:b + 1], func=AF.Ln,
                                                  bias=EPS, scale=1.0 / D))
                ph.wait("act", t)
                t_r = ph.done("act", ACT.activation(out=st[:, 8 + b:9 + b], in_=st[:, 4 + b:5 + b], func=AF.Exp,
                                                    scale=-0.5))
                ph.wait("dve", t_r, t_par, hbf_free[b])
                t_h = ph.done("dve", DVE.scalar_tensor_tensor(out=hbf[b][:], in0=xt[0][:, b, :],
                                                              scalar=st[:, 8 + b:9 + b], in1=gmix[:],
                                                              op0=ALU.mult, op1=ALU.mult))
                toks.append(t_h)
            xt_free[0] = toks[-1]
            stat_free[slot] = toks[-1]
            h_toks[i] = toks

        def emit_transposes(i):
            slot = i % 2
            ready = []
            for b in range(4):
                tb = b % 2
                ph.wait("pe", h_toks[i][b], cst["t_identb"], tpst[tb])
                for c in range(8):
                    ins = PE.transpose(out=pb[:, tb, c * 128:(c + 1) * 128], in_=hbf[b][:, c * 128:(c + 1) * 128],
                                       identity=cst["identb"][:])
                t_tp = ph.done("pe", ins)
                hbf_free[b] = t_tp
                ph.wait("act", t_tp, hT_free[0] if b == 0 else None)
                t_e = ph.done("act", ACT.activation(out=hT[slot][:, :, b * 128:(b + 1) * 128],
                                                    in_=pb[:, tb, :].rearrange("p (c t) -> p c t", c=8),
                                                    func=AF.Copy))
                tpst[tb] = t_e
                ready.append(t_e)
            return ready

        emit_xload(0)
        emit_norm(0)
        if cfg.ntile > 1:
            emit_xload(1)
        hT_ready_next = emit_transposes(0)
        for i in range(cfg.ntile):
            slot = i % 2
            seq_start = (i % cfg.tps == 0)
            r0 = i * T
            hT_ready = hT_ready_next
            hTs = hT[slot]

            def fm_group(col0):
                bk = mm_bank()
                ph.wait("pe", hT_ready, t_w, mm_free[bk])
                for k in range(8):
                    ins = PE.matmul(pf[:, bk, :], lhsT=Win[:, k, col0:col0 + 128], rhs=hTs[:, k, :],
                                    start=(k == 0), stop=(k == 7))
                return bk, ph.done("pe", ins)

            def tm_group(b, col0, n):
                bk = mm_bank()
                ph.wait("pe", hT_ready, t_w, mm_free[bk])
                for k in range(8):
                    ins = PE.matmul(pf[:, bk, 0:n], lhsT=hTs[:, k, b * 128:(b + 1) * 128], rhs=Win[:, k, col0:col0 + n],
                                    start=(k == 0), stop=(k == 7))
                return bk, ph.done("pe", ins)

            if seq_start:
                ph.wait("dve", [pre_free[c] for c in range(8)])
                t_z = ph.done("dve", DVE.memset(pre[:, :, 0:3], 0.0))
            else:
                t_z = None
            si_toks = {}
            for c in range(8):
                bk, t_m = fm_group(C_QK + c * 128)
                ph.wait("act", t_m, pre_free[c], t_z)
                t_cp = ph.done("act", ACT.activation(out=pre[:, c, 3:T + 3], in_=pf[:, bk, :], func=AF.Copy))
                mm_free[bk] = t_cp
                ai = state["acc"] % 2
                state["acc"] += 1
                a_ = acc[ai]
                en, EN = ("dve", DVE)
                ph.wait(en, t_cp, t_par, acc_free[ai], t_z)
                ph.wait("act", t_cp, t_par, acc_free[ai], t_z)
                t = ph.done("act", ACT.activation(out=a_[:], in_=pre[:, c, 0:T], func=AF.Copy, scale=cw[:, c * 4:c * 4 + 1]))
                for j in range(1, 4):
                    ph.wait(en, t)
                    t = ph.done(en, EN.scalar_tensor_tensor(out=a_[:], in0=pre[:, c, j:j + T],
                                                            scalar=cw[:, c * 4 + j:c * 4 + j + 1], in1=a_[:],
                                                            op0=ALU.mult, op1=ALU.add))
                t_acc = t
                ph.wait(en, t_acc)
                t_halo = ph.done(en, EN.tensor_copy(out=pre[:, c, 0:3], in_=pre[:, c, T:T + 3]))
                pre_free[c] = t_halo
                ph.wait("act", t_acc, st_tok["qk"] if c == 0 else None, st_tok["km"] if c == 4 else None)
                t_si = ph.done("act", ACT.activation(out=qk_st[:, c, :], in_=a_[:], func=AF.Silu, bias=cb[:, c:c + 1]))
                acc_free[ai] = t_si
                si_toks[c] = t_si
            ph.wait("sp", t_si)
            st_tok["qk"] = ph.dma("sp", scr["qkT"][:, :, r0:r0 + T].rearrange("c p t -> p c t"), qk_st[:], st_sems["qk"])
            ph.wait("pe", small_free, hT_ready, t_w)
            for b in range(4):
                for k in range(8):
                    ins = PE.matmul(pf[:, 5, b * 8:(b + 1) * 8], lhsT=hTs[:, k, b * 128:(b + 1) * 128], rhs=Win[:, k, C_G:C_G + 8],
                                    start=(k == 0), stop=(k == 7))
            t_g = ph.done("pe", ins)
            gi = state["gsb"] % 2
            state["gsb"] += 1
            gs = gsb[gi]
            gs3 = gs[:, 0:32].rearrange("p (b n) -> p b n", b=4)
            ph.wait("dve", t_g, t_par, gsb_free[gi])
            t = ph.done("dve", DVE.tensor_tensor(out=gs3, in0=pf[:, 5, 0:32].rearrange("p (b n) -> p b n", b=4),
                                                 in1=bg4[:].rearrange("p (b n) -> p b n", b=4), op=ALU.add))
            ph.wait("act", t)
            t = ph.done("act", ACT.activation(out=gs[:, 32:48].rearrange("p (b n) -> p b n", b=4), in_=gs3[:, :, 4:8], func=AF.Exp, scale=-1.0))
            ph.wait("act", t)
            t_sp = ph.done("act", ACT.activation(out=gs[:, 48:64], in_=gs[:, 32:48], func=AF.Ln, bias=1.0))
            for b in range(4):
                bk, t_m = tm_group(b, C_VM, 512)
                ph.wait("act", t_m, st_tok["vm"] if b == 0 else None)
                t_vm = ph.done("act", ACT.activation(out=vm_st[:, b, :], in_=pf[:, bk, :], func=AF.Copy))
                mm_free[bk] = t_vm
                bk, t_m = tm_group(b, C_VS, 512)
                ph.wait("act", t_m, st_tok["vs"] if b == 0 else None)
                t_vs = ph.done("act", ACT.activation(out=vs_st[:, b, :], in_=pf[:, bk, :], func=AF.Copy))
                mm_free[bk] = t_vs
            ph.wait("pe", t_sp, t_negU, t_negO)
            for b in range(4):
                PE.matmul(pf[:, 5, 32 + b * 8:36 + b * 8], lhsT=negU[:], rhs=gs[:, 48 + b * 4:52 + b * 4], start=True, stop=True)
                ins = PE.matmul(pf[:, 5, 36 + b * 8:40 + b * 8], lhsT=negO[:], rhs=gs[:, 48 + b * 4:52 + b * 4], start=True, stop=True)
            t_b = ph.done("pe", ins)
            bps3 = pf[:, 5, 32:64].rearrange("p (b n) -> p b n", b=4)
            ph.wait("act", t_b, st_tok["g12"])
            t_e1 = ph.done("act", ACT.activation(out=g12_st[:, :, 0:8], in_=bps3, func=AF.Exp))
            ph.wait("dve", t_b, t_e1)
            t = ph.done("dve", DVE.tensor_tensor(out=gs[:, 32:48].rearrange("p (b n) -> p b n", b=4), in0=gs3[:, :, 0:4],
                                                 in1=bps3[:, :, 0:4], op=ALU.subtract))
            ph.wait("act", t)
            t_e2 = ph.done("act", ACT.activation(out=g12_st[:, :, 8:12], in_=gs[:, 32:48].rearrange("p (b n) -> p b n", b=4),
                                                 func=AF.Exp, bias=LOGS))
            small_free = [t_e1, t]
            gsb_free[gi] = t_e2
            g12_last = t_e2
            ph.wait("sp", g12_last)
            st_tok["g12"] = ph.dma("sp", scr["g12"][r0:r0 + T, :].rearrange("(b p) n -> p b n", p=128), g12_st[:], st_sems["g12"])
            ph.wait("sp", t_vm)
            st_tok["vm"] = ph.dma("sp", scr["vm"][r0:r0 + T, :].rearrange("(b p) n -> p b n", p=128), vm_st[:], st_sems["vm"])
            ph.wait("sp", t_vs)
            st_tok["vs"] = ph.dma("sp", scr["vs"][r0:r0 + T, :].rearrange("(b p) n -> p b n", p=128), vs_st[:], st_sems["vs"])

            for c in range(4, 8):
                h = c - 4
                kb_ = h % 2
                ph.wait("pe", si_toks[c], tpst[kb_])
                for b in range(4):
                    ins = PE.transpose(out=pb[:, kb_, b * 128:(b + 1) * 128], in_=qk_st[:, c, b * 128:(b + 1) * 128],
                                       identity=cst["identb"][:])
                t_t = ph.done("pe", ins)
                ph.wait("act", t_t, st_tok["km"])
                t_k = ph.done("act", ACT.activation(out=km_st[:, :, h * 128:(h + 1) * 128],
                                                    in_=pb[:, kb_, 0:512].rearrange("p (b d) -> p b d", b=4), func=AF.Copy))
                tpst[kb_] = t_k
            ph.wait("sp", t_k)
            st_tok["km"] = ph.dma("sp", scr["km"][r0:r0 + T, :].rearrange("(b p) n -> p b n", p=128), km_st[:], st_sems["km"])

            qjobs = [(col, stg, key, gcol, cc) for (col, stg, key, gcol) in ((C_QS, qs_st, "qs", 0), (C_KS, ks_st, "ks", 1))
                     for cc in range(4)]
            pend = None

            def qk_tail(job):
                (col, stg, key, gcol, cc), bk, si, t_sq = job
                nonlocal_ss = ss_box
                ph.wait("pe", t_sq, t_bones, nonlocal_ss[0])
                t_ss = ph.done("pe", PE.matmul(pf[:, 4, :], lhsT=bones[:], rhs=sq[si][:], start=True, stop=True))
                sq_free[si] = t_ss
                ph.wait("act", t_ss, rstd_free[si])
                t_ln = ph.done("act", ACT.activation(out=rstd[si][:], in_=pf[:, 4, :], func=AF.Ln, bias=EPS))
                nonlocal_ss[0] = t_ln
                ph.wait("act", t_ln)
                t_rs = ph.done("act", ACT.activation(out=rstd[si][:], in_=rstd[si][:], func=AF.Exp, scale=-0.5))
                ph.wait("dve", t_rs, t_gq, st_tok[key] if cc == 0 else None)
                t_o = ph.done("dve", DVE.scalar_tensor_tensor(out=stg[:, cc, :], in0=pf[:, bk, :],
                                                              scalar=gqk[:, gcol:gcol + 1], in1=rstd[si][:],
                                                              op0=ALU.mult, op1=ALU.mult))
                mm_free[bk] = t_o
                rstd_free[si] = t_o
                if cc == 3:
                    ph.wait("sp", t_o)
                    st_tok[key] = ph.dma("sp", scr[key + "T"][:, :, r0:r0 + T].rearrange("c p t -> p c t"), stg[:], st_sems[key])

            for job in qjobs:
                col, stg, key, gcol, cc = job
                bk, t_m = fm_group(col + cc * 128)
                si = state["sq"] % 2
                state["sq"] += 1
                ph.wait("act", t_m, sq_free[si])
                t_sq = ph.done("act", ACT.activation(out=sq[si][:], in_=pf[:, bk, :], func=AF.Square))
                if pend is not None:
                    qk_tail(pend)
                pend = (job, bk, si, t_sq)
            qk_tail(pend)

            if i + 1 < cfg.ntile:
                emit_norm(i + 1)
                if i + 2 < cfg.ntile:
                    emit_xload(i + 2)
            for c in range(16):
                bk, t_m = fm_group(C_GP + c * 128)
                ph.wait("act", t_m, st_tok["g"] if c == 0 else None)
                t_o = ph.done("act", ACT.activation(out=g_st[:, c, :], in_=pf[:, bk, :], func=AF.Sigmoid))
                mm_free[bk] = t_o
            ph.wait("sp", t_o)
            st_tok["g"] = ph.dma("sp", scr["gT"][:, :, r0:r0 + T].rearrange("c p t -> p c t"), g_st[:], st_sems["g"])
            for b in range(4):
                bk, t_m = tm_group(b, C_OM, 512)
                ph.wait("act", t_m, st_tok["om"] if b == 0 else None)
                t_o = ph.done("act", ACT.activation(out=om_st[:, b, :], in_=pf[:, bk, :], func=AF.Sigmoid))
                mm_free[bk] = t_o
            ph.wait("sp", t_o)
            st_tok["om"] = ph.dma("sp", scr["om"][r0:r0 + T, :].rearrange("(b p) n -> p b n", p=128), om_st[:], st_sems["om"])

            hT_free[0] = t_m
            if i + 1 < cfg.ntile:
                hT_ready_next = emit_transposes(i + 1)
        ph.final = [v for v in st_tok.values() if v is not None]


def scratch_specs(cfg):
    n = cfg.ntok
    return {
        "qkT": ([8, 128, n], BF16), "km": ([n, 512], BF16), "vm": ([n, 512], BF16), "om": ([n, 512], BF16),
        "g12": ([n, 12], F32), "qsT": ([4, 128, n], BF16), "ksT": ([4, 128, n], BF16), "vs": ([n, 512], BF16),
        "gT": ([16, 128, n], BF16), "yaT": ([4, 128, n], BF16), "ybT": ([4, 128, n], BF16),
        "x1": ([n, D], F32), "xmid": ([n, D], F32),
    }


WEIGHT_SPECS = {
    "g_mix": [DEPTH, D], "w_in": [DEPTH, D, IN_COLS], "cwl": [DEPTH, 128, 32], "cbl": [DEPTH, 128, 8],
    "b_gates": [DEPTH, 8], "gqk": [DEPTH, 128, 2], "w_br_a": [DEPTH, 512, D], "w_br_b": [DEPTH, 512, D],
    "w_out": [DEPTH, D, D], "g_ffn": [DEPTH, D], "w_gu": [DEPTH, D, 2 * D_FF], "w_down": [DEPTH, D_FF, D],
}


def build_program(cfg, plan, ext_in=(), ext_out=()):
    nc = bass.Bass("TRN2", target_bir_lowering=False)
    W = {k: nc.dram_tensor(k, s, F32, kind="ExternalInput").ap() for k, s in WEIGHT_SPECS.items()}
    tens = {}
    tens["x"] = nc.dram_tensor("x", [cfg.ntok, D], F32, kind="ExternalInput").ap()
    tens["out"] = nc.dram_tensor("out", [cfg.ntok, D], F32, kind="ExternalOutput").ap()
    for k, (s, dt) in scratch_specs(cfg).items():
        kind = "ExternalInput" if k in ext_in else ("ExternalOutput" if k in ext_out else "Internal")
        tens[k] = nc.dram_tensor(k, s, dt, kind=kind).ap()
    for (pname, L, xin, xout) in plan:
        PHASES[pname](nc, cfg, L, tens.get(xin), W, tens, tens.get(xout))
    return nc


def host_layout_weights(inp):
    f = lambda a: np.ascontiguousarray(np.asarray(a, dtype=np.float32))
    cw = f(inp["conv_w"])
    cwl = f(cw.reshape(DEPTH, 4, 8, 128).transpose(0, 3, 2, 1).reshape(DEPTH, 128, 32))
    cbl = f(f(inp["conv_b"]).reshape(DEPTH, 8, 128).transpose(0, 2, 1))
    gq = f(inp["g_q"])
    gk = f(inp["g_k"])
    gqk = f(np.stack([np.concatenate([gq, gq], 1), np.concatenate([gk, gk], 1)], axis=2))
    return {
        "g_mix": f(inp["g_mix"]), "w_in": f(inp["w_in"]), "cwl": cwl, "cbl": cbl, "b_gates": f(inp["b_gates"]),
        "gqk": gqk, "w_br_a": f(inp["w_br_a"]), "w_br_b": f(inp["w_br_b"]), "w_out": f(inp["w_out"]),
        "g_ffn": f(inp["g_ffn"]), "w_gu": f(inp["w_gu"]), "w_down": f(inp["w_down"]),
    }


PHASES = {"p1": lambda nc, cfg, L, xin, W, tens, xout: phase1(nc, cfg, L, xin, W, tens)}


def phase2(nc, cfg, L, scr):
    with Phase(nc, f"p2l{L}") as ph:
        PE, ACT, DVE, POOL, SP = (ph.engs[k] for k in ("pe", "act", "dve", "pool", "sp"))
        NS = cfg.nseq
        qk_sb = [[ph.sb(f"qk{s}{p}", [128, 8, T], BF16) for p in range(2)] for s in range(NS)]
        km_sb = [[ph.sb(f"km{s}{p}", [128, 4, 512], BF16) for p in range(2)] for s in range(NS)]
        va_sb = [[ph.sb(f"va{s}{p}", [128, 4, 4, 129], BF16) for p in range(2)] for s in range(NS)]
        om_sb = [[ph.sb(f"om{s}{p}", [128, 4, 512], BF16) for p in range(2)] for s in range(NS)]
        g_sb = [[ph.sb(f"g{s}{p}", [128, 4, 12], F32) for p in range(2)] for s in range(NS)]
        C32 = [ph.sb(f"C32_{s}", [128, 4, 129], F32) for s in range(NS)]
        Cbf = [ph.sb(f"Cbf_{s}", [128, 4, 129], BF16) for s in range(NS)]
        ya_sb = [ph.sb(f"ya{i}", [128, 512], BF16) for i in range(2)]
        yaT_st = [ph.sb(f"yaT{s}", [128, 4, T], BF16) for s in range(NS)]
        NR = 4
        sT_sb = [ph.sb(f"sT{i}", [128, 128], BF16) for i in range(NR)]
        kw_sb = [ph.sb(f"kw{i}", [128, 128], BF16) for i in range(NR)]
        wk2 = [ph.sb(f"wk2_{i}", [128, 4], F32) for i in range(NR)]
        dtmp = [ph.sb(f"dtmp{i}", [128, 4], F32) for i in range(NR)]
        pf = ph.ps("pf", [128, 6, 512], F32)
        pb = ph.ps("pb", [128, 2, 1024], BF16)
        cst = make_consts(ph)
        maskLE, t_mask = tri_f32(ph, "maskLE", 1.0, "le")
        t_init = []
        for s in range(NS):
            t_init.append(ph.done("pool", POOL.memset(C32[s][:], 0.0)))
            t_init.append(ph.done("pool", POOL.memset(Cbf[s][:], 0.0)))
            for p in range(2):
                t_init.append(ph.done("pool", POOL.memset(va_sb[s][p][:, :, :, 128:129], 1.0)))
        t_init = t_init[-1]

        s_ld = [[ph.sem(f"ld{s}{p}") for p in range(2)] for s in range(NS)]
        s_st = [ph.sem(f"st{s}") for s in range(NS)]
        st_tok = [None] * NS
        slot_free = [[[] for p in range(2)] for s in range(NS)]
        C_tok = [[t_init] * 4 for s in range(NS)]
        Cbf_tok = [[t_init] * 4 for s in range(NS)]
        Cbf_read = [[None] * 4 for s in range(NS)]
        st_free = [None, None]
        acc_free = [None, None]
        cps_free = [None, None]
        sT_free = [None] * NR
        kw_free = [None] * NR
        wk2_free = [None] * NR
        dtmp_free = [None] * NR
        ya_free = [None, None]
        pb_free = [None, None]
        cnt = {"u": 0, "c": 0}

        ld_toks = {}

        def emit_loads(t):
            par = t % 2
            for s in range(NS):
                r0 = s * cfg.S + t * T
                ph.wait("sp", slot_free[s][par])
                sem = s_ld[s][par]
                ph.dma("sp", qk_sb[s][par][:], scr["qkT"][:, :, r0:r0 + T].rearrange("c p t -> p c t"), sem)
                ph.dma("sp", km_sb[s][par][:], scr["km"][r0:r0 + T, :].rearrange("(b p) n -> p b n", p=128), sem)
                for b in range(4):
                    ph.dma("sp", va_sb[s][par][:, b, :, 0:128],
                           scr["vm"][r0 + b * 128:r0 + (b + 1) * 128, :].rearrange("p (h e) -> p h e", h=4), sem)
                ph.dma("sp", om_sb[s][par][:], scr["om"][r0:r0 + T, :].rearrange("(b p) n -> p b n", p=128), sem)
                ld_toks[(t, s)] = ph.dma("sp", g_sb[s][par][:], scr["g12"][r0:r0 + T, :].rearrange("(b p) n -> p b n", p=128), sem)

        emit_loads(0)
        for t in range(cfg.tps):
            par = t % 2
            if t + 1 < cfg.tps:
                emit_loads(t + 1)
            ld_tok = [ld_toks[(t, s)] for s in range(NS)]
            last = [None] * NS
            for j in range(4):
                jr = slice(j * 128, (j + 1) * 128)
                for s in range(NS):
                    qk, km, va, om, g = qk_sb[s][par], km_sb[s][par], va_sb[s][par], om_sb[s][par], g_sb[s][par]
                    ci = cnt["c"] % NR
                    cnt["c"] += 1
                    ph.wait("dve", ld_tok[s], wk2_free[ci])
                    t_wk2 = ph.done("dve", DVE.tensor_tensor(out=wk2[ci][:], in0=g[:, j, 8:12], in1=g[:, j, 4:8], op=ALU.mult))
                    yi = cnt["c"] % 2
                    y_toks = []
                    for h in range(4):
                        u = cnt["u"]
                        cnt["u"] += 1
                        r = u % NR
                        b2 = u % 2
                        ph.wait("pe", ld_tok[s], st_free[b2])
                        t_st = ph.done("pe", PE.matmul(pf[:, b2, 0:128], lhsT=qk[:, 4 + h, jr], rhs=qk[:, h, jr], start=True, stop=True))
                        ph.wait("dve", t_st, t_mask, sT_free[r])
                        t_sT = ph.done("dve", DVE.scalar_tensor_tensor(out=sT_sb[r][:], in0=pf[:, b2, 0:128], scalar=g[:, j, 8 + h:9 + h],
                                                                       in1=maskLE[:], op0=ALU.mult, op1=ALU.mult))
                        st_free[b2] = t_sT
                        ph.wait("pool", ld_tok[s], t_wk2, kw_free[r])
                        t_kw = ph.done("pool", POOL.tensor_scalar_mul(out=kw_sb[r][:], in0=km[:, j, h * 128:(h + 1) * 128],
                                                                      scalar1=wk2[ci][:, h:h + 1]))
                        ph.wait("pe", Cbf_tok[s][h], t_sT, acc_free[b2])
                        PE.matmul(pf[:, 2 + b2, 0:129], lhsT=qk[:, h, jr], rhs=Cbf[s][:, h, :], start=True, stop=False)
                        t_acc = ph.done("pe", PE.matmul(pf[:, 2 + b2, 0:129], lhsT=sT_sb[r][:], rhs=va[:, j, h, :], start=False, stop=True))
                        sT_free[r] = t_acc
                        ph.wait("pe", t_kw, cps_free[b2])
                        t_cps = ph.done("pe", PE.matmul(pf[:, 4 + b2, 0:129], lhsT=kw_sb[r][:], rhs=va[:, j, h, :], start=True, stop=True))
                        kw_free[r] = t_cps
                        ph.wait("dve", t_cps, C_tok[s][h], Cbf_tok[s][h])
                        t_c = ph.done("dve", DVE.scalar_tensor_tensor(out=C32[s][:, h, :], in0=C32[s][:, h, :], scalar=g[:, j, 4 + h:5 + h],
                                                                      in1=pf[:, 4 + b2, 0:129], op0=ALU.mult, op1=ALU.add))
                        C_tok[s][h] = t_c
                        cps_free[b2] = t_c
                        ph.wait("act", t_c, t_acc)
                        Cbf_tok[s][h] = ph.done("act", ACT.activation(out=Cbf[s][:, h, :], in_=C32[s][:, h, :], func=AF.Copy))
                        d = dtmp[r]
                        ph.wait("dve", t_acc, dtmp_free[r])
                        t1 = ph.done("dve", DVE.tensor_tensor(out=d[:, 0:1], in0=pf[:, 2 + b2, 128:129], in1=g[:, j, h:h + 1], op=ALU.mult))
                        ph.wait("dve", t1)
                        t1 = ph.done("dve", DVE.scalar_tensor_tensor(out=d[:, 1:2], in0=d[:, 0:1], scalar=-1.0, in1=d[:, 0:1],
                                                                     op0=ALU.mult, op1=ALU.max))
                        ph.wait("dve", t1)
                        t1 = ph.done("dve", DVE.tensor_scalar_max(out=d[:, 1:2], in0=d[:, 1:2], scalar1=1.0))
                        ph.wait("dve", t1)
                        t1 = ph.done("dve", DVE.reciprocal(out=d[:, 2:3], in_=d[:, 1:2]))
                        ph.wait("dve", t1)
                        t1 = ph.done("dve", DVE.tensor_tensor(out=d[:, 3:4], in0=d[:, 2:3], in1=g[:, j, h:h + 1], op=ALU.mult))
                        ph.wait("dve", t1, ya_free[yi] if h == 0 else None)
                        t_y = ph.done("dve", DVE.scalar_tensor_tensor(out=ya_sb[yi][:, h * 128:(h + 1) * 128], in0=pf[:, 2 + b2, 0:128],
                                                                      scalar=d[:, 3:4], in1=om[:, j, h * 128:(h + 1) * 128],
                                                                      op0=ALU.mult, op1=ALU.mult))
                        acc_free[b2] = t_y
                        dtmp_free[r] = t_y
                        y_toks.append(t_y)
                        last[s] = [t_y, t_cps, t_acc, t_kw]
                    wk2_free[ci] = t_kw
                    tb = cnt["c"] % 2
                    ph.wait("pe", y_toks, cst["t_identb"], pb_free[tb])
                    for h in range(4):
                        ins = PE.transpose(out=pb[:, tb, h * 128:(h + 1) * 128], in_=ya_sb[yi][:, h * 128:(h + 1) * 128],
                                           identity=cst["identb"][:])
                    t_tp = ph.done("pe", ins)
                    ya_free[yi] = t_tp
                    ph.wait("act", t_tp, st_tok[s] if j == 0 else None)
                    t_ev = ph.done("act", ACT.activation(out=yaT_st[s][:, :, jr], in_=pb[:, tb, 0:512].rearrange("p (h l) -> p h l", h=4),
                                                         func=AF.Copy))
                    pb_free[tb] = t_ev
                    last[s].append(t_ev)
            for s in range(NS):
                r0 = s * cfg.S + t * T
                ph.wait("act", last[s][-1])
                st_tok[s] = ph.dma("act", scr["yaT"][:, :, r0:r0 + T].rearrange("c p t -> p c t"), yaT_st[s][:], s_st[s])
                slot_free[s][par] = list(last[s])
        ph.final = [x for x in st_tok if x is not None]


PHASES["p2"] = lambda nc, cfg, L, xin, W, tens, xout: phase2(nc, cfg, L, tens)


def phase3(nc, cfg, L, scr):
    with Phase(nc, f"p3l{L}") as ph:
        PE, ACT, DVE, POOL, SP = (ph.engs[k] for k in ("pe", "act", "dve", "pool", "sp"))
        S = cfg.S
        NB = S // 128
        NQT = S // T
        kT = ph.sb("kT", [128, 4, S], BF16)
        qT = ph.sb("qT", [128, 4, S], BF16)
        vv = ph.sb("vv", [128, NB, 512], BF16)
        e_sb = [ph.sb(f"e{i}", [128, 2, T], F32) for i in range(2)]
        sp_sb = [ph.sb(f"sp{i}", [128, 2, T], BF16) for i in range(2)]
        Ss = [ph.sb(f"Ss{i}", [128, T], BF16) for i in range(3)]
        Stmp = ph.sb("Stmp", [128, T], BF16)
        aT_sb = [ph.sb(f"aT{i}", [128, 2, T], BF16) for i in range(2)]
        yb_st = [ph.sb(f"yb{i}", [64, T], BF16) for i in range(2)]
        pf = ph.ps("pf", [128, 8, 512], F32)
        cst = make_consts(ph)
        identb = cst["identb"]
        negTri, t_tri = tri_bf16(ph, "negTri", -1.0, "ge")
        negOne, t_one = tri_bf16(ph, "negOne", -1.0, "all")
        nm_f = ph.sb("nm_f", [128, T], F32)
        negmask = []
        t_nm = None
        for i in range(4):
            m = ph.sb(f"negmask{i}", [128, T], BF16)
            ph.wait("pool", t_nm)
            t = ph.done("pool", POOL.memset(nm_f[:], NEG))
            ph.wait("pool", t)
            t = ph.done("pool", POOL.affine_select(out=nm_f[:], in_=nm_f[:], pattern=[[-1, T]], compare_op=ALU.is_ge,
                                                   fill=0.0, base=i * 128, channel_multiplier=1))
            ph.wait("pool", t)
            t_nm = ph.done("pool", POOL.tensor_copy(out=m[:], in_=nm_f[:]))
            negmask.append(m)
        t_consts = [cst["t_identb"], t_tri, t_one, t_nm]

        s_ld = ph.sem("ld")
        s_yb = [ph.sem("yb0"), ph.sem("yb1")]
        zfree = [None, None]
        spfree = [None, None]
        Ssfree = [None, None, None]
        Stmp_free = [None]
        Afree = [None]
        aTfree = [None, None]
        ofree = [None, None]
        ybfree = [None, None]
        prev_done = []
        gk = {"k": 0, "grp": 0}

        for s in range(cfg.nseq):
            c0 = s * S
            ph.wait("sp", prev_done)
            ph.dma("sp", kT[:], scr["ksT"][:, :, c0:c0 + S].rearrange("c p t -> p c t"), s_ld)
            ph.dma("sp", qT[:], scr["qsT"][:, :, c0:c0 + S].rearrange("c p t -> p c t"), s_ld)
            ld_tok = ph.dma("sp", vv[:], scr["vs"][c0:c0 + S, :].rearrange("(b p) n -> p b n", p=128), s_ld)
            units = []
            for h in range(SB_H):
                for qt in range(NQT):
                    npair = 2 * (qt + 1)
                    for m in range(npair):
                        kb = 4 * qt + 3 - 2 * m
                        units.append(dict(h=h, qt=qt, m=m, kb=kb, last=(m == npair - 1), diag=(m < 2),
                                          i=kb - 4 * qt, grp=gk["grp"]))
                    gk["grp"] += 1
            NU = len(units)
            k0 = gk["k"]

            def operands(U, j):
                hc = U["h"] // 2
                p0 = (U["h"] % 2) * 64
                kb = U["kb"] - j
                lk = kT[p0:p0 + 64, hc, kb * 128:(kb + 1) * 128]
                rq = qT[p0:p0 + 64, hc, U["qt"] * T:(U["qt"] + 1) * T]
                return lk, rq

            def stage0(U, k):
                b = k % 2
                ph.wait("pe", ld_tok, t_consts, zfree[b])
                for j in range(2):
                    lk, rq = operands(U, j)
                    ins = PE.matmul(pf[:, 2 * b + j, :], lhsT=lk, rhs=rq, start=True, stop=not U["diag"])
                    if U["diag"]:
                        ins = PE.matmul(pf[:, 2 * b + j, :], lhsT=identb[:], rhs=negmask[U["i"] - j][:], start=False, stop=True)
                U["t_z"] = ph.done("pe", ins)
                ph.wait("act", U["t_z"])
                U["t_e"] = ph.done("act", ACT.activation(out=e_sb[b][:], in_=pf[:, 2 * b:2 * b + 2, :], func=AF.Exp))
                zfree[b] = U["t_e"]
                ph.wait("act", U["t_e"], spfree[b])
                U["t_sp"] = ph.done("act", ACT.activation(out=sp_sb[b][:], in_=e_sb[b][:], func=AF.Ln, bias=1.0))
                U["t_ss"] = None
                if not U["last"]:
                    m = U["m"]
                    dst = Ss[(m + 1) % 3]
                    ph.wait("dve", U["t_sp"], Ssfree[(m + 1) % 3])
                    if m == 0:
                        U["t_ss"] = ph.done("dve", DVE.tensor_tensor(out=dst[:], in0=sp_sb[b][:, 0, :], in1=sp_sb[b][:, 1, :], op=ALU.add))
                    else:
                        ph.wait("dve", U["t_ssprev"], Stmp_free[0])
                        t = ph.done("dve", DVE.tensor_tensor(out=Stmp[:], in0=Ss[m % 3][:], in1=sp_sb[b][:, 0, :], op=ALU.add))
                        ph.wait("dve", t)
                        U["t_ss"] = ph.done("dve", DVE.tensor_tensor(out=dst[:], in0=Stmp[:], in1=sp_sb[b][:, 1, :], op=ALU.add))
                        Stmp_free[0] = U["t_ss"]

            def stage1(U, k):
                b = k % 2
                m = U["m"]
                ph.wait("pe", U["t_sp"], Afree[0], U.get("t_ssprev"))
                for j in range(2):
                    lk, rq = operands(U, j)
                    mms = [(lk, rq), (negTri[:], sp_sb[b][:, j, :])]
                    if j == 1:
                        mms.append((negOne[:], sp_sb[b][:, 0, :]))
                    if m > 0:
                        mms.append((negOne[:], Ss[m % 3][:]))
                    if U["diag"]:
                        mms.append((identb[:], negmask[U["i"] - j][:]))
                    for jj, (l_, r_) in enumerate(mms):
                        ins = PE.matmul(pf[:, 4 + j, :], lhsT=l_, rhs=r_, start=(jj == 0), stop=(jj == len(mms) - 1))
                U["t_A"] = ph.done("pe", ins)
                if m > 0:
                    Ssfree[m % 3] = U["t_A"]
                spfree[b] = [U["t_A"], U["t_ss"]]
                ph.wait("act", U["t_A"], aTfree[b])
                U["t_a"] = ph.done("act", ACT.activation(out=aT_sb[b][:], in_=pf[:, 4:6, :], func=AF.Exp))
                Afree[0] = U["t_a"]

            def stage2(U, k):
                b = k % 2
                g2 = U["grp"] % 2
                h = U["h"]
                ph.wait("pe", U["t_a"], ofree[g2] if U["m"] == 0 else None)
                for j in range(2):
                    ins = PE.matmul(pf[0:64, 6 + g2, :], lhsT=vv[:, U["kb"] - j, h * 64:(h + 1) * 64], rhs=aT_sb[b][:, j, :],
                                    start=(U["m"] == 0 and j == 0), stop=(U["last"] and j == 1))
                U["t_av"] = ph.done("pe", ins)
                aTfree[b] = U["t_av"]
                if U["last"]:
                    ph.wait("dve", U["t_av"], ybfree[g2])
                    t_ev = ph.done("dve", DVE.tensor_copy(out=yb_st[g2][:], in_=pf[0:64, 6 + g2, :]))
                    ofree[g2] = t_ev
                    r0 = c0 + U["qt"] * T
                    p0 = (h % 2) * 64
                    ph.wait("sp", t_ev)
                    ybfree[g2] = ph.dma("sp", scr["ybT"][h // 2, p0:p0 + 64, r0:r0 + T], yb_st[g2][:], s_yb[g2])
                    U["t_ev"] = t_ev

            for step in range(NU + 2):
                if step < NU:
                    U = units[step]
                    if U["m"] > 0:
                        U["t_ssprev"] = units[step - 1]["t_ss"]
                    stage0(U, k0 + step)
                if 0 <= step - 1 < NU:
                    stage1(units[step - 1], k0 + step - 1)
                if 0 <= step - 2 < NU:
                    stage2(units[step - 2], k0 + step - 2)
            gk["k"] = k0 + NU
            lastU = units[-1]
            prev_done = [lastU["t_av"], lastU["t_a"], lastU["t_ev"]]
        ph.final = [x for x in ybfree if x is not None]


PHASES["p3"] = lambda nc, cfg, L, xin, W, tens, xout: phase3(nc, cfg, L, tens)


def phase4(nc, cfg, L, x_in, W, scr):
    with Phase(nc, f"p4l{L}") as ph:
        PE, ACT, DVE, POOL, SP = (ph.engs[k] for k in ("pe", "act", "dve", "pool", "sp"))
        Wa = ph.sb("wa", [128, 4, D], BF16)
        Wb = ph.sb("wb", [128, 4, D], BF16)
        Wo = ph.sb("wo", [128, 8, D], BF16)
        ya = [ph.sb(f"ya{i}", [128, 4, T], BF16) for i in range(2)]
        yb = [ph.sb(f"yb{i}", [128, 4, T], BF16) for i in range(2)]
        gT = [ph.sb(f"gT{i}", [128, 16, T], BF16) for i in range(2)]
        xs = [ph.sb(f"xs{i}", [128, 4, D], F32) for i in range(2)]
        mixT = ph.sb("mixT", [128, 8, T], BF16)
        tmpa = [ph.sb(f"tmpa{i}", [128, T], F32) for i in range(2)]
        tmpb = [ph.sb(f"tmpb{i}", [128, T], F32) for i in range(2)]
        pf = ph.ps("pf", [128, 8, 512], F32)
        s_w = ph.sem("w")
        ph.dma("pool", Wa[:], W["w_br_a"][L].rearrange("(k p) n -> p k n", p=128), s_w)
        ph.dma("pool", Wb[:], W["w_br_b"][L].rearrange("(k p) n -> p k n", p=128), s_w)
        wo_v = W["w_out"][L].rearrange("(k p) n -> p k n", p=128)
        ph.dma("pool", Wo[:, 0:4, :], wo_v[:, 0:4, :], s_w)
        t_w = ph.dma("pool", Wo[:, 4:8, :], wo_v[:, 4:8, :], s_w)
        s_ld = [ph.sem("ld0"), ph.sem("ld1")]
        s_st = [ph.sem("st0"), ph.sem("st1")]
        slot_free = [[], []]
        st_tok = [None, None]
        ld_toks = {}

        def emit_loads(i):
            p = i % 2
            r0 = i * T
            ph.wait("sp", slot_free[p], st_tok[p])
            ph.dma("sp", ya[p][:], scr["yaT"][:, :, r0:r0 + T].rearrange("c p t -> p c t"), s_ld[p])
            ph.dma("sp", yb[p][:], scr["ybT"][:, :, r0:r0 + T].rearrange("c p t -> p c t"), s_ld[p])
            ph.dma("sp", gT[p][:, 0:8, :], scr["gT"][0:8, :, r0:r0 + T].rearrange("c p t -> p c t"), s_ld[p])
            ph.dma("sp", gT[p][:, 8:16, :], scr["gT"][8:16, :, r0:r0 + T].rearrange("c p t -> p c t"), s_ld[p])
            ld_toks[i] = ph.dma("sp", xs[p][:], x_in[r0:r0 + T, :].rearrange("(b p) d -> p b d", p=128), s_ld[p])

        bank_free = [None] * 8
        tmpa_free = [None, None]
        tmpb_free = [None, None]
        mix_free = None
        emit_loads(0)
        cnt = 0
        for i in range(cfg.ntile):
            p = i % 2
            if i + 1 < cfg.ntile:
                emit_loads(i + 1)
            ld = ld_toks[i]
            mix_toks = []
            for c in range(8):
                ba, bb = (2 * c) % 4, (2 * c + 1) % 4
                ph.wait("pe", ld, t_w, bank_free[ba])
                for k in range(4):
                    ins = PE.matmul(pf[:, ba, :], lhsT=Wa[:, k, c * 128:(c + 1) * 128], rhs=ya[p][:, k, :], start=(k == 0), stop=(k == 3))
                t_pa = ph.done("pe", ins)
                ph.wait("pe", bank_free[bb])
                for k in range(4):
                    ins = PE.matmul(pf[:, bb, :], lhsT=Wb[:, k, c * 128:(c + 1) * 128], rhs=yb[p][:, k, :], start=(k == 0), stop=(k == 3))
                t_pb = ph.done("pe", ins)
                ti = c % 2
                ph.wait("dve", t_pa, ld, tmpa_free[ti])
                t_a = ph.done("dve", DVE.tensor_tensor(out=tmpa[ti][:], in0=pf[:, ba, :], in1=gT[p][:, c, :], op=ALU.mult))
                bank_free[ba] = t_a
                ph.wait("dve", t_pb, tmpb_free[ti])
                t_b = ph.done("dve", DVE.tensor_tensor(out=tmpb[ti][:], in0=pf[:, bb, :], in1=gT[p][:, 8 + c, :], op=ALU.mult))
                bank_free[bb] = t_b
                ph.wait("pool", t_a, t_b, mix_free if c == 0 else None)
                t_m = ph.done("pool", POOL.tensor_tensor(out=mixT[:, c, :], in0=tmpa[ti][:], in1=tmpb[ti][:], op=ALU.add))
                tmpa_free[ti] = t_m
                tmpb_free[ti] = t_m
                mix_toks.append(t_m)
            for b in range(4):
                for hf in range(2):
                    bk = 4 + (cnt % 4)
                    cnt += 1
                    ph.wait("pe", mix_toks, bank_free[bk])
                    for k in range(8):
                        ins = PE.matmul(pf[:, bk, :], lhsT=mixT[:, k, b * 128:(b + 1) * 128], rhs=Wo[:, k, hf * 512:(hf + 1) * 512],
                                        start=(k == 0), stop=(k == 7))
                    t_po = ph.done("pe", ins)
                    ph.wait("dve", t_po, ld)
                    t_x = ph.done("dve", DVE.tensor_tensor(out=xs[p][:, b, hf * 512:(hf + 1) * 512], in0=pf[:, bk, :],
                                                           in1=xs[p][:, b, hf * 512:(hf + 1) * 512], op=ALU.add))
                    bank_free[bk] = t_x
            mix_free = t_po
            r0 = i * T
            ph.wait("act", t_x)
            st_tok[p] = ph.dma("act", scr["x1"][r0:r0 + T, :].rearrange("(b p) d -> p b d", p=128), xs[p][:], s_st[p])
            slot_free[p] = [t_po, t_x, t_m]
        ph.final = [x for x in st_tok if x is not None]


def phase5(nc, cfg, L, W, scr, x_out):
    with Phase(nc, f"p5l{L}") as ph:
        PE, ACT, DVE, POOL, SP = (ph.engs[k] for k in ("pe", "act", "dve", "pool", "sp"))
        NC_FF = D_FF // 128
        Wgu = ph.sb("wgu", [128, 8, 2 * D_FF], BF16)
        Wd = ph.sb("wd", [128, NC_FF, D], BF16)
        gf = ph.sb("gf", [128, D], F32)
        xs = [ph.sb(f"xs{i}", [128, 4, D], F32) for i in range(2)]
        stat = [ph.sb(f"stat{i}", [128, 16], F32) for i in range(2)]
        hbf = [ph.sb(f"hbf{i}", [128, D], BF16) for i in range(4)]
        hT = ph.sb("hT", [128, 8, T], BF16)
        act = ph.sb("act", [128, NC_FF, T], BF16)
        pf = ph.ps("pf", [128, 6, 512], F32)
        pb = ph.ps("pb", [128, 2, 1024], BF16)
        cst = make_consts(ph)
        s_w = ph.sem("w")
        s_p = ph.sem("par")
        t_par = ph.dma("sp", gf[:], W["g_ffn"][L:L + 1, :].partition_broadcast(128), s_p)
        wg_v = W["w_gu"][L].rearrange("(k p) n -> p k n", p=128)
        for k in range(8):
            ph.dma("pool", Wgu[:, k, :], wg_v[:, k, :], s_w)
        wd_v = W["w_down"][L].rearrange("(k p) n -> p k n", p=128)
        for k0 in range(0, NC_FF, 6):
            k1 = min(NC_FF, k0 + 6)
            t_w = ph.dma("pool", Wd[:, k0:k1, :], wd_v[:, k0:k1, :], s_w)
        s_ld = [ph.sem("ld0"), ph.sem("ld1")]
        s_st = [ph.sem("st0"), ph.sem("st1")]
        slot_free = [[], []]
        st_tok = [None, None]
        ld_toks = {}

        def emit_loads(i):
            p = i % 2
            r0 = i * T
            ph.wait("sp", slot_free[p], st_tok[p])
            ld_toks[i] = ph.dma("sp", xs[p][:], scr["x1"][r0:r0 + T, :].rearrange("(b p) d -> p b d", p=128), s_ld[p])

        hbf_free = [None] * 4
        stat_free = [None, None]
        hT_free = None
        tp_free = [None, None]
        h_toks = {}

        def emit_norm(i):
            p = i % 2
            st = stat[p]
            toks = []
            for b in range(4):
                ph.wait("act", ld_toks[i], stat_free[p] if b == 0 else None, hbf_free[b])
                t = ph.done("act", ACT.activation(out=hbf[b][:], in_=xs[p][:, b, :], func=AF.Square, accum_out=st[:, b:b + 1]))
                ph.wait("act", t)
                t = ph.done("act", ACT.activation(out=st[:, 4 + b:5 + b], in_=st[:, b:b + 1], func=AF.Ln, bias=EPS, scale=1.0 / D))
                ph.wait("act", t)
                t_r = ph.done("act", ACT.activation(out=st[:, 8 + b:9 + b], in_=st[:, 4 + b:5 + b], func=AF.Exp, scale=-0.5))
                ph.wait("dve", t_r, t_par, hbf_free[b])
                t_h = ph.done("dve", DVE.scalar_tensor_tensor(out=hbf[b][:], in0=xs[p][:, b, :], scalar=st[:, 8 + b:9 + b], in1=gf[:],
                                                              op0=ALU.mult, op1=ALU.mult))
                toks.append(t_h)
            stat_free[p] = toks[-1]
            h_toks[i] = toks

        def emit_transposes(i):
            ready = []
            for b in range(4):
                tb = b % 2
                ph.wait("pe", h_toks[i][b], cst["t_identb"], tp_free[tb])
                for c in range(8):
                    ins = PE.transpose(out=pb[:, tb, c * 128:(c + 1) * 128], in_=hbf[b][:, c * 128:(c + 1) * 128], identity=cst["identb"][:])
                t_tp = ph.done("pe", ins)
                hbf_free[b] = t_tp
                ph.wait("act", t_tp, hT_free if b == 0 else None)
                t_e = ph.done("act", ACT.activation(out=hT[:, :, b * 128:(b + 1) * 128], in_=pb[:, tb, :].rearrange("p (c t) -> p c t", c=8),
                                                    func=AF.Copy))
                tp_free[tb] = t_e
                ready.append(t_e)
            return ready

        bank_free = [None] * 6
        sg_free = [None, None]
        act_free = None
        emit_loads(0)
        emit_norm(0)
        hT_ready = emit_transposes(0)
        cnt = 0
        for i in range(cfg.ntile):
            p = i % 2
            if i + 1 < cfg.ntile:
                emit_loads(i + 1)
            a_toks = []
            for c in range(NC_FF):
                bg_, bu_ = (2 * c) % 4, (2 * c + 1) % 4
                ph.wait("pe", hT_ready, t_w, bank_free[bg_])
                for k in range(8):
                    ins = PE.matmul(pf[:, bg_, :], lhsT=Wgu[:, k, c * 128:(c + 1) * 128], rhs=hT[:, k, :], start=(k == 0), stop=(k == 7))
                t_g = ph.done("pe", ins)
                ph.wait("pe", bank_free[bu_])
                for k in range(8):
                    ins = PE.matmul(pf[:, bu_, :], lhsT=Wgu[:, k, D_FF + c * 128:D_FF + (c + 1) * 128], rhs=hT[:, k, :],
                                    start=(k == 0), stop=(k == 7))
                t_u = ph.done("pe", ins)
                ph.wait("act", t_g, act_free if c == 0 else None)
                t_s = ph.done("act", ACT.activation(out=act[:, c, :], in_=pf[:, bg_, :], func=AF.Silu))
                bank_free[bg_] = t_s
                ph.wait("dve", t_s, t_u)
                t_a = ph.done("dve", DVE.tensor_tensor(out=act[:, c, :], in0=pf[:, bu_, :], in1=act[:, c, :], op=ALU.mult))
                bank_free[bu_] = t_a
                a_toks.append(t_a)
            hT_free = t_u
            if i + 1 < cfg.ntile:
                emit_norm(i + 1)
            for b in range(4):
                for hf in range(2):
                    bk = 4 + (cnt % 2)
                    cnt += 1
                    ph.wait("pe", a_toks, bank_free[bk])
                    for k in range(NC_FF):
                        ins = PE.matmul(pf[:, bk, :], lhsT=act[:, k, b * 128:(b + 1) * 128], rhs=Wd[:, k, hf * 512:(hf + 1) * 512],
                                        start=(k == 0), stop=(k == NC_FF - 1))
                    t_pd = ph.done("pe", ins)
                    ph.wait("dve", t_pd)
                    t_x = ph.done("dve", DVE.tensor_tensor(out=xs[p][:, b, hf * 512:(hf + 1) * 512], in0=pf[:, bk, :],
                                                           in1=xs[p][:, b, hf * 512:(hf + 1) * 512], op=ALU.add))
                    bank_free[bk] = t_x
            act_free = t_pd
            r0 = i * T
            ph.wait("act", t_x)
            st_tok[p] = ph.dma("act", x_out[r0:r0 + T, :].rearrange("(b p) d -> p b d", p=128), xs[p][:], s_st[p])
            slot_free[p] = [t_x]
            if i + 1 < cfg.ntile:
                hT_ready = emit_transposes(i + 1)
        ph.final = [x for x in st_tok if x is not None]


PHASES["p4"] = lambda nc, cfg, L, xin, W, tens, xout: phase4(nc, cfg, L, xin, W, tens)
PHASES["p5"] = lambda nc, cfg, L, xin, W, tens, xout: phase5(nc, cfg, L, W, tens, xout)


def full_plan():
    plan = []
    for L in range(DEPTH):
        xin = "x" if L == 0 else "xmid"
        xout = "xmid" if L == 0 else "out"
        plan += [("p1", L, xin, None), ("p2", L, None, None), ("p3", L, None, None), ("p4", L, xin, None), ("p5", L, None, xout)]
    return plan


_CACHE = {}


def kernel(x, g_mix, w_in, conv_w, conv_b, b_gates, g_q, g_k, w_br_a, w_br_b, w_out, g_ffn, w_gu, w_down):
    x = np.asarray(x, dtype=np.float32)
    B, S, _ = x.shape
    nseq = B // NCORES
    cfg = Cfg(nseq=nseq, S=S)
    key = (nseq, S)
    if key not in _CACHE:
        _CACHE[key] = build_program(cfg, full_plan())
    nc = _CACHE[key]
    Wd = host_layout_weights(dict(conv_w=conv_w, conv_b=conv_b, g_q=g_q, g_k=g_k, g_mix=g_mix, w_in=w_in, b_gates=b_gates,
                                  w_br_a=w_br_a, w_br_b=w_br_b, w_out=w_out, g_ffn=g_ffn, w_gu=w_gu, w_down=w_down))
    in_maps = []
    for c in range(NCORES):
        m = dict(Wd)
        m["x"] = np.ascontiguousarray(x[c * nseq:(c + 1) * nseq].reshape(nseq * S, D))
        in_maps.append(m)
    res = run_bass_kernel_spmd(nc, in_maps, core_ids=list(range(NCORES)))
    out = np.concatenate([np.asarray(r["out"], dtype=np.float32).reshape(nseq, S, D) for r in res.results], axis=0)
    return out
```

```python
import math
from contextlib import ExitStack

import numpy as np
import concourse.bass as bass
import concourse.mybir as mybir
from concourse.bass_utils import run_bass_kernel_spmd

F32 = mybir.dt.float32
BF16 = mybir.dt.bfloat16
AF = mybir.ActivationFunctionType
ALU = mybir.AluOpType

D = 1024
DEPTH = 2
NCORES = 8
ML_H = 4
SB_H = 8
D_FF = 2816
IN_COLS = 5640
EPS = 1e-6
T = 512
C_QK, C_VM, C_OM, C_G, C_QS, C_KS, C_VS, C_GP = 0, 1024, 1536, 2048, 2056, 2568, 3080, 3592
NEG = -30000.0


class Cfg:
    def __init__(self, nseq=2, S=4096):
        self.nseq = nseq
        self.S = S
        self.ntok = nseq * S
        self.ntile = self.ntok // T
        self.tps = S // T


class Sem:
    def __init__(self, h):
        self.h = h
        self.v = 0


class Phase:
    def __init__(self, nc, name):
        self.nc = nc
        self.name = name
        self.es = ExitStack()
        self.waited = {}
        self.engs = {"pe": nc.tensor, "act": nc.scalar, "dve": nc.vector, "pool": nc.gpsimd, "sp": nc.sync}
        self.esem = {}
        self.nsem = 0
        self.all_sems = []
        self.final = []

    def __enter__(self):
        self.es.__enter__()
        for e in ("pe", "act", "dve", "pool"):
            self.esem[e] = self.sem(e)
        return self

    def __exit__(self, *a):
        if a[0] is None:
            self.wait("sp", *self.final)
            self.nc.all_engine_barrier()
            for h in self.all_sems:
                self.nc.gpsimd.sem_clear(h)
            self.nc.all_engine_barrier()
        return self.es.__exit__(*a)

    def sem(self, name):
        self.nsem += 1
        h = self.es.enter_context(self.nc.semaphore(f"{self.name}_{name}_{self.nsem}"))
        self.all_sems.append(h)
        return Sem(h)

    def sb(self, name, shape, dt):
        return self.es.enter_context(self.nc.sbuf_tensor(f"{self.name}_{name}", list(shape), dt))

    def ps(self, name, shape, dt):
        return self.es.enter_context(self.nc.psum_tensor(f"{self.name}_{name}", list(shape), dt))

    def done(self, e, ins):
        s = self.esem[e]
        if s.v >= 30000:
            s = self.sem(e)
            self.esem[e] = s
        ins.then_inc(s.h, 1)
        s.v += 1
        return (s, s.v)

    def wait(self, e, *toks):
        eng = self.engs[e]
        for tok in toks:
            if tok is None:
                continue
            if isinstance(tok, (list,)):
                self.wait(e, *tok)
                continue
            s, v = tok
            key = (e, id(s))
            if self.waited.get(key, 0) >= v:
                continue
            eng.wait_ge(s.h, v)
            self.waited[key] = v

    def dma(self, q, out, in_, sem, **kw):
        ins = self.engs[q].dma_start(out=out, in_=in_, **kw)
        ins.then_inc(sem.h, 16)
        sem.v += 16
        return (sem, sem.v)


def make_consts(ph):
    P = ph.engs["pool"]
    c = {}
    f1 = ph.sb("c_f1", [128, 128], F32)
    c["identb"] = ph.sb("c_identb", [128, 128], BF16)
    t = ph.done("pool", P.memset(f1[:], 1.0))
    ph.wait("pool", t)
    t = ph.done("pool", P.affine_select(out=f1[:], in_=f1[:], pattern=[[-1, 128]], compare_op=ALU.is_equal,
                                        fill=0.0, base=0, channel_multiplier=1))
    ph.wait("pool", t)
    c["t_identb"] = ph.done("pool", P.tensor_copy(out=c["identb"][:], in_=f1[:]))
    c["_f1"] = f1
    return c


def tri_f32(ph, name, val, kind):
    P = ph.engs["pool"]
    t_ = ph.sb(name, [128, 128], F32)
    t = ph.done("pool", P.memset(t_[:], val))
    if kind != "all":
        ph.wait("pool", t)
        if kind == "le":
            pat, cm = [[1, 128]], -1
        else:
            pat, cm = [[-1, 128]], 1
        t = ph.done("pool", P.affine_select(out=t_[:], in_=t_[:], pattern=pat, compare_op=ALU.is_ge,
                                            fill=0.0, base=0, channel_multiplier=cm))
    return t_, t


def tri_bf16(ph, name, val, kind):
    P = ph.engs["pool"]
    f, t = tri_f32(ph, name + "_f", val, kind)
    b = ph.sb(name, [128, 128], BF16)
    ph.wait("pool", t)
    t = ph.done("pool", P.tensor_copy(out=b[:], in_=f[:]))
    return b, t


def phase1(nc, cfg, L, x_in, W, scr):
    with Phase(nc, f"p1l{L}") as ph:
        PE, ACT, DVE, POOL, SP = (ph.engs[k] for k in ("pe", "act", "dve", "pool", "sp"))
        Win = ph.sb("win", [128, 8, IN_COLS], BF16)
        gmix = ph.sb("gmix", [128, D], F32)
        cw = ph.sb("cw", [128, 32], F32)
        cb = ph.sb("cb", [128, 8], F32)
        bg = ph.sb("bg", [128, 8], F32)
        gqk = ph.sb("gqk", [128, 2], F32)
        xt = [ph.sb("xt0", [128, 4, D], F32)] * 2
        stat = [ph.sb(f"stat{i}", [128, 16], F32) for i in range(2)]
        hbf = [ph.sb(f"hbf{i}", [128, D], BF16) for i in range(4)]
        hT = [ph.sb("hT0", [128, 8, T], BF16)] * 2
        pre = ph.sb("pre", [128, 8, T + 3], F32)
        acc = [ph.sb(f"acc{i}", [128, T], F32) for i in range(2)]
        sq = [ph.sb(f"sq{i}", [128, T], BF16) for i in range(2)]
        rstd = [ph.sb(f"rstd{i}", [128, T], F32) for i in range(2)]
        gsb = [ph.sb(f"gsb{i}", [128, 64], F32) for i in range(2)]
        bg4 = ph.sb("bg4", [128, 32], F32)
        qk_st = ph.sb("qk_st", [128, 8, T], BF16)
        km_st = ph.sb("km_st", [128, 4, 512], BF16)
        vm_st = ph.sb("vm_st", [128, 4, 512], BF16)
        om_st = ph.sb("om_st", [128, 4, 512], BF16)
        vs_st = ph.sb("vs_st", [128, 4, 512], BF16)
        qs_st = ph.sb("qs_st", [128, 4, T], BF16)
        ks_st = ph.sb("ks_st", [128, 4, T], BF16)
        g_st = ph.sb("g_st", [128, 16, T], BF16)
        g12_st = ph.sb("g12_st", [128, 4, 12], F32)
        pf = ph.ps("pf", [128, 6, 512], F32)
        pb = ph.ps("pb", [128, 2, 1024], BF16)

        cst = make_consts(ph)
        negU, t_negU = tri_f32(ph, "negU", -1.0, "le")
        negO, t_negO = tri_f32(ph, "negO", -1.0, "all")
        bones = ph.sb("bones", [128, 128], BF16)
        t = ph.done("pool", POOL.memset(bones[:], 0.0))
        ph.wait("pool", t)
        POOL.memset(bones[0:64, 0:64], 1.0 / 64)
        t_bones = ph.done("pool", POOL.memset(bones[64:128, 64:128], 1.0 / 64))

        s_w = ph.sem("w")
        s_p = ph.sem("par")
        t_par = []
        t_par.append(ph.dma("sp", gmix[:], W["g_mix"][L:L + 1, :].partition_broadcast(128), s_p))
        t_par.append(ph.dma("sp", cw[:], W["cwl"][L], s_p))
        t_par.append(ph.dma("sp", cb[:], W["cbl"][L], s_p))
        t_par.append(ph.dma("sp", bg[:], W["b_gates"][L:L + 1, :].partition_broadcast(128), s_p))
        for b in range(4):
            t_par.append(ph.dma("sp", bg4[:, b * 8:(b + 1) * 8], W["b_gates"][L:L + 1, :].partition_broadcast(128), s_p))
        t_par.append(ph.dma("sp", gqk[:], W["gqk"][L], s_p))
        t_par = t_par[-1]
        t_w = None
        wv = W["w_in"][L].rearrange("(k p) n -> p k n", p=128)
        for k in range(8):
            t_w = ph.dma("pool", Win[:, k, :], wv[:, k, :], s_w)
        ph.wait("dve", t_par)
        t_gq = ph.done("dve", DVE.tensor_scalar_mul(out=gqk[:, 0:1], in0=gqk[:, 0:1], scalar1=0.125))

        s_x = [ph.sem("x0")] * 2
        st_sems = {k: ph.sem("st_" + k) for k in ("qk", "km", "vm", "om", "vs", "qs", "ks", "g", "g12")}
        st_tok = {k: None for k in st_sems}
        xt_free = [None]
        hT_free = [None, None]
        hbf_free = [None] * 4
        stat_free = [None, None]
        tp_free = None
        kmT_free = None
        mm_free = [None] * 4
        ss_box = [None]
        small_free = None
        acc_free = [None, None]
        sq_free = [None, None]
        rstd_free = [None, None]
        gsb_free = [None, None]
        pre_free = [None] * 8
        state = {"mm": 0, "acc": 0, "sq": 0, "gsb": 0}
        LOGS = -0.5 * math.log(128.0)

        def mm_bank():
            b = state["mm"] % 4
            state["mm"] += 1
            return b

        x_toks = {}
        h_toks = {}
        tpst = {"tp_free": None, 0: None, 1: None}

        def emit_xload(i):
            r0 = i * T
            ph.wait("sp", xt_free[0])
            x_toks[i] = ph.dma("sp", xt[0][:], x_in[r0:r0 + T, :].rearrange("(b p) d -> p b d", p=128), s_x[0])

        def emit_norm(i):
            slot = i % 2
            st = stat[slot]
            toks = []
            for b in range(4):
                ph.wait("act", x_toks[i], stat_free[slot] if b == 0 else None, hbf_free[b])
                t = ph.done("act", ACT.activation(out=hbf[b][:], in_=xt[0][:, b, :], func=AF.Square,
                                                  accum_out=st[:, b:b + 1]))
                ph.wait("act", t)
                t = ph.done("act", ACT.activation(out=st[:, 4 + b:5 + b], in_=st[:, b:b + 1], func=AF.Ln,
                                                  bias=EPS, scale=1.0 / D))
                ph.wait("act", t)
                t_r = ph.done("act", ACT.activation(out=st[:, 8 + b:9 + b], in_=st[:, 4 + b:5 + b], func=AF.Exp,
                                                    scale=-0.5))
                ph.wait("dve", t_r, t_par, hbf_free[b])
                t_h = ph.done("dve", DVE.scalar_tensor_tensor(out=hbf[b][:], in0=xt[0][:, b, :],
                                                              scalar=st[:, 8 + b:9 + b], in1=gmix[:],
                                                              op0=ALU.mult, op1=ALU.mult))
                toks.append(t_h)
            xt_free[0] = toks[-1]
            stat_free[slot] = toks[-1]
            h_toks[i] = toks

        def emit_transposes(i):
            slot = i % 2
            ready = []
            for b in range(4):
                tb = b % 2
                ph.wait("pe", h_toks[i][b], cst["t_identb"], tpst[tb])
                for c in range(8):
                    ins = PE.transpose(out=pb[:, tb, c * 128:(c + 1) * 128], in_=hbf[b][:, c * 128:(c + 1) * 128],
                                       identity=cst["identb"][:])
                t_tp = ph.done("pe", ins)
                hbf_free[b] = t_tp
                ph.wait("act", t_tp, hT_free[0] if b == 0 else None)
                t_e = ph.done("act", ACT.activation(out=hT[slot][:, :, b * 128:(b + 1) * 128],
                                                    in_=pb[:, tb, :].rearrange("p (c t) -> p c t", c=8),
                                                    func=AF.Copy))
                tpst[tb] = t_e
                ready.append(t_e)
            return ready

        emit_xload(0)
        emit_norm(0)
        if cfg.ntile > 1:
            emit_xload(1)
        hT_ready_next = emit_transposes(0)
        for i in range(cfg.ntile):
            slot = i % 2
            seq_start = (i % cfg.tps == 0)
            r0 = i * T
            hT_ready = hT_ready_next
            hTs = hT[slot]

            def fm_group(col0):
                bk = mm_bank()
                ph.wait("pe", hT_ready, t_w, mm_free[bk])
                for k in range(8):
                    ins = PE.matmul(pf[:, bk, :], lhsT=Win[:, k, col0:col0 + 128], rhs=hTs[:, k, :],
                                    start=(k == 0), stop=(k == 7))
                return bk, ph.done("pe", ins)

            def tm_group(b, col0, n):
                bk = mm_bank()
                ph.wait("pe", hT_ready, t_w, mm_free[bk])
                for k in range(8):
                    ins = PE.matmul(pf[:, bk, 0:n], lhsT=hTs[:, k, b * 128:(b + 1) * 128], rhs=Win[:, k, col0:col0 + n],
                                    start=(k == 0), stop=(k == 7))
                return bk, ph.done("pe", ins)

            if seq_start:
                ph.wait("dve", [pre_free[c] for c in range(8)])
                t_z = ph.done("dve", DVE.memset(pre[:, :, 0:3], 0.0))
            else:
                t_z = None
            si_toks = {}
            for c in range(8):
                bk, t_m = fm_group(C_QK + c * 128)
                ph.wait("act", t_m, pre_free[c], t_z)
                t_cp = ph.done("act", ACT.activation(out=pre[:, c, 3:T + 3], in_=pf[:, bk, :], func=AF.Copy))
                mm_free[bk] = t_cp
                ai = state["acc"] % 2
                state["acc"] += 1
                a_ = acc[ai]
                en, EN = ("dve", DVE)
                ph.wait(en, t_cp, t_par, acc_free[ai], t_z)
                ph.wait("act", t_cp, t_par, acc_free[ai], t_z)
                t = ph.done("act", ACT.activation(out=a_[:], in_=pre[:, c, 0:T], func=AF.Copy, scale=cw[:, c * 4:c * 4 + 1]))
                for j in range(1, 4):
                    ph.wait(en, t)
                    t = ph.done(en, EN.scalar_tensor_tensor(out=a_[:], in0=pre[:, c, j:j + T],
                                                            scalar=cw[:, c * 4 + j:c * 4 + j + 1], in1=a_[:],
                                                            op0=ALU.mult, op1=ALU.add))
                t_acc = t
                ph.wait(en, t_acc)
                t_halo = ph.done(en, EN.tensor_copy(out=pre[:, c, 0:3], in_=pre[:, c, T:T + 3]))
                pre_free[c] = t_halo
                ph.wait("act", t_acc, st_tok["qk"] if c == 0 else None, st_tok["km"] if c == 4 else None)
                t_si = ph.done("act", ACT.activation(out=qk_st[:, c, :], in_=a_[:], func=AF.Silu, bias=cb[:, c:c + 1]))
                acc_free[ai] = t_si
                si_toks[c] = t_si
            ph.wait("sp", t_si)
            st_tok["qk"] = ph.dma("sp", scr["qkT"][:, :, r0:r0 + T].rearrange("c p t -> p c t"), qk_st[:], st_sems["qk"])
            ph.wait("pe", small_free, hT_ready, t_w)
            for b in range(4):
                for k in range(8):
                    ins = PE.matmul(pf[:, 5, b * 8:(b + 1) * 8], lhsT=hTs[:, k, b * 128:(b + 1) * 128], rhs=Win[:, k, C_G:C_G + 8],
                                    start=(k == 0), stop=(k == 7))
            t_g = ph.done("pe", ins)
            gi = state["gsb"] % 2
            state["gsb"] += 1
            gs = gsb[gi]
            gs3 = gs[:, 0:32].rearrange("p (b n) -> p b n", b=4)
            ph.wait("dve", t_g, t_par, gsb_free[gi])
            t = ph.done("dve", DVE.tensor_tensor(out=gs3, in0=pf[:, 5, 0:32].rearrange("p (b n) -> p b n", b=4),
                                                 in1=bg4[:].rearrange("p (b n) -> p b n", b=4), op=ALU.add))
            ph.wait("act", t)
            t = ph.done("act", ACT.activation(out=gs[:, 32:48].rearrange("p (b n) -> p b n", b=4), in_=gs3[:, :, 4:8], func=AF.Exp, scale=-1.0))
            ph.wait("act", t)
            t_sp = ph.done("act", ACT.activation(out=gs[:, 48:64], in_=gs[:, 32:48], func=AF.Ln, bias=1.0))
            for b in range(4):
                bk, t_m = tm_group(b, C_VM, 512)
                ph.wait("act", t_m, st_tok["vm"] if b == 0 else None)
                t_vm = ph.done("act", ACT.activation(out=vm_st[:, b, :], in_=pf[:, bk, :], func=AF.Copy))
                mm_free[bk] = t_vm
                bk, t_m = tm_group(b, C_VS, 512)
                ph.wait("act", t_m, st_tok["vs"] if b == 0 else None)
                t_vs = ph.done("act", ACT.activation(out=vs_st[:, b, :], in_=pf[:, bk, :], func=AF.Copy))
                mm_free[bk] = t_vs
            ph.wait("pe", t_sp, t_negU, t_negO)
            for b in range(4):
                PE.matmul(pf[:, 5, 32 + b * 8:36 + b * 8], lhsT=negU[:], rhs=gs[:, 48 + b * 4:52 + b * 4], start=True, stop=True)
                ins = PE.matmul(pf[:, 5, 36 + b * 8:40 + b * 8], lhsT=negO[:], rhs=gs[:, 48 + b * 4:52 + b * 4], start=True, stop=True)
            t_b = ph.done("pe", ins)
            bps3 = pf[:, 5, 32:64].rearrange("p (b n) -> p b n", b=4)
            ph.wait("act", t_b, st_tok["g12"])
            t_e1 = ph.done("act", ACT.activation(out=g12_st[:, :, 0:8], in_=bps3, func=AF.Exp))
            ph.wait("dve", t_b, t_e1)
            t = ph.done("dve", DVE.tensor_tensor(out=gs[:, 32:48].rearrange("p (b n) -> p b n", b=4), in0=gs3[:, :, 0:4],
                                                 in1=bps3[:, :, 0:4], op=ALU.subtract))
            ph.wait("act", t)
            t_e2 = ph.done("act", ACT.activation(out=g12_st[:, :, 8:12], in_=gs[:, 32:48].rearrange("p (b n) -> p b n", b=4),
                                                 func=AF.Exp, bias=LOGS))
            small_free = [t_e1, t]
            gsb_free[gi] = t_e2
            g12_last = t_e2
            ph.wait("sp", g12_last)
            st_tok["g12"] = ph.dma("sp", scr["g12"][r0:r0 + T, :].rearrange("(b p) n -> p b n", p=128), g12_st[:], st_sems["g12"])
            ph.wait("sp", t_vm)
            st_tok["vm"] = ph.dma("sp", scr["vm"][r0:r0 + T, :].rearrange("(b p) n -> p b n", p=128), vm_st[:], st_sems["vm"])
            ph.wait("sp", t_vs)
            st_tok["vs"] = ph.dma("sp", scr["vs"][r0:r0 + T, :].rearrange("(b p) n -> p b n", p=128), vs_st[:], st_sems["vs"])

            for c in range(4, 8):
                h = c - 4
                kb_ = h % 2
                ph.wait("pe", si_toks[c], tpst[kb_])
                for b in range(4):
                    ins = PE.transpose(out=pb[:, kb_, b * 128:(b + 1) * 128], in_=qk_st[:, c, b * 128:(b + 1) * 128],
                                       identity=cst["identb"][:])
                t_t = ph.done("pe", ins)
                ph.wait("act", t_t, st_tok["km"])
                t_k = ph.done("act", ACT.activation(out=km_st[:, :, h * 128:(h + 1) * 128],
                                                    in_=pb[:, kb_, 0:512].rearrange("p (b d) -> p b d", b=4), func=AF.Copy))
                tpst[kb_] = t_k
            ph.wait("sp", t_k)
            st_tok["km"] = ph.dma("sp", scr["km"][r0:r0 + T, :].rearrange("(b p) n -> p b n", p=128), km_st[:], st_sems["km"])

            qjobs = [(col, stg, key, gcol, cc) for (col, stg, key, gcol) in ((C_QS, qs_st, "qs", 0), (C_KS, ks_st, "ks", 1))
                     for cc in range(4)]
            pend = None

            def qk_tail(job):
                (col, stg, key, gcol, cc), bk, si, t_sq = job
                nonlocal_ss = ss_box
                ph.wait("pe", t_sq, t_bones, nonlocal_ss[0])
                t_ss = ph.done("pe", PE.matmul(pf[:, 4, :], lhsT=bones[:], rhs=sq[si][:], start=True, stop=True))
                sq_free[si] = t_ss
                ph.wait("act", t_ss, rstd_free[si])
                t_ln = ph.done("act", ACT.activation(out=rstd[si][:], in_=pf[:, 4, :], func=AF.Ln, bias=EPS))
                nonlocal_ss[0] = t_ln
                ph.wait("act", t_ln)
                t_rs = ph.done("act", ACT.activation(out=rstd[si][:], in_=rstd[si][:], func=AF.Exp, scale=-0.5))
                ph.wait("dve", t_rs, t_gq, st_tok[key] if cc == 0 else None)
                t_o = ph.done("dve", DVE.scalar_tensor_tensor(out=stg[:, cc, :], in0=pf[:, bk, :],
                                                              scalar=gqk[:, gcol:gcol + 1], in1=rstd[si][:],
                                                              op0=ALU.mult, op1=ALU.mult))
                mm_free[bk] = t_o
                rstd_free[si] = t_o
                if cc == 3:
                    ph.wait("sp", t_o)
                    st_tok[key] = ph.dma("sp", scr[key + "T"][:, :, r0:r0 + T].rearrange("c p t -> p c t"), stg[:], st_sems[key])

            for job in qjobs:
                col, stg, key, gcol, cc = job
                bk, t_m = fm_group(col + cc * 128)
                si = state["sq"] % 2
                state["sq"] += 1
                ph.wait("act", t_m, sq_free[si])
                t_sq = ph.done("act", ACT.activation(out=sq[si][:], in_=pf[:, bk, :], func=AF.Square))
                if pend is not None:
                    qk_tail(pend)
                pend = (job, bk, si, t_sq)
            qk_tail(pend)

            if i + 1 < cfg.ntile:
                emit_norm(i + 1)
                if i + 2 < cfg.ntile:
                    emit_xload(i + 2)
            for c in range(16):
                bk, t_m = fm_group(C_GP + c * 128)
                ph.wait("act", t_m, st_tok["g"] if c == 0 else None)
                t_o = ph.done("act", ACT.activation(out=g_st[:, c, :], in_=pf[:, bk, :], func=AF.Sigmoid))
                mm_free[bk] = t_o
            ph.wait("sp", t_o)
            st_tok["g"] = ph.dma("sp", scr["gT"][:, :, r0:r0 + T].rearrange("c p t -> p c t"), g_st[:], st_sems["g"])
            for b in range(4):
                bk, t_m = tm_group(b, C_OM, 512)
                ph.wait("act", t_m, st_tok["om"] if b == 0 else None)
                t_o = ph.done("act", ACT.activation(out=om_st[:, b, :], in_=pf[:, bk, :], func=AF.Sigmoid))
                mm_free[bk] = t_o
            ph.wait("sp", t_o)
            st_tok["om"] = ph.dma("sp", scr["om"][r0:r0 + T, :].rearrange("(b p) n -> p b n", p=128), om_st[:], st_sems["om"])

            hT_free[0] = t_m
            if i + 1 < cfg.ntile:
                hT_ready_next = emit_transposes(i + 1)
        ph.final = [v for v in st_tok.values() if v is not None]


def scratch_specs(cfg):
    n = cfg.ntok
    return {
        "qkT": ([8, 128, n], BF16), "km": ([n, 512], BF16), "vm": ([n, 512], BF16), "om": ([n, 512], BF16),
        "g12": ([n, 12], F32), "qsT": ([4, 128, n], BF16), "ksT": ([4, 128, n], BF16), "vs": ([n, 512], BF16),
        "gT": ([16, 128, n], BF16), "yaT": ([4, 128, n], BF16), "ybT": ([4, 128, n], BF16),
        "x1": ([n, D], F32), "xmid": ([n, D], F32),
    }


WEIGHT_SPECS = {
    "g_mix": [DEPTH, D], "w_in": [DEPTH, D, IN_COLS], "cwl": [DEPTH, 128, 32], "cbl": [DEPTH, 128, 8],
    "b_gates": [DEPTH, 8], "gqk": [DEPTH, 128, 2], "w_br_a": [DEPTH, 512, D], "w_br_b": [DEPTH, 512, D],
    "w_out": [DEPTH, D, D], "g_ffn": [DEPTH, D], "w_gu": [DEPTH, D, 2 * D_FF], "w_down": [DEPTH, D_FF, D],
}


def build_program(cfg, plan, ext_in=(), ext_out=()):
    nc = bass.Bass("TRN2", target_bir_lowering=False)
    W = {k: nc.dram_tensor(k, s, F32, kind="ExternalInput").ap() for k, s in WEIGHT_SPECS.items()}
    tens = {}
    tens["x"] = nc.dram_tensor("x", [cfg.ntok, D], F32, kind="ExternalInput").ap()
    tens["out"] = nc.dram_tensor("out", [cfg.ntok, D], F32, kind="ExternalOutput").ap()
    for k, (s, dt) in scratch_specs(cfg).items():
        kind = "ExternalInput" if k in ext_in else ("ExternalOutput" if k in ext_out else "Internal")
        tens[k] = nc.dram_tensor(k, s, dt, kind=kind).ap()
    for (pname, L, xin, xout) in plan:
        PHASES[pname](nc, cfg, L, tens.get(xin), W, tens, tens.get(xout))
    return nc


def host_layout_weights(inp):
    f = lambda a: np.ascontiguousarray(np.asarray(a, dtype=np.float32))
    cw = f(inp["conv_w"])
    cwl = f(cw.reshape(DEPTH, 4, 8, 128).transpose(0, 3, 2, 1).reshape(DEPTH, 128, 32))
    cbl = f(f(inp["conv_b"]).reshape(DEPTH, 8, 128).transpose(0, 2, 1))
    gq = f(inp["g_q"])
    gk = f(inp["g_k"])
    gqk = f(np.stack([np.concatenate([gq, gq], 1), np.concatenate([gk, gk], 1)], axis=2))
    return {
        "g_mix": f(inp["g_mix"]), "w_in": f(inp["w_in"]), "cwl": cwl, "cbl": cbl, "b_gates": f(inp["b_gates"]),
        "gqk": gqk, "w_br_a": f(inp["w_br_a"]), "w_br_b": f(inp["w_br_b"]), "w_out": f(inp["w_out"]),
        "g_ffn": f(inp["g_ffn"]), "w_gu": f(inp["w_gu"]), "w_down": f(inp["w_down"]),
    }


PHASES = {"p1": lambda nc, cfg, L, xin, W, tens, xout: phase1(nc, cfg, L, xin, W, tens)}


def phase2(nc, cfg, L, scr):
    with Phase(nc, f"p2l{L}") as ph:
        PE, ACT, DVE, POOL, SP = (ph.engs[k] for k in ("pe", "act", "dve", "pool", "sp"))
        NS = cfg.nseq
        qk_sb = [[ph.sb(f"qk{s}{p}", [128, 8, T], BF16) for p in range(2)] for s in range(NS)]
        km_sb = [[ph.sb(f"km{s}{p}", [128, 4, 512], BF16) for p in range(2)] for s in range(NS)]
        va_sb = [[ph.sb(f"va{s}{p}", [128, 4, 4, 129], BF16) for p in range(2)] for s in range(NS)]
        om_sb = [[ph.sb(f"om{s}{p}", [128, 4, 512], BF16) for p in range(2)] for s in range(NS)]
        g_sb = [[ph.sb(f"g{s}{p}", [128, 4, 12], F32) for p in range(2)] for s in range(NS)]
        C32 = [ph.sb(f"C32_{s}", [128, 4, 129], F32) for s in range(NS)]
        Cbf = [ph.sb(f"Cbf_{s}", [128, 4, 129], BF16) for s in range(NS)]
        ya_sb = [ph.sb(f"ya{i}", [128, 512], BF16) for i in range(2)]
        yaT_st = [ph.sb(f"yaT{s}", [128, 4, T], BF16) for s in range(NS)]
        NR = 4
        sT_sb = [ph.sb(f"sT{i}", [128, 128], BF16) for i in range(NR)]
        kw_sb = [ph.sb(f"kw{i}", [128, 128], BF16) for i in range(NR)]
        wk2 = [ph.sb(f"wk2_{i}", [128, 4], F32) for i in range(NR)]
        dtmp = [ph.sb(f"dtmp{i}", [128, 4], F32) for i in range(NR)]
        pf = ph.ps("pf", [128, 6, 512], F32)
        pb = ph.ps("pb", [128, 2, 1024], BF16)
        cst = make_consts(ph)
        maskLE, t_mask = tri_f32(ph, "maskLE", 1.0, "le")
        t_init = []
        for s in range(NS):
            t_init.append(ph.done("pool", POOL.memset(C32[s][:], 0.0)))
            t_init.append(ph.done("pool", POOL.memset(Cbf[s][:], 0.0)))
            for p in range(2):
                t_init.append(ph.done("pool", POOL.memset(va_sb[s][p][:, :, :, 128:129], 1.0)))
        t_init = t_init[-1]

        s_ld = [[ph.sem(f"ld{s}{p}") for p in range(2)] for s in range(NS)]
        s_st = [ph.sem(f"st{s}") for s in range(NS)]
        st_tok = [None] * NS
        slot_free = [[[] for p in range(2)] for s in range(NS)]
        C_tok = [[t_init] * 4 for s in range(NS)]
        Cbf_tok = [[t_init] * 4 for s in range(NS)]
        Cbf_read = [[None] * 4 for s in range(NS)]
        st_free = [None, None]
        acc_free = [None, None]
        cps_free = [None, None]
        sT_free = [None] * NR
        kw_free = [None] * NR
        wk2_free = [None] * NR
        dtmp_free = [None] * NR
        ya_free = [None, None]
        pb_free = [None, None]
        cnt = {"u": 0, "c": 0}

        ld_toks = {}

        def emit_loads(t):
            par = t % 2
            for s in range(NS):
                r0 = s * cfg.S + t * T
                ph.wait("sp", slot_free[s][par])
                sem = s_ld[s][par]
                ph.dma("sp", qk_sb[s][par][:], scr["qkT"][:, :, r0:r0 + T].rearrange("c p t -> p c t"), sem)
                ph.dma("sp", km_sb[s][par][:], scr["km"][r0:r0 + T, :].rearrange("(b p) n -> p b n", p=128), sem)
                for b in range(4):
                    ph.dma("sp", va_sb[s][par][:, b, :, 0:128],
                           scr["vm"][r0 + b * 128:r0 + (b + 1) * 128, :].rearrange("p (h e) -> p h e", h=4), sem)
                ph.dma("sp", om_sb[s][par][:], scr["om"][r0:r0 + T, :].rearrange("(b p) n -> p b n", p=128), sem)
                ld_toks[(t, s)] = ph.dma("sp", g_sb[s][par][:], scr["g12"][r0:r0 + T, :].rearrange("(b p) n -> p b n", p=128), sem)

        emit_loads(0)
        for t in range(cfg.tps):
            par = t % 2
            if t + 1 < cfg.tps:
                emit_loads(t + 1)
            ld_tok = [ld_toks[(t, s)] for s in range(NS)]
            last = [None] * NS
            for j in range(4):
                jr = slice(j * 128, (j + 1) * 128)
                for s in range(NS):
                    qk, km, va, om, g = qk_sb[s][par], km_sb[s][par], va_sb[s][par], om_sb[s][par], g_sb[s][par]
                    ci = cnt["c"] % NR
                    cnt["c"] += 1
                    ph.wait("dve", ld_tok[s], wk2_free[ci])
                    t_wk2 = ph.done("dve", DVE.tensor_tensor(out=wk2[ci][:], in0=g[:, j, 8:12], in1=g[:, j, 4:8], op=ALU.mult))
                    yi = cnt["c"] % 2
                    y_toks = []
                    hs = [dict() for _ in range(4)]

                    def partA(h):
                        H = hs[h]
                        u = cnt["u"]
                        cnt["u"] += 1
                        H["r"] = r = u % NR
                        H["b2"] = b2 = u % 2
                        ph.wait("pe", ld_tok[s], st_free[b2])
                        t_st = ph.done("pe", PE.matmul(pf[:, b2, 0:128], lhsT=qk[:, 4 + h, jr], rhs=qk[:, h, jr], start=True, stop=True))
                        ph.wait("dve", t_st, t_mask, sT_free[r])
                        H["t_sT"] = ph.done("dve", DVE.scalar_tensor_tensor(out=sT_sb[r][:], in0=pf[:, b2, 0:128], scalar=g[:, j, 8 + h:9 + h],
                                                                            in1=maskLE[:], op0=ALU.mult, op1=ALU.mult))
                        st_free[b2] = H["t_sT"]
                        ph.wait("act", ld_tok[s], t_wk2, kw_free[r])
                        H["t_kw"] = ph.done("act", ACT.activation(out=kw_sb[r][:], in_=km[:, j, h * 128:(h + 1) * 128], func=AF.Copy,
                                                                  scale=wk2[ci][:, h:h + 1]))

                    def partPE(h):
                        H = hs[h]
                        r, b2 = H["r"], H["b2"]
                        ph.wait("pe", Cbf_tok[s][h], H["t_sT"], acc_free[b2])
                        PE.matmul(pf[:, 2 + b2, 0:129], lhsT=qk[:, h, jr], rhs=Cbf[s][:, h, :], start=True, stop=False)
                        H["t_acc"] = ph.done("pe", PE.matmul(pf[:, 2 + b2, 0:129], lhsT=sT_sb[r][:], rhs=va[:, j, h, :], start=False, stop=True))
                        sT_free[r] = H["t_acc"]
                        ph.wait("pe", H["t_kw"], cps_free[b2])
                        H["t_cps"] = ph.done("pe", PE.matmul(pf[:, 4 + b2, 0:129], lhsT=kw_sb[r][:], rhs=va[:, j, h, :], start=True, stop=True))
                        kw_free[r] = H["t_cps"]

                    def partB(h):
                        H = hs[h]
                        r, b2 = H["r"], H["b2"]
                        t_acc, t_cps = H["t_acc"], H["t_cps"]
                        ph.wait("dve", t_cps, C_tok[s][h], Cbf_tok[s][h])
                        t_c = ph.done("dve", DVE.scalar_tensor_tensor(out=C32[s][:, h, :], in0=C32[s][:, h, :], scalar=g[:, j, 4 + h:5 + h],
                                                                      in1=pf[:, 4 + b2, 0:129], op0=ALU.mult, op1=ALU.add))
                        C_tok[s][h] = t_c
                        cps_free[b2] = t_c
                        ph.wait("act", t_c, t_acc)
                        Cbf_tok[s][h] = ph.done("act", ACT.activation(out=Cbf[s][:, h, :], in_=C32[s][:, h, :], func=AF.Copy))
                        d = dtmp[r]
                        ph.wait("dve", t_acc, dtmp_free[r])
                        t1 = ph.done("dve", DVE.tensor_tensor(out=d[:, 0:1], in0=pf[:, 2 + b2, 128:129], in1=g[:, j, h:h + 1], op=ALU.mult))
                        ph.wait("dve", t1)
                        t1 = ph.done("dve", DVE.scalar_tensor_tensor(out=d[:, 1:2], in0=d[:, 0:1], scalar=-1.0, in1=d[:, 0:1],
                                                                     op0=ALU.mult, op1=ALU.max))
                        ph.wait("dve", t1)
                        t1 = ph.done("dve", DVE.tensor_scalar_max(out=d[:, 1:2], in0=d[:, 1:2], scalar1=1.0))
                        ph.wait("dve", t1)
                        t1 = ph.done("dve", DVE.reciprocal(out=d[:, 2:3], in_=d[:, 1:2]))
                        ph.wait("dve", t1)
                        t1 = ph.done("dve", DVE.tensor_tensor(out=d[:, 3:4], in0=d[:, 2:3], in1=g[:, j, h:h + 1], op=ALU.mult))
                        ph.wait("dve", t1, ya_free[yi] if h == 0 else None)
                        t_y = ph.done("dve", DVE.scalar_tensor_tensor(out=ya_sb[yi][:, h * 128:(h + 1) * 128], in0=pf[:, 2 + b2, 0:128],
                                                                      scalar=d[:, 3:4], in1=om[:, j, h * 128:(h + 1) * 128],
                                                                      op0=ALU.mult, op1=ALU.mult))
                        acc_free[b2] = t_y
                        dtmp_free[r] = t_y
                        y_toks.append(t_y)
                        last[s] = [t_y, t_cps, t_acc, H["t_kw"]]

                    for h in range(4):
                        partA(h)
                    t_kw = hs[3]["t_kw"]
                    for h0 in (0, 2):
                        partPE(h0)
                        partPE(h0 + 1)
                        partB(h0)
                        partB(h0 + 1)
                    wk2_free[ci] = t_kw
                    tb = cnt["c"] % 2
                    ph.wait("pe", y_toks, cst["t_identb"], pb_free[tb])
                    for h in range(4):
                        ins = PE.transpose(out=pb[:, tb, h * 128:(h + 1) * 128], in_=ya_sb[yi][:, h * 128:(h + 1) * 128],
                                           identity=cst["identb"][:])
                    t_tp = ph.done("pe", ins)
                    ya_free[yi] = t_tp
                    ph.wait("act", t_tp, st_tok[s] if j == 0 else None)
                    t_ev = ph.done("act", ACT.activation(out=yaT_st[s][:, :, jr], in_=pb[:, tb, 0:512].rearrange("p (h l) -> p h l", h=4),
                                                         func=AF.Copy))
                    pb_free[tb] = t_ev
                    last[s].append(t_ev)
            for s in range(NS):
                r0 = s * cfg.S + t * T
                ph.wait("act", last[s][-1])
                st_tok[s] = ph.dma("act", scr["yaT"][:, :, r0:r0 + T].rearrange("c p t -> p c t"), yaT_st[s][:], s_st[s])
                slot_free[s][par] = list(last[s])
        ph.final = [x for x in st_tok if x is not None]


PHASES["p2"] = lambda nc, cfg, L, xin, W, tens, xout: phase2(nc, cfg, L, tens)


def phase3(nc, cfg, L, scr):
    with Phase(nc, f"p3l{L}") as ph:
        PE, ACT, DVE, POOL, SP = (ph.engs[k] for k in ("pe", "act", "dve", "pool", "sp"))
        S = cfg.S
        NB = S // 128
        NQT = S // T
        kT = ph.sb("kT", [128, 4, S], BF16)
        qT = ph.sb("qT", [128, 4, S], BF16)
        vv = ph.sb("vv", [128, NB, 512], BF16)
        e_sb = [ph.sb(f"e{i}", [128, 2, T], F32) for i in range(2)]
        sp_sb = [ph.sb(f"sp{i}", [128, 2, T], BF16) for i in range(2)]
        Ss = [ph.sb(f"Ss{i}", [128, T], BF16) for i in range(3)]
        Stmp = ph.sb("Stmp", [128, T], BF16)
        aT_sb = [ph.sb(f"aT{i}", [128, 2, T], BF16) for i in range(2)]
        yb_st = [ph.sb(f"yb{i}", [64, T], BF16) for i in range(2)]
        pf = ph.ps("pf", [128, 8, 512], F32)
        cst = make_consts(ph)
        identb = cst["identb"]
        negTri, t_tri = tri_bf16(ph, "negTri", -1.0, "ge")
        negOne, t_one = tri_bf16(ph, "negOne", -1.0, "all")
        nm_f = ph.sb("nm_f", [128, T], F32)
        negmask = []
        t_nm = None
        for i in range(4):
            m = ph.sb(f"negmask{i}", [128, T], BF16)
            ph.wait("pool", t_nm)
            t = ph.done("pool", POOL.memset(nm_f[:], NEG))
            ph.wait("pool", t)
            t = ph.done("pool", POOL.affine_select(out=nm_f[:], in_=nm_f[:], pattern=[[-1, T]], compare_op=ALU.is_ge,
                                                   fill=0.0, base=i * 128, channel_multiplier=1))
            ph.wait("pool", t)
            t_nm = ph.done("pool", POOL.tensor_copy(out=m[:], in_=nm_f[:]))
            negmask.append(m)
        t_consts = [cst["t_identb"], t_tri, t_one, t_nm]

        s_ld = ph.sem("ld")
        s_yb = [ph.sem("yb0"), ph.sem("yb1")]
        zfree = [None, None]
        spfree = [None, None]
        Ssfree = [None, None, None]
        Stmp_free = [None]
        Afree = [None]
        aTfree = [None, None]
        ofree = [None, None]
        ybfree = [None, None]
        prev_done = []
        gk = {"k": 0, "grp": 0}

        for s in range(cfg.nseq):
            c0 = s * S
            ph.wait("sp", prev_done)
            ph.dma("sp", kT[:], scr["ksT"][:, :, c0:c0 + S].rearrange("c p t -> p c t"), s_ld)
            ph.dma("sp", qT[:], scr["qsT"][:, :, c0:c0 + S].rearrange("c p t -> p c t"), s_ld)
            ld_tok = ph.dma("sp", vv[:], scr["vs"][c0:c0 + S, :].rearrange("(b p) n -> p b n", p=128), s_ld)
            units = []
            for h in range(SB_H):
                for qt in range(NQT):
                    npair = 2 * (qt + 1)
                    for m in range(npair):
                        kb = 4 * qt + 3 - 2 * m
                        units.append(dict(h=h, qt=qt, m=m, kb=kb, last=(m == npair - 1), diag=(m < 2),
                                          i=kb - 4 * qt, grp=gk["grp"]))
                    gk["grp"] += 1
            NU = len(units)
            k0 = gk["k"]

            def operands(U, j):
                hc = U["h"] // 2
                p0 = (U["h"] % 2) * 64
                kb = U["kb"] - j
                lk = kT[p0:p0 + 64, hc, kb * 128:(kb + 1) * 128]
                rq = qT[p0:p0 + 64, hc, U["qt"] * T:(U["qt"] + 1) * T]
                return lk, rq

            def stage0(U, k):
                b = k % 2
                ph.wait("pe", ld_tok, t_consts, zfree[b])
                for j in range(2):
                    lk, rq = operands(U, j)
                    ins = PE.matmul(pf[:, 2 * b + j, :], lhsT=lk, rhs=rq, start=True, stop=not U["diag"])
                    if U["diag"]:
                        ins = PE.matmul(pf[:, 2 * b + j, :], lhsT=identb[:], rhs=negmask[U["i"] - j][:], start=False, stop=True)
                U["t_z"] = ph.done("pe", ins)
                ph.wait("act", U["t_z"])
                U["t_e"] = ph.done("act", ACT.activation(out=e_sb[b][:], in_=pf[:, 2 * b:2 * b + 2, :], func=AF.Exp))
                zfree[b] = U["t_e"]
                ph.wait("act", U["t_e"], spfree[b])
                U["t_sp"] = ph.done("act", ACT.activation(out=sp_sb[b][:], in_=e_sb[b][:], func=AF.Ln, bias=1.0))
                U["t_ss"] = None
                if not U["last"]:
                    m = U["m"]
                    dst = Ss[(k + 1) % 3]
                    ph.wait("dve", U["t_sp"], Ssfree[(k + 1) % 3])
                    if m == 0:
                        U["t_ss"] = ph.done("dve", DVE.tensor_tensor(out=dst[:], in0=sp_sb[b][:, 0, :], in1=sp_sb[b][:, 1, :], op=ALU.add))
                    else:
                        ph.wait("dve", U["t_ssprev"], Stmp_free[0])
                        t = ph.done("dve", DVE.tensor_tensor(out=Stmp[:], in0=Ss[k % 3][:], in1=sp_sb[b][:, 0, :], op=ALU.add))
                        ph.wait("dve", t)
                        U["t_ss"] = ph.done("dve", DVE.tensor_tensor(out=dst[:], in0=Stmp[:], in1=sp_sb[b][:, 1, :], op=ALU.add))
                        Stmp_free[0] = U["t_ss"]

            def stage1(U, k):
                b = k % 2
                m = U["m"]
                ph.wait("pe", U["t_sp"], Afree[0], U.get("t_ssprev"))
                for j in range(2):
                    lk, rq = operands(U, j)
                    mms = [(lk, rq), (negTri[:], sp_sb[b][:, j, :])]
                    if j == 1:
                        mms.append((negOne[:], sp_sb[b][:, 0, :]))
                    if m > 0:
                        mms.append((negOne[:], Ss[k % 3][:]))
                    if U["diag"]:
                        mms.append((identb[:], negmask[U["i"] - j][:]))
                    for jj, (l_, r_) in enumerate(mms):
                        ins = PE.matmul(pf[:, 4 + j, :], lhsT=l_, rhs=r_, start=(jj == 0), stop=(jj == len(mms) - 1))
                U["t_A"] = ph.done("pe", ins)
                if m > 0:
                    Ssfree[k % 3] = U["t_A"]
                spfree[b] = [U["t_A"], U["t_ss"]]
                ph.wait("act", U["t_A"], aTfree[b])
                U["t_a"] = ph.done("act", ACT.activation(out=aT_sb[b][:], in_=pf[:, 4:6, :], func=AF.Exp))
                Afree[0] = U["t_a"]

            def stage2(U, k):
                b = k % 2
                g2 = U["grp"] % 2
                h = U["h"]
                ph.wait("pe", U["t_a"], ofree[g2] if U["m"] == 0 else None)
                for j in range(2):
                    ins = PE.matmul(pf[0:64, 6 + g2, :], lhsT=vv[:, U["kb"] - j, h * 64:(h + 1) * 64], rhs=aT_sb[b][:, j, :],
                                    start=(U["m"] == 0 and j == 0), stop=(U["last"] and j == 1))
                U["t_av"] = ph.done("pe", ins)
                aTfree[b] = U["t_av"]
                if U["last"]:
                    ph.wait("dve", U["t_av"], ybfree[g2])
                    t_ev = ph.done("dve", DVE.tensor_copy(out=yb_st[g2][:], in_=pf[0:64, 6 + g2, :]))
                    ofree[g2] = t_ev
                    r0 = c0 + U["qt"] * T
                    p0 = (h % 2) * 64
                    ph.wait("sp", t_ev)
                    ybfree[g2] = ph.dma("sp", scr["ybT"][h // 2, p0:p0 + 64, r0:r0 + T], yb_st[g2][:], s_yb[g2])
                    U["t_ev"] = t_ev

            for step in range(NU + 2):
                if step < NU:
                    U = units[step]
                    if U["m"] > 0:
                        U["t_ssprev"] = units[step - 1]["t_ss"]
                    stage0(U, k0 + step)
                if 0 <= step - 1 < NU:
                    stage1(units[step - 1], k0 + step - 1)
                if 0 <= step - 2 < NU:
                    stage2(units[step - 2], k0 + step - 2)
            gk["k"] = k0 + NU
            lastU = units[-1]
            prev_done = [lastU["t_av"], lastU["t_a"], lastU["t_ev"]]
        ph.final = [x for x in ybfree if x is not None]


PHASES["p3"] = lambda nc, cfg, L, xin, W, tens, xout: phase3(nc, cfg, L, tens)


def phase4(nc, cfg, L, x_in, W, scr):
    with Phase(nc, f"p4l{L}") as ph:
        PE, ACT, DVE, POOL, SP = (ph.engs[k] for k in ("pe", "act", "dve", "pool", "sp"))
        Wa = ph.sb("wa", [128, 4, D], BF16)
        Wb = ph.sb("wb", [128, 4, D], BF16)
        Wo = ph.sb("wo", [128, 8, D], BF16)
        ya = [ph.sb(f"ya{i}", [128, 4, T], BF16) for i in range(2)]
        yb = [ph.sb(f"yb{i}", [128, 4, T], BF16) for i in range(2)]
        gT = [ph.sb(f"gT{i}", [128, 16, T], BF16) for i in range(2)]
        xs = [ph.sb(f"xs{i}", [128, 4, D], F32) for i in range(2)]
        mixT = ph.sb("mixT", [128, 8, T], BF16)
        tmpa = [ph.sb(f"tmpa{i}", [128, T], F32) for i in range(2)]
        tmpb = [ph.sb(f"tmpb{i}", [128, T], F32) for i in range(2)]
        pf = ph.ps("pf", [128, 8, 512], F32)
        s_w = ph.sem("w")
        ph.dma("pool", Wa[:], W["w_br_a"][L].rearrange("(k p) n -> p k n", p=128), s_w)
        ph.dma("pool", Wb[:], W["w_br_b"][L].rearrange("(k p) n -> p k n", p=128), s_w)
        wo_v = W["w_out"][L].rearrange("(k p) n -> p k n", p=128)
        ph.dma("pool", Wo[:, 0:4, :], wo_v[:, 0:4, :], s_w)
        t_w = ph.dma("pool", Wo[:, 4:8, :], wo_v[:, 4:8, :], s_w)
        s_ld = [ph.sem("ld0"), ph.sem("ld1")]
        s_st = [ph.sem("st0"), ph.sem("st1")]
        slot_free = [[], []]
        st_tok = [None, None]
        ld_toks = {}

        def emit_loads(i):
            p = i % 2
            r0 = i * T
            ph.wait("sp", slot_free[p], st_tok[p])
            ph.dma("sp", ya[p][:], scr["yaT"][:, :, r0:r0 + T].rearrange("c p t -> p c t"), s_ld[p])
            ph.dma("sp", yb[p][:], scr["ybT"][:, :, r0:r0 + T].rearrange("c p t -> p c t"), s_ld[p])
            ph.dma("sp", gT[p][:, 0:8, :], scr["gT"][0:8, :, r0:r0 + T].rearrange("c p t -> p c t"), s_ld[p])
            ph.dma("sp", gT[p][:, 8:16, :], scr["gT"][8:16, :, r0:r0 + T].rearrange("c p t -> p c t"), s_ld[p])
            ld_toks[i] = ph.dma("sp", xs[p][:], x_in[r0:r0 + T, :].rearrange("(b p) d -> p b d", p=128), s_ld[p])

        bank_free = [None] * 8
        tmpa_free = [None, None]
        tmpb_free = [None, None]
        mix_free = None
        emit_loads(0)
        cnt = 0
        for i in range(cfg.ntile):
            p = i % 2
            if i + 1 < cfg.ntile:
                emit_loads(i + 1)
            ld = ld_toks[i]
            mix_toks = []
            for c in range(8):
                ba, bb = (2 * c) % 4, (2 * c + 1) % 4
                ph.wait("pe", ld, t_w, bank_free[ba])
                for k in range(4):
                    ins = PE.matmul(pf[:, ba, :], lhsT=Wa[:, k, c * 128:(c + 1) * 128], rhs=ya[p][:, k, :], start=(k == 0), stop=(k == 3))
                t_pa = ph.done("pe", ins)
                ph.wait("pe", bank_free[bb])
                for k in range(4):
                    ins = PE.matmul(pf[:, bb, :], lhsT=Wb[:, k, c * 128:(c + 1) * 128], rhs=yb[p][:, k, :], start=(k == 0), stop=(k == 3))
                t_pb = ph.done("pe", ins)
                ti = c % 2
                ph.wait("dve", t_pa, ld, tmpa_free[ti])
                t_a = ph.done("dve", DVE.tensor_tensor(out=tmpa[ti][:], in0=pf[:, ba, :], in1=gT[p][:, c, :], op=ALU.mult))
                bank_free[ba] = t_a
                ph.wait("dve", t_pb, tmpb_free[ti])
                t_b = ph.done("dve", DVE.tensor_tensor(out=tmpb[ti][:], in0=pf[:, bb, :], in1=gT[p][:, 8 + c, :], op=ALU.mult))
                bank_free[bb] = t_b
                ph.wait("pool", t_a, t_b, mix_free if c == 0 else None)
                t_m = ph.done("pool", POOL.tensor_tensor(out=mixT[:, c, :], in0=tmpa[ti][:], in1=tmpb[ti][:], op=ALU.add))
                tmpa_free[ti] = t_m
                tmpb_free[ti] = t_m
                mix_toks.append(t_m)
            for b in range(4):
                for hf in range(2):
                    bk = 4 + (cnt % 4)
                    cnt += 1
                    ph.wait("pe", mix_toks, bank_free[bk])
                    for k in range(8):
                        ins = PE.matmul(pf[:, bk, :], lhsT=mixT[:, k, b * 128:(b + 1) * 128], rhs=Wo[:, k, hf * 512:(hf + 1) * 512],
                                        start=(k == 0), stop=(k == 7))
                    t_po = ph.done("pe", ins)
                    ph.wait("dve", t_po, ld)
                    t_x = ph.done("dve", DVE.tensor_tensor(out=xs[p][:, b, hf * 512:(hf + 1) * 512], in0=pf[:, bk, :],
                                                           in1=xs[p][:, b, hf * 512:(hf + 1) * 512], op=ALU.add))
                    bank_free[bk] = t_x
            mix_free = t_po
            r0 = i * T
            ph.wait("act", t_x)
            st_tok[p] = ph.dma("act", scr["x1"][r0:r0 + T, :].rearrange("(b p) d -> p b d", p=128), xs[p][:], s_st[p])
            slot_free[p] = [t_po, t_x, t_m]
        ph.final = [x for x in st_tok if x is not None]


def phase5(nc, cfg, L, W, scr, x_out):
    with Phase(nc, f"p5l{L}") as ph:
        PE, ACT, DVE, POOL, SP = (ph.engs[k] for k in ("pe", "act", "dve", "pool", "sp"))
        NC_FF = D_FF // 128
        Wgu = ph.sb("wgu", [128, 8, 2 * D_FF], BF16)
        Wd = ph.sb("wd", [128, NC_FF, D], BF16)
        gf = ph.sb("gf", [128, D], F32)
        xs = [ph.sb(f"xs{i}", [128, 4, D], F32) for i in range(2)]
        stat = [ph.sb(f"stat{i}", [128, 16], F32) for i in range(2)]
        hbf = [ph.sb(f"hbf{i}", [128, D], BF16) for i in range(4)]
        hT = ph.sb("hT", [128, 8, T], BF16)
        act = ph.sb("act", [128, NC_FF, T], BF16)
        pf = ph.ps("pf", [128, 6, 512], F32)
        pb = ph.ps("pb", [128, 2, 1024], BF16)
        cst = make_consts(ph)
        s_w = ph.sem("w")
        s_p = ph.sem("par")
        t_par = ph.dma("sp", gf[:], W["g_ffn"][L:L + 1, :].partition_broadcast(128), s_p)
        wg_v = W["w_gu"][L].rearrange("(k p) n -> p k n", p=128)
        for k in range(8):
            ph.dma("pool", Wgu[:, k, :], wg_v[:, k, :], s_w)
        wd_v = W["w_down"][L].rearrange("(k p) n -> p k n", p=128)
        for k0 in range(0, NC_FF, 6):
            k1 = min(NC_FF, k0 + 6)
            t_w = ph.dma("pool", Wd[:, k0:k1, :], wd_v[:, k0:k1, :], s_w)
        s_ld = [ph.sem("ld0"), ph.sem("ld1")]
        s_st = [ph.sem("st0"), ph.sem("st1")]
        slot_free = [[], []]
        st_tok = [None, None]
        ld_toks = {}

        def emit_loads(i):
            p = i % 2
            r0 = i * T
            ph.wait("sp", slot_free[p], st_tok[p])
            ld_toks[i] = ph.dma("sp", xs[p][:], scr["x1"][r0:r0 + T, :].rearrange("(b p) d -> p b d", p=128), s_ld[p])

        hbf_free = [None] * 4
        stat_free = [None, None]
        hT_free = None
        tp_free = [None, None]
        h_toks = {}

        def emit_norm(i):
            p = i % 2
            st = stat[p]
            toks = []
            for b in range(4):
                ph.wait("act", ld_toks[i], stat_free[p] if b == 0 else None, hbf_free[b])
                t = ph.done("act", ACT.activation(out=hbf[b][:], in_=xs[p][:, b, :], func=AF.Square, accum_out=st[:, b:b + 1]))
                ph.wait("act", t)
                t = ph.done("act", ACT.activation(out=st[:, 4 + b:5 + b], in_=st[:, b:b + 1], func=AF.Ln, bias=EPS, scale=1.0 / D))
                ph.wait("act", t)
                t_r = ph.done("act", ACT.activation(out=st[:, 8 + b:9 + b], in_=st[:, 4 + b:5 + b], func=AF.Exp, scale=-0.5))
                ph.wait("dve", t_r, t_par, hbf_free[b])
                t_h = ph.done("dve", DVE.scalar_tensor_tensor(out=hbf[b][:], in0=xs[p][:, b, :], scalar=st[:, 8 + b:9 + b], in1=gf[:],
                                                              op0=ALU.mult, op1=ALU.mult))
                toks.append(t_h)
            stat_free[p] = toks[-1]
            h_toks[i] = toks

        def emit_transposes(i):
            ready = []
            for b in range(4):
                tb = b % 2
                ph.wait("pe", h_toks[i][b], cst["t_identb"], tp_free[tb])
                for c in range(8):
                    ins = PE.transpose(out=pb[:, tb, c * 128:(c + 1) * 128], in_=hbf[b][:, c * 128:(c + 1) * 128], identity=cst["identb"][:])
                t_tp = ph.done("pe", ins)
                hbf_free[b] = t_tp
                ph.wait("act", t_tp, hT_free if b == 0 else None)
                t_e = ph.done("act", ACT.activation(out=hT[:, :, b * 128:(b + 1) * 128], in_=pb[:, tb, :].rearrange("p (c t) -> p c t", c=8),
                                                    func=AF.Copy))
                tp_free[tb] = t_e
                ready.append(t_e)
            return ready

        bank_free = [None] * 6
        sg_free = [None, None]
        act_free = None
        emit_loads(0)
        emit_norm(0)
        hT_ready = emit_transposes(0)
        cnt = 0
        for i in range(cfg.ntile):
            p = i % 2
            if i + 1 < cfg.ntile:
                emit_loads(i + 1)
            a_toks = []
            for c in range(NC_FF):
                bg_, bu_ = (2 * c) % 4, (2 * c + 1) % 4
                ph.wait("pe", hT_ready, t_w, bank_free[bg_])
                for k in range(8):
                    ins = PE.matmul(pf[:, bg_, :], lhsT=Wgu[:, k, c * 128:(c + 1) * 128], rhs=hT[:, k, :], start=(k == 0), stop=(k == 7))
                t_g = ph.done("pe", ins)
                ph.wait("pe", bank_free[bu_])
                for k in range(8):
                    ins = PE.matmul(pf[:, bu_, :], lhsT=Wgu[:, k, D_FF + c * 128:D_FF + (c + 1) * 128], rhs=hT[:, k, :],
                                    start=(k == 0), stop=(k == 7))
                t_u = ph.done("pe", ins)
                ph.wait("act", t_g, act_free if c == 0 else None)
                t_s = ph.done("act", ACT.activation(out=act[:, c, :], in_=pf[:, bg_, :], func=AF.Silu))
                bank_free[bg_] = t_s
                ph.wait("dve", t_s, t_u)
                t_a = ph.done("dve", DVE.tensor_tensor(out=act[:, c, :], in0=pf[:, bu_, :], in1=act[:, c, :], op=ALU.mult))
                bank_free[bu_] = t_a
                a_toks.append(t_a)
            hT_free = t_u
            if i + 1 < cfg.ntile:
                emit_norm(i + 1)
            for b in range(4):
                for hf in range(2):
                    bk = 4 + (cnt % 2)
                    cnt += 1
                    ph.wait("pe", a_toks, bank_free[bk])
                    for k in range(NC_FF):
                        ins = PE.matmul(pf[:, bk, :], lhsT=act[:, k, b * 128:(b + 1) * 128], rhs=Wd[:, k, hf * 512:(hf + 1) * 512],
                                        start=(k == 0), stop=(k == NC_FF - 1))
                    t_pd = ph.done("pe", ins)
                    ph.wait("dve", t_pd)
                    t_x = ph.done("dve", DVE.tensor_tensor(out=xs[p][:, b, hf * 512:(hf + 1) * 512], in0=pf[:, bk, :],
                                                           in1=xs[p][:, b, hf * 512:(hf + 1) * 512], op=ALU.add))
                    bank_free[bk] = t_x
            act_free = t_pd
            r0 = i * T
            ph.wait("act", t_x)
            st_tok[p] = ph.dma("act", x_out[r0:r0 + T, :].rearrange("(b p) d -> p b d", p=128), xs[p][:], s_st[p])
            slot_free[p] = [t_x]
            if i + 1 < cfg.ntile:
                hT_ready = emit_transposes(i + 1)
        ph.final = [x for x in st_tok if x is not None]


PHASES["p4"] = lambda nc, cfg, L, xin, W, tens, xout: phase4(nc, cfg, L, xin, W, tens)
PHASES["p5"] = lambda nc, cfg, L, xin, W, tens, xout: phase5(nc, cfg, L, W, tens, xout)


def full_plan():
    plan = []
    for L in range(DEPTH):
        xin = "x" if L == 0 else "xmid"
        xout = "xmid" if L == 0 else "out"
        plan += [("p1", L, xin, None), ("p2", L, None, None), ("p3", L, None, None), ("p4", L, xin, None), ("p5", L, None, xout)]
    return plan


_CACHE = {}


def kernel(x, g_mix, w_in, conv_w, conv_b, b_gates, g_q, g_k, w_br_a, w_br_b, w_out, g_ffn, w_gu, w_down):
    x = np.asarray(x, dtype=np.float32)
    B, S, _ = x.shape
    nseq = B // NCORES
    cfg = Cfg(nseq=nseq, S=S)
    key = (nseq, S)
    if key not in _CACHE:
        _CACHE[key] = build_program(cfg, full_plan())
    nc = _CACHE[key]
    Wd = host_layout_weights(dict(conv_w=conv_w, conv_b=conv_b, g_q=g_q, g_k=g_k, g_mix=g_mix, w_in=w_in, b_gates=b_gates,
                                  w_br_a=w_br_a, w_br_b=w_br_b, w_out=w_out, g_ffn=g_ffn, w_gu=w_gu, w_down=w_down))
    in_maps = []
    for c in range(NCORES):
        m = dict(Wd)
        m["x"] = np.ascontiguousarray(x[c * nseq:(c + 1) * nseq].reshape(nseq * S, D))
        in_maps.append(m)
    res = run_bass_kernel_spmd(nc, in_maps, core_ids=list(range(NCORES)))
    out = np.concatenate([np.asarray(r["out"], dtype=np.float32).reshape(nseq, S, D) for r in res.results], axis=0)
    return out
```

```python
import math
from contextlib import ExitStack

import numpy as np
import concourse.bass as bass
import concourse.mybir as mybir
from concourse.bass_utils import run_bass_kernel_spmd

F32 = mybir.dt.float32
BF16 = mybir.dt.bfloat16
AF = mybir.ActivationFunctionType
ALU = mybir.AluOpType

D = 1024
DEPTH = 2
NCORES = 8
ML_H = 4
SB_H = 8
D_FF = 2816
IN_COLS = 5640
EPS = 1e-6
T = 512
C_QK, C_VM, C_OM, C_G, C_QS, C_KS, C_VS, C_GP = 0, 1024, 1536, 2048, 2056, 2568, 3080, 3592
NEG = -30000.0


class Cfg:
    def __init__(self, nseq=2, S=4096):
        self.nseq = nseq
        self.S = S
        self.ntok = nseq * S
        self.ntile = self.ntok // T
        self.tps = S // T


class Sem:
    def __init__(self, h):
        self.h = h
        self.v = 0


class Phase:
    def __init__(self, nc, name):
        self.nc = nc
        self.name = name
        self.es = ExitStack()
        self.waited = {}
        self.engs = {"pe": nc.tensor, "act": nc.scalar, "dve": nc.vector, "pool": nc.gpsimd, "sp": nc.sync}
        self.esem = {}
        self.nsem = 0
        self.all_sems = []
        self.final = []

    def __enter__(self):
        self.es.__enter__()
        for e in ("pe", "act", "dve", "pool"):
            self.esem[e] = self.sem(e)
        return self

    def __exit__(self, *a):
        if a[0] is None:
            self.wait("sp", *self.final)
            self.nc.all_engine_barrier()
            for h in self.all_sems:
                self.nc.gpsimd.sem_clear(h)
            self.nc.all_engine_barrier()
        return self.es.__exit__(*a)

    def sem(self, name):
        self.nsem += 1
        h = self.es.enter_context(self.nc.semaphore(f"{self.name}_{name}_{self.nsem}"))
        self.all_sems.append(h)
        return Sem(h)

    def sb(self, name, shape, dt):
        return self.es.enter_context(self.nc.sbuf_tensor(f"{self.name}_{name}", list(shape), dt))

    def ps(self, name, shape, dt):
        return self.es.enter_context(self.nc.psum_tensor(f"{self.name}_{name}", list(shape), dt))

    def done(self, e, ins):
        s = self.esem[e]
        if s.v >= 30000:
            s = self.sem(e)
            self.esem[e] = s
        ins.then_inc(s.h, 1)
        s.v += 1
        return (s, s.v)

    def wait(self, e, *toks):
        eng = self.engs[e]
        for tok in toks:
            if tok is None:
                continue
            if isinstance(tok, (list,)):
                self.wait(e, *tok)
                continue
            s, v = tok
            key = (e, id(s))
            if self.waited.get(key, 0) >= v:
                continue
            eng.wait_ge(s.h, v)
            self.waited[key] = v

    def dma(self, q, out, in_, sem, **kw):
        ins = self.engs[q].dma_start(out=out, in_=in_, **kw)
        ins.then_inc(sem.h, 16)
        sem.v += 16
        return (sem, sem.v)


def make_consts(ph):
    P = ph.engs["pool"]
    c = {}
    f1 = ph.sb("c_f1", [128, 128], F32)
    c["identb"] = ph.sb("c_identb", [128, 128], BF16)
    t = ph.done("pool", P.memset(f1[:], 1.0))
    ph.wait("pool", t)
    t = ph.done("pool", P.affine_select(out=f1[:], in_=f1[:], pattern=[[-1, 128]], compare_op=ALU.is_equal,
                                        fill=0.0, base=0, channel_multiplier=1))
    ph.wait("pool", t)
    c["t_identb"] = ph.done("pool", P.tensor_copy(out=c["identb"][:], in_=f1[:]))
    c["_f1"] = f1
    return c


def tri_f32(ph, name, val, kind):
    P = ph.engs["pool"]
    t_ = ph.sb(name, [128, 128], F32)
    t = ph.done("pool", P.memset(t_[:], val))
    if kind != "all":
        ph.wait("pool", t)
        if kind == "le":
            pat, cm = [[1, 128]], -1
        else:
            pat, cm = [[-1, 128]], 1
        t = ph.done("pool", P.affine_select(out=t_[:], in_=t_[:], pattern=pat, compare_op=ALU.is_ge,
                                            fill=0.0, base=0, channel_multiplier=cm))
    return t_, t


def tri_bf16(ph, name, val, kind):
    P = ph.engs["pool"]
    f, t = tri_f32(ph, name + "_f", val, kind)
    b = ph.sb(name, [128, 128], BF16)
    ph.wait("pool", t)
    t = ph.done("pool", P.tensor_copy(out=b[:], in_=f[:]))
    return b, t


def phase1(nc, cfg, L, x_in, W, scr):
    with Phase(nc, f"p1l{L}") as ph:
        PE, ACT, DVE, POOL, SP = (ph.engs[k] for k in ("pe", "act", "dve", "pool", "sp"))
        Win = ph.sb("win", [128, 8, IN_COLS], BF16)
        gmix = ph.sb("gmix", [128, D], F32)
        cw = ph.sb("cw", [128, 32], F32)
        cb = ph.sb("cb", [128, 8], F32)
        bg = ph.sb("bg", [128, 8], F32)
        gqk = ph.sb("gqk", [128, 2], F32)
        xt = [ph.sb("xt0", [128, 4, D], F32)] * 2
        stat = [ph.sb(f"stat{i}", [128, 16], F32) for i in range(2)]
        hbf = [ph.sb(f"hbf{i}", [128, D], BF16) for i in range(4)]
        hT = [ph.sb("hT0", [128, 8, T], BF16)] * 2
        pre = ph.sb("pre", [128, 8, T + 3], F32)
        acc = [ph.sb(f"acc{i}", [128, T], F32) for i in range(2)]
        sq = [ph.sb(f"sq{i}", [128, T], BF16) for i in range(2)]
        rstd = [ph.sb(f"rstd{i}", [128, T], F32) for i in range(2)]
        gsb = [ph.sb(f"gsb{i}", [128, 64], F32) for i in range(2)]
        bg4 = ph.sb("bg4", [128, 32], F32)
        qk_st = ph.sb("qk_st", [128, 8, T], BF16)
        km_st = ph.sb("km_st", [128, 4, 512], BF16)
        vm_st = ph.sb("vm_st", [128, 4, 512], BF16)
        om_st = ph.sb("om_st", [128, 4, 512], BF16)
        vs_st = ph.sb("vs_st", [128, 4, 512], BF16)
        qs_st = ph.sb("qs_st", [128, 4, T], BF16)
        ks_st = ph.sb("ks_st", [128, 4, T], BF16)
        g_st = ph.sb("g_st", [128, 16, T], BF16)
        g12_st = ph.sb("g12_st", [128, 4, 12], F32)
        pf = ph.ps("pf", [128, 6, 512], F32)
        pb = ph.ps("pb", [128, 2, 1024], BF16)

        cst = make_consts(ph)
        negU, t_negU = tri_f32(ph, "negU", -1.0, "le")
        negO, t_negO = tri_f32(ph, "negO", -1.0, "all")
        bones = ph.sb("bones", [128, 128], BF16)
        t = ph.done("pool", POOL.memset(bones[:], 0.0))
        ph.wait("pool", t)
        POOL.memset(bones[0:64, 0:64], 1.0 / 64)
        t_bones = ph.done("pool", POOL.memset(bones[64:128, 64:128], 1.0 / 64))

        s_w = ph.sem("w")
        s_p = ph.sem("par")
        t_par = []
        t_par.append(ph.dma("sp", gmix[:], W["g_mix"][L:L + 1, :].partition_broadcast(128), s_p))
        t_par.append(ph.dma("sp", cw[:], W["cwl"][L], s_p))
        t_par.append(ph.dma("sp", cb[:], W["cbl"][L], s_p))
        t_par.append(ph.dma("sp", bg[:], W["b_gates"][L:L + 1, :].partition_broadcast(128), s_p))
        for b in range(4):
            t_par.append(ph.dma("sp", bg4[:, b * 8:(b + 1) * 8], W["b_gates"][L:L + 1, :].partition_broadcast(128), s_p))
        t_par.append(ph.dma("sp", gqk[:], W["gqk"][L], s_p))
        t_par = t_par[-1]
        t_w = None
        wv = W["w_in"][L].rearrange("(k p) n -> p k n", p=128)
        for k in range(8):
            t_w = ph.dma("pool", Win[:, k, :], wv[:, k, :], s_w)
        ph.wait("dve", t_par)
        t_gq = ph.done("dve", DVE.tensor_scalar_mul(out=gqk[:, 0:1], in0=gqk[:, 0:1], scalar1=0.125))

        s_x = [ph.sem("x0")] * 2
        st_sems = {k: ph.sem("st_" + k) for k in ("qk", "km", "vm", "om", "vs", "qs", "ks", "g", "g12")}
        st_tok = {k: None for k in st_sems}
        xt_free = [None]
        hT_free = [None, None]
        hbf_free = [None] * 4
        stat_free = [None, None]
        tp_free = None
        kmT_free = None
        mm_free = [None] * 4
        ss_box = [None]
        small_free = None
        acc_free = [None, None]
        sq_free = [None, None]
        rstd_free = [None, None]
        gsb_free = [None, None]
        pre_free = [None] * 8
        state = {"mm": 0, "acc": 0, "sq": 0, "gsb": 0}
        LOGS = -0.5 * math.log(128.0)

        def mm_bank():
            b = state["mm"] % 4
            state["mm"] += 1
            return b

        x_toks = {}
        h_toks = {}
        tpst = {"tp_free": None, 0: None, 1: None}

        def emit_xload(i):
            r0 = i * T
            ph.wait("sp", xt_free[0])
            x_toks[i] = ph.dma("sp", xt[0][:], x_in[r0:r0 + T, :].rearrange("(b p) d -> p b d", p=128), s_x[0])

        def emit_norm(i):
            slot = i % 2
            st = stat[slot]
            toks = []
            for b in range(4):
                ph.wait("act", x_toks[i], stat_free[slot] if b == 0 else None, hbf_free[b])
                t = ph.done("act", ACT.activation(out=hbf[b][:], in_=xt[0][:, b, :], func=AF.Square,
                                                  accum_out=st[:, b:b + 1]))
                ph.wait("act", t)
                t = ph.done("act", ACT.activation(out=st[:, 4 + b:5 + b], in_=st[:, b:b + 1], func=AF.Ln,
                                                  bias=EPS, scale=1.0 / D))
                ph.wait("act", t)
                t_r = ph.done("act", ACT.activation(out=st[:, 8 + b:9 + b], in_=st[:, 4 + b:5 + b], func=AF.Exp,
                                                    scale=-0.5))
                ph.wait("dve", t_r, t_par, hbf_free[b])
                t_h = ph.done("dve", DVE.scalar_tensor_tensor(out=hbf[b][:], in0=xt[0][:, b, :],
                                                              scalar=st[:, 8 + b:9 + b], in1=gmix[:],
                                                              op0=ALU.mult, op1=ALU.mult))
                toks.append(t_h)
            xt_free[0] = toks[-1]
            stat_free[slot] = toks[-1]
            h_toks[i] = toks

        def emit_transposes(i):
            slot = i % 2
            ready = []
            for b in range(4):
                tb = b % 2
                ph.wait("pe", h_toks[i][b], cst["t_identb"], tpst[tb])
                for c in range(8):
                    ins = PE.transpose(out=pb[:, tb, c * 128:(c + 1) * 128], in_=hbf[b][:, c * 128:(c + 1) * 128],
                                       identity=cst["identb"][:])
                t_tp = ph.done("pe", ins)
                hbf_free[b] = t_tp
                ph.wait("act", t_tp, hT_free[0] if b == 0 else None)
                t_e = ph.done("act", ACT.activation(out=hT[slot][:, :, b * 128:(b + 1) * 128],
                                                    in_=pb[:, tb, :].rearrange("p (c t) -> p c t", c=8),
                                                    func=AF.Copy))
                tpst[tb] = t_e
                ready.append(t_e)
            return ready

        emit_xload(0)
        emit_norm(0)
        if cfg.ntile > 1:
            emit_xload(1)
        hT_ready_next = emit_transposes(0)
        for i in range(cfg.ntile):
            slot = i % 2
            seq_start = (i % cfg.tps == 0)
            r0 = i * T
            hT_ready = hT_ready_next
            hTs = hT[slot]

            def fm_group(col0):
                bk = mm_bank()
                ph.wait("pe", hT_ready, t_w, mm_free[bk])
                for k in range(8):
                    ins = PE.matmul(pf[:, bk, :], lhsT=Win[:, k, col0:col0 + 128], rhs=hTs[:, k, :],
                                    start=(k == 0), stop=(k == 7))
                return bk, ph.done("pe", ins)

            def tm_group(b, col0, n):
                bk = mm_bank()
                ph.wait("pe", hT_ready, t_w, mm_free[bk])
                for k in range(8):
                    ins = PE.matmul(pf[:, bk, 0:n], lhsT=hTs[:, k, b * 128:(b + 1) * 128], rhs=Win[:, k, col0:col0 + n],
                                    start=(k == 0), stop=(k == 7))
                return bk, ph.done("pe", ins)

            if seq_start:
                ph.wait("dve", [pre_free[c] for c in range(8)])
                t_z = ph.done("dve", DVE.memset(pre[:, :, 0:3], 0.0))
            else:
                t_z = None
            si_toks = {}
            pend_si = []

            def flush_silu():
                c_, a2, ai2, t_acc2 = pend_si.pop(0)
                ph.wait("act", t_acc2, st_tok["qk"], st_tok["km"] if c_ >= 4 else None)
                t_si2 = ph.done("act", ACT.activation(out=qk_st[:, c_, :], in_=a2[:], func=AF.Silu, bias=cb[:, c_:c_ + 1]))
                acc_free[ai2] = t_si2
                si_toks[c_] = t_si2
                return t_si2

            for c in range(8):
                bk, t_m = fm_group(C_QK + c * 128)
                ph.wait("act", t_m, pre_free[c], t_z)
                t_cp = ph.done("act", ACT.activation(out=pre[:, c, 3:T + 3], in_=pf[:, bk, :], func=AF.Copy))
                mm_free[bk] = t_cp
                ai = state["acc"] % 2
                state["acc"] += 1
                a_ = acc[ai]
                en, EN = ("dve", DVE)
                ph.wait(en, t_cp, t_par, acc_free[ai], t_z)
                ph.wait("act", t_cp, t_par, acc_free[ai], t_z)
                t = ph.done("act", ACT.activation(out=a_[:], in_=pre[:, c, 0:T], func=AF.Copy, scale=cw[:, c * 4:c * 4 + 1]))
                if pend_si:
                    flush_silu()
                for j in range(1, 4):
                    ph.wait(en, t)
                    t = ph.done(en, EN.scalar_tensor_tensor(out=a_[:], in0=pre[:, c, j:j + T],
                                                            scalar=cw[:, c * 4 + j:c * 4 + j + 1], in1=a_[:],
                                                            op0=ALU.mult, op1=ALU.add))
                t_acc = t
                ph.wait(en, t_acc)
                t_halo = ph.done(en, EN.tensor_copy(out=pre[:, c, 0:3], in_=pre[:, c, T:T + 3]))
                pre_free[c] = t_halo
                pend_si.append((c, a_, ai, t_acc))
            t_si = flush_silu()
            ph.wait("sp", t_si)
            st_tok["qk"] = ph.dma("sp", scr["qkT"][:, :, r0:r0 + T].rearrange("c p t -> p c t"), qk_st[:], st_sems["qk"])
            ph.wait("pe", small_free, hT_ready, t_w)
            for b in range(4):
                for k in range(8):
                    ins = PE.matmul(pf[:, 5, b * 8:(b + 1) * 8], lhsT=hTs[:, k, b * 128:(b + 1) * 128], rhs=Win[:, k, C_G:C_G + 8],
                                    start=(k == 0), stop=(k == 7))
            t_g = ph.done("pe", ins)
            gi = state["gsb"] % 2
            state["gsb"] += 1
            gs = gsb[gi]
            gs3 = gs[:, 0:32].rearrange("p (b n) -> p b n", b=4)
            ph.wait("dve", t_g, t_par, gsb_free[gi])
            t = ph.done("dve", DVE.tensor_tensor(out=gs3, in0=pf[:, 5, 0:32].rearrange("p (b n) -> p b n", b=4),
                                                 in1=bg4[:].rearrange("p (b n) -> p b n", b=4), op=ALU.add))
            ph.wait("act", t)
            t = ph.done("act", ACT.activation(out=gs[:, 32:48].rearrange("p (b n) -> p b n", b=4), in_=gs3[:, :, 4:8], func=AF.Exp, scale=-1.0))
            ph.wait("act", t)
            t_sp = ph.done("act", ACT.activation(out=gs[:, 48:64], in_=gs[:, 32:48], func=AF.Ln, bias=1.0))
            for b in range(4):
                bk, t_m = tm_group(b, C_VM, 512)
                ph.wait("act", t_m, st_tok["vm"] if b == 0 else None)
                t_vm = ph.done("act", ACT.activation(out=vm_st[:, b, :], in_=pf[:, bk, :], func=AF.Copy))
                mm_free[bk] = t_vm
                bk, t_m = tm_group(b, C_VS, 512)
                ph.wait("act", t_m, st_tok["vs"] if b == 0 else None)
                t_vs = ph.done("act", ACT.activation(out=vs_st[:, b, :], in_=pf[:, bk, :], func=AF.Copy))
                mm_free[bk] = t_vs
            ph.wait("pe", t_sp, t_negU, t_negO)
            for b in range(4):
                PE.matmul(pf[:, 5, 32 + b * 8:36 + b * 8], lhsT=negU[:], rhs=gs[:, 48 + b * 4:52 + b * 4], start=True, stop=True)
                ins = PE.matmul(pf[:, 5, 36 + b * 8:40 + b * 8], lhsT=negO[:], rhs=gs[:, 48 + b * 4:52 + b * 4], start=True, stop=True)
            t_b = ph.done("pe", ins)
            bps3 = pf[:, 5, 32:64].rearrange("p (b n) -> p b n", b=4)
            ph.wait("act", t_b, st_tok["g12"])
            t_e1 = ph.done("act", ACT.activation(out=g12_st[:, :, 0:8], in_=bps3, func=AF.Exp))
            ph.wait("dve", t_b, t_e1)
            t = ph.done("dve", DVE.tensor_tensor(out=gs[:, 32:48].rearrange("p (b n) -> p b n", b=4), in0=gs3[:, :, 0:4],
                                                 in1=bps3[:, :, 0:4], op=ALU.subtract))
            ph.wait("act", t)
            t_e2 = ph.done("act", ACT.activation(out=g12_st[:, :, 8:12], in_=gs[:, 32:48].rearrange("p (b n) -> p b n", b=4),
                                                 func=AF.Exp, bias=LOGS))
            small_free = [t_e1, t]
            gsb_free[gi] = t_e2
            g12_last = t_e2
            ph.wait("sp", g12_last)
            st_tok["g12"] = ph.dma("sp", scr["g12"][r0:r0 + T, :].rearrange("(b p) n -> p b n", p=128), g12_st[:], st_sems["g12"])
            ph.wait("sp", t_vm)
            st_tok["vm"] = ph.dma("sp", scr["vm"][r0:r0 + T, :].rearrange("(b p) n -> p b n", p=128), vm_st[:], st_sems["vm"])
            ph.wait("sp", t_vs)
            st_tok["vs"] = ph.dma("sp", scr["vs"][r0:r0 + T, :].rearrange("(b p) n -> p b n", p=128), vs_st[:], st_sems["vs"])

            for c in range(4, 8):
                h = c - 4
                kb_ = h % 2
                ph.wait("pe", si_toks[c], tpst[kb_])
                for b in range(4):
                    ins = PE.transpose(out=pb[:, kb_, b * 128:(b + 1) * 128], in_=qk_st[:, c, b * 128:(b + 1) * 128],
                                       identity=cst["identb"][:])
                t_t = ph.done("pe", ins)
                ph.wait("act", t_t, st_tok["km"])
                t_k = ph.done("act", ACT.activation(out=km_st[:, :, h * 128:(h + 1) * 128],
                                                    in_=pb[:, kb_, 0:512].rearrange("p (b d) -> p b d", b=4), func=AF.Copy))
                tpst[kb_] = t_k
            ph.wait("sp", t_k)
            st_tok["km"] = ph.dma("sp", scr["km"][r0:r0 + T, :].rearrange("(b p) n -> p b n", p=128), km_st[:], st_sems["km"])

            qjobs = [(col, stg, key, gcol, cc) for (col, stg, key, gcol) in ((C_QS, qs_st, "qs", 0), (C_KS, ks_st, "ks", 1))
                     for cc in range(4)]
            pend = None

            def qk_tail(job):
                (col, stg, key, gcol, cc), bk, si, t_sq = job
                nonlocal_ss = ss_box
                ph.wait("pe", t_sq, t_bones, nonlocal_ss[0])
                t_ss = ph.done("pe", PE.matmul(pf[:, 4, :], lhsT=bones[:], rhs=sq[si][:], start=True, stop=True))
                sq_free[si] = t_ss
                ph.wait("act", t_ss, rstd_free[si])
                t_ln = ph.done("act", ACT.activation(out=rstd[si][:], in_=pf[:, 4, :], func=AF.Ln, bias=EPS))
                nonlocal_ss[0] = t_ln
                ph.wait("act", t_ln)
                t_rs = ph.done("act", ACT.activation(out=rstd[si][:], in_=rstd[si][:], func=AF.Exp, scale=-0.5))
                ph.wait("dve", t_rs, t_gq, st_tok[key] if cc == 0 else None)
                t_o = ph.done("dve", DVE.scalar_tensor_tensor(out=stg[:, cc, :], in0=pf[:, bk, :],
                                                              scalar=gqk[:, gcol:gcol + 1], in1=rstd[si][:],
                                                              op0=ALU.mult, op1=ALU.mult))
                mm_free[bk] = t_o
                rstd_free[si] = t_o
                if cc == 3:
                    ph.wait("sp", t_o)
                    st_tok[key] = ph.dma("sp", scr[key + "T"][:, :, r0:r0 + T].rearrange("c p t -> p c t"), stg[:], st_sems[key])

            for job in qjobs:
                col, stg, key, gcol, cc = job
                bk, t_m = fm_group(col + cc * 128)
                si = state["sq"] % 2
                state["sq"] += 1
                ph.wait("act", t_m, sq_free[si])
                t_sq = ph.done("act", ACT.activation(out=sq[si][:], in_=pf[:, bk, :], func=AF.Square))
                if pend is not None:
                    qk_tail(pend)
                pend = (job, bk, si, t_sq)
            qk_tail(pend)

            if i + 1 < cfg.ntile:
                emit_norm(i + 1)
                if i + 2 < cfg.ntile:
                    emit_xload(i + 2)
            for c in range(16):
                bk, t_m = fm_group(C_GP + c * 128)
                ph.wait("act", t_m, st_tok["g"] if c == 0 else None)
                t_o = ph.done("act", ACT.activation(out=g_st[:, c, :], in_=pf[:, bk, :], func=AF.Sigmoid))
                mm_free[bk] = t_o
            ph.wait("sp", t_o)
            st_tok["g"] = ph.dma("sp", scr["gT"][:, :, r0:r0 + T].rearrange("c p t -> p c t"), g_st[:], st_sems["g"])
            for b in range(4):
                bk, t_m = tm_group(b, C_OM, 512)
                ph.wait("act", t_m, st_tok["om"] if b == 0 else None)
                t_o = ph.done("act", ACT.activation(out=om_st[:, b, :], in_=pf[:, bk, :], func=AF.Sigmoid))
                mm_free[bk] = t_o
            ph.wait("sp", t_o)
            st_tok["om"] = ph.dma("sp", scr["om"][r0:r0 + T, :].rearrange("(b p) n -> p b n", p=128), om_st[:], st_sems["om"])

            hT_free[0] = t_m
            if i + 1 < cfg.ntile:
                hT_ready_next = emit_transposes(i + 1)
        ph.final = [v for v in st_tok.values() if v is not None]


def scratch_specs(cfg):
    n = cfg.ntok
    return {
        "qkT": ([8, 128, n], BF16), "km": ([n, 512], BF16), "vm": ([n, 512], BF16), "om": ([n, 512], BF16),
        "g12": ([n, 12], F32), "qsT": ([4, 128, n], BF16), "ksT": ([4, 128, n], BF16), "vs": ([n, 512], BF16),
        "gT": ([16, 128, n], BF16), "yaT": ([4, 128, n], BF16), "ybT": ([4, 128, n], BF16),
        "x1": ([n, D], F32), "xmid": ([n, D], F32),
    }


WEIGHT_SPECS = {
    "g_mix": [DEPTH, D], "w_in": [DEPTH, D, IN_COLS], "cwl": [DEPTH, 128, 32], "cbl": [DEPTH, 128, 8],
    "b_gates": [DEPTH, 8], "gqk": [DEPTH, 128, 2], "w_br_a": [DEPTH, 512, D], "w_br_b": [DEPTH, 512, D],
    "w_out": [DEPTH, D, D], "g_ffn": [DEPTH, D], "w_gu": [DEPTH, D, 2 * D_FF], "w_down": [DEPTH, D_FF, D],
}


def build_program(cfg, plan, ext_in=(), ext_out=()):
    nc = bass.Bass("TRN2", target_bir_lowering=False)
    W = {k: nc.dram_tensor(k, s, F32, kind="ExternalInput").ap() for k, s in WEIGHT_SPECS.items()}
    tens = {}
    tens["x"] = nc.dram_tensor("x", [cfg.ntok, D], F32, kind="ExternalInput").ap()
    tens["out"] = nc.dram_tensor("out", [cfg.ntok, D], F32, kind="ExternalOutput").ap()
    for k, (s, dt) in scratch_specs(cfg).items():
        kind = "ExternalInput" if k in ext_in else ("ExternalOutput" if k in ext_out else "Internal")
        tens[k] = nc.dram_tensor(k, s, dt, kind=kind).ap()
    for (pname, L, xin, xout) in plan:
        PHASES[pname](nc, cfg, L, tens.get(xin), W, tens, tens.get(xout))
    return nc


def host_layout_weights(inp):
    f = lambda a: np.ascontiguousarray(np.asarray(a, dtype=np.float32))
    cw = f(inp["conv_w"])
    cwl = f(cw.reshape(DEPTH, 4, 8, 128).transpose(0, 3, 2, 1).reshape(DEPTH, 128, 32))
    cbl = f(f(inp["conv_b"]).reshape(DEPTH, 8, 128).transpose(0, 2, 1))
    gq = f(inp["g_q"])
    gk = f(inp["g_k"])
    gqk = f(np.stack([np.concatenate([gq, gq], 1), np.concatenate([gk, gk], 1)], axis=2))
    return {
        "g_mix": f(inp["g_mix"]), "w_in": f(inp["w_in"]), "cwl": cwl, "cbl": cbl, "b_gates": f(inp["b_gates"]),
        "gqk": gqk, "w_br_a": f(inp["w_br_a"]), "w_br_b": f(inp["w_br_b"]), "w_out": f(inp["w_out"]),
        "g_ffn": f(inp["g_ffn"]), "w_gu": f(inp["w_gu"]), "w_down": f(inp["w_down"]),
    }


PHASES = {"p1": lambda nc, cfg, L, xin, W, tens, xout: phase1(nc, cfg, L, xin, W, tens)}


def phase2(nc, cfg, L, scr):
    with Phase(nc, f"p2l{L}") as ph:
        PE, ACT, DVE, POOL, SP = (ph.engs[k] for k in ("pe", "act", "dve", "pool", "sp"))
        NS = cfg.nseq
        qk_sb = [[ph.sb(f"qk{s}{p}", [128, 8, T], BF16) for p in range(2)] for s in range(NS)]
        km_sb = [[ph.sb(f"km{s}{p}", [128, 4, 512], BF16) for p in range(2)] for s in range(NS)]
        va_sb = [[ph.sb(f"va{s}{p}", [128, 4, 4, 129], BF16) for p in range(2)] for s in range(NS)]
        om_sb = [[ph.sb(f"om{s}{p}", [128, 4, 512], BF16) for p in range(2)] for s in range(NS)]
        g_sb = [[ph.sb(f"g{s}{p}", [128, 4, 12], F32) for p in range(2)] for s in range(NS)]
        C32 = [ph.sb(f"C32_{s}", [128, 4, 129], F32) for s in range(NS)]
        Cbf = [ph.sb(f"Cbf_{s}", [128, 4, 129], BF16) for s in range(NS)]
        ya_sb = [ph.sb(f"ya{i}", [128, 512], BF16) for i in range(2)]
        yaT_st = [ph.sb(f"yaT{s}", [128, 4, T], BF16) for s in range(NS)]
        NR = 4
        sT_sb = [ph.sb(f"sT{i}", [128, 128], BF16) for i in range(NR)]
        kw_sb = [ph.sb(f"kw{i}", [128, 128], BF16) for i in range(NR)]
        wk2 = [ph.sb(f"wk2_{i}", [128, 4], F32) for i in range(NR)]
        dtmp = [ph.sb(f"dtmp{i}", [128, 4], F32) for i in range(NR)]
        pf = ph.ps("pf", [128, 6, 512], F32)
        pb = ph.ps("pb", [128, 2, 1024], BF16)
        cst = make_consts(ph)
        maskLE, t_mask = tri_f32(ph, "maskLE", 1.0, "le")
        t_init = []
        for s in range(NS):
            t_init.append(ph.done("pool", POOL.memset(C32[s][:], 0.0)))
            t_init.append(ph.done("pool", POOL.memset(Cbf[s][:], 0.0)))
            for p in range(2):
                t_init.append(ph.done("pool", POOL.memset(va_sb[s][p][:, :, :, 128:129], 1.0)))
        t_init = t_init[-1]

        s_ld = [[ph.sem(f"ld{s}{p}") for p in range(2)] for s in range(NS)]
        s_st = [ph.sem(f"st{s}") for s in range(NS)]
        st_tok = [None] * NS
        slot_free = [[[] for p in range(2)] for s in range(NS)]
        C_tok = [[t_init] * 4 for s in range(NS)]
        Cbf_tok = [[t_init] * 4 for s in range(NS)]
        Cbf_read = [[None] * 4 for s in range(NS)]
        st_free = [None, None]
        acc_free = [None, None]
        cps_free = [None, None]
        sT_free = [None] * NR
        kw_free = [None] * NR
        wk2_free = [None] * NR
        dtmp_free = [None] * NR
        ya_free = [None, None]
        pb_free = [None, None]
        cnt = {"u": 0, "c": 0}

        ld_toks = {}

        def emit_loads(t):
            par = t % 2
            for s in range(NS):
                r0 = s * cfg.S + t * T
                ph.wait("sp", slot_free[s][par])
                sem = s_ld[s][par]
                ph.dma("sp", qk_sb[s][par][:], scr["qkT"][:, :, r0:r0 + T].rearrange("c p t -> p c t"), sem)
                ph.dma("sp", km_sb[s][par][:], scr["km"][r0:r0 + T, :].rearrange("(b p) n -> p b n", p=128), sem)
                for b in range(4):
                    ph.dma("sp", va_sb[s][par][:, b, :, 0:128],
                           scr["vm"][r0 + b * 128:r0 + (b + 1) * 128, :].rearrange("p (h e) -> p h e", h=4), sem)
                ph.dma("sp", om_sb[s][par][:], scr["om"][r0:r0 + T, :].rearrange("(b p) n -> p b n", p=128), sem)
                ld_toks[(t, s)] = ph.dma("sp", g_sb[s][par][:], scr["g12"][r0:r0 + T, :].rearrange("(b p) n -> p b n", p=128), sem)

        emit_loads(0)
        for t in range(cfg.tps):
            par = t % 2
            if t + 1 < cfg.tps:
                emit_loads(t + 1)
            ld_tok = [ld_toks[(t, s)] for s in range(NS)]
            last = [None] * NS
            for j in range(4):
                jr = slice(j * 128, (j + 1) * 128)
                for s in range(NS):
                    qk, km, va, om, g = qk_sb[s][par], km_sb[s][par], va_sb[s][par], om_sb[s][par], g_sb[s][par]
                    ci = cnt["c"] % NR
                    cnt["c"] += 1
                    ph.wait("dve", ld_tok[s], wk2_free[ci])
                    t_wk2 = ph.done("dve", DVE.tensor_tensor(out=wk2[ci][:], in0=g[:, j, 8:12], in1=g[:, j, 4:8], op=ALU.mult))
                    yi = cnt["c"] % 2
                    y_toks = []
                    hs = [dict() for _ in range(4)]

                    def partA(h):
                        H = hs[h]
                        u = cnt["u"]
                        cnt["u"] += 1
                        H["r"] = r = u % NR
                        H["b2"] = b2 = u % 2
                        ph.wait("pe", ld_tok[s], st_free[b2])
                        t_st = ph.done("pe", PE.matmul(pf[:, b2, 0:128], lhsT=qk[:, 4 + h, jr], rhs=qk[:, h, jr], start=True, stop=True))
                        ph.wait("dve", t_st, t_mask, sT_free[r])
                        H["t_sT"] = ph.done("dve", DVE.scalar_tensor_tensor(out=sT_sb[r][:], in0=pf[:, b2, 0:128], scalar=g[:, j, 8 + h:9 + h],
                                                                            in1=maskLE[:], op0=ALU.mult, op1=ALU.mult))
                        st_free[b2] = H["t_sT"]
                        ph.wait("act", ld_tok[s], t_wk2, kw_free[r])
                        H["t_kw"] = ph.done("act", ACT.activation(out=kw_sb[r][:], in_=km[:, j, h * 128:(h + 1) * 128], func=AF.Copy,
                                                                  scale=wk2[ci][:, h:h + 1]))

                    def partPE(h):
                        H = hs[h]
                        r, b2 = H["r"], H["b2"]
                        ph.wait("pe", Cbf_tok[s][h], H["t_sT"], acc_free[b2])
                        PE.matmul(pf[:, 2 + b2, 0:129], lhsT=qk[:, h, jr], rhs=Cbf[s][:, h, :], start=True, stop=False)
                        H["t_acc"] = ph.done("pe", PE.matmul(pf[:, 2 + b2, 0:129], lhsT=sT_sb[r][:], rhs=va[:, j, h, :], start=False, stop=True))
                        sT_free[r] = H["t_acc"]
                        ph.wait("pe", H["t_kw"], cps_free[b2])
                        H["t_cps"] = ph.done("pe", PE.matmul(pf[:, 4 + b2, 0:129], lhsT=kw_sb[r][:], rhs=va[:, j, h, :], start=True, stop=True))
                        kw_free[r] = H["t_cps"]

                    def partB(h):
                        H = hs[h]
                        r, b2 = H["r"], H["b2"]
                        t_acc, t_cps = H["t_acc"], H["t_cps"]
                        ph.wait("dve", t_cps, C_tok[s][h], Cbf_tok[s][h])
                        t_c = ph.done("dve", DVE.scalar_tensor_tensor(out=C32[s][:, h, :], in0=C32[s][:, h, :], scalar=g[:, j, 4 + h:5 + h],
                                                                      in1=pf[:, 4 + b2, 0:129], op0=ALU.mult, op1=ALU.add))
                        C_tok[s][h] = t_c
                        cps_free[b2] = t_c
                        ph.wait("act", t_c, t_acc)
                        Cbf_tok[s][h] = ph.done("act", ACT.activation(out=Cbf[s][:, h, :], in_=C32[s][:, h, :], func=AF.Copy))
                        d = dtmp[r]
                        ph.wait("dve", t_acc, dtmp_free[r])
                        t1 = ph.done("dve", DVE.tensor_tensor(out=d[:, 0:1], in0=pf[:, 2 + b2, 128:129], in1=g[:, j, h:h + 1], op=ALU.mult))
                        ph.wait("dve", t1)
                        t1 = ph.done("dve", DVE.scalar_tensor_tensor(out=d[:, 1:2], in0=d[:, 0:1], scalar=-1.0, in1=d[:, 0:1],
                                                                     op0=ALU.mult, op1=ALU.max))
                        ph.wait("dve", t1)
                        t1 = ph.done("dve", DVE.tensor_scalar_max(out=d[:, 1:2], in0=d[:, 1:2], scalar1=1.0))
                        ph.wait("dve", t1)
                        t1 = ph.done("dve", DVE.reciprocal(out=d[:, 2:3], in_=d[:, 1:2]))
                        ph.wait("dve", t1)
                        t1 = ph.done("dve", DVE.tensor_tensor(out=d[:, 3:4], in0=d[:, 2:3], in1=g[:, j, h:h + 1], op=ALU.mult))
                        ph.wait("dve", t1, ya_free[yi] if h == 0 else None)
                        t_y = ph.done("dve", DVE.scalar_tensor_tensor(out=ya_sb[yi][:, h * 128:(h + 1) * 128], in0=pf[:, 2 + b2, 0:128],
                                                                      scalar=d[:, 3:4], in1=om[:, j, h * 128:(h + 1) * 128],
                                                                      op0=ALU.mult, op1=ALU.mult))
                        acc_free[b2] = t_y
                        dtmp_free[r] = t_y
                        y_toks.append(t_y)
                        last[s] = [t_y, t_cps, t_acc, H["t_kw"]]

                    for h in range(4):
                        partA(h)
                    t_kw = hs[3]["t_kw"]
                    for h0 in (0, 2):
                        partPE(h0)
                        partPE(h0 + 1)
                        partB(h0)
                        partB(h0 + 1)
                    wk2_free[ci] = t_kw
                    tb = cnt["c"] % 2
                    ph.wait("pe", y_toks, cst["t_identb"], pb_free[tb])
                    for h in range(4):
                        ins = PE.transpose(out=pb[:, tb, h * 128:(h + 1) * 128], in_=ya_sb[yi][:, h * 128:(h + 1) * 128],
                                           identity=cst["identb"][:])
                    t_tp = ph.done("pe", ins)
                    ya_free[yi] = t_tp
                    ph.wait("act", t_tp, st_tok[s] if j == 0 else None)
                    t_ev = ph.done("act", ACT.activation(out=yaT_st[s][:, :, jr], in_=pb[:, tb, 0:512].rearrange("p (h l) -> p h l", h=4),
                                                         func=AF.Copy))
                    pb_free[tb] = t_ev
                    last[s].append(t_ev)
            for s in range(NS):
                r0 = s * cfg.S + t * T
                ph.wait("act", last[s][-1])
                st_tok[s] = ph.dma("act", scr["yaT"][:, :, r0:r0 + T].rearrange("c p t -> p c t"), yaT_st[s][:], s_st[s])
                slot_free[s][par] = list(last[s])
        ph.final = [x for x in st_tok if x is not None]


PHASES["p2"] = lambda nc, cfg, L, xin, W, tens, xout: phase2(nc, cfg, L, tens)


def phase3(nc, cfg, L, scr):
    with Phase(nc, f"p3l{L}") as ph:
        PE, ACT, DVE, POOL, SP = (ph.engs[k] for k in ("pe", "act", "dve", "pool", "sp"))
        S = cfg.S
        NB = S // 128
        NQT = S // T
        kT = ph.sb("kT", [128, 4, S], BF16)
        qT = ph.sb("qT", [128, 4, S], BF16)
        vv = ph.sb("vv", [128, NB, 512], BF16)
        e_sb = [ph.sb(f"e{i}", [128, 2, T], F32) for i in range(2)]
        sp_sb = [ph.sb(f"sp{i}", [128, 2, T], BF16) for i in range(2)]
        Ss = [ph.sb(f"Ss{i}", [128, T], BF16) for i in range(3)]
        Stmp = ph.sb("Stmp", [128, T], BF16)
        aT_sb = [ph.sb(f"aT{i}", [128, 2, T], BF16) for i in range(2)]
        yb_st = [ph.sb(f"yb{i}", [64, T], BF16) for i in range(2)]
        pf = ph.ps("pf", [128, 8, 512], F32)
        cst = make_consts(ph)
        identb = cst["identb"]
        negTri, t_tri = tri_bf16(ph, "negTri", -1.0, "ge")
        negOne, t_one = tri_bf16(ph, "negOne", -1.0, "all")
        nm_f = ph.sb("nm_f", [128, T], F32)
        negmask = []
        t_nm = None
        for i in range(4):
            m = ph.sb(f"negmask{i}", [128, T], BF16)
            ph.wait("pool", t_nm)
            t = ph.done("pool", POOL.memset(nm_f[:], NEG))
            ph.wait("pool", t)
            t = ph.done("pool", POOL.affine_select(out=nm_f[:], in_=nm_f[:], pattern=[[-1, T]], compare_op=ALU.is_ge,
                                                   fill=0.0, base=i * 128, channel_multiplier=1))
            ph.wait("pool", t)
            t_nm = ph.done("pool", POOL.tensor_copy(out=m[:], in_=nm_f[:]))
            negmask.append(m)
        t_consts = [cst["t_identb"], t_tri, t_one, t_nm]

        s_ld = ph.sem("ld")
        s_yb = [ph.sem("yb0"), ph.sem("yb1")]
        zfree = [None, None]
        spfree = [None, None]
        Ssfree = [None, None, None]
        Stmp_free = [None]
        Afree = [None]
        aTfree = [None, None]
        ofree = [None, None]
        ybfree = [None, None]
        prev_done = []
        gk = {"k": 0, "grp": 0}

        for s in range(cfg.nseq):
            c0 = s * S
            ph.wait("sp", prev_done)
            ph.dma("sp", kT[:], scr["ksT"][:, :, c0:c0 + S].rearrange("c p t -> p c t"), s_ld)
            ph.dma("sp", qT[:], scr["qsT"][:, :, c0:c0 + S].rearrange("c p t -> p c t"), s_ld)
            ld_tok = ph.dma("sp", vv[:], scr["vs"][c0:c0 + S, :].rearrange("(b p) n -> p b n", p=128), s_ld)
            units = []
            for h in range(SB_H):
                for qt in range(NQT):
                    npair = 2 * (qt + 1)
                    for m in range(npair):
                        kb = 4 * qt + 3 - 2 * m
                        units.append(dict(h=h, qt=qt, m=m, kb=kb, last=(m == npair - 1), diag=(m < 2),
                                          i=kb - 4 * qt, grp=gk["grp"]))
                    gk["grp"] += 1
            NU = len(units)
            k0 = gk["k"]

            def operands(U, j):
                hc = U["h"] // 2
                p0 = (U["h"] % 2) * 64
                kb = U["kb"] - j
                lk = kT[p0:p0 + 64, hc, kb * 128:(kb + 1) * 128]
                rq = qT[p0:p0 + 64, hc, U["qt"] * T:(U["qt"] + 1) * T]
                return lk, rq

            def stage0(U, k):
                b = k % 2
                ph.wait("pe", ld_tok, t_consts, zfree[b])
                for j in range(2):
                    lk, rq = operands(U, j)
                    ins = PE.matmul(pf[:, 2 * b + j, :], lhsT=lk, rhs=rq, start=True, stop=not U["diag"])
                    if U["diag"]:
                        ins = PE.matmul(pf[:, 2 * b + j, :], lhsT=identb[:], rhs=negmask[U["i"] - j][:], start=False, stop=True)
                U["t_z"] = ph.done("pe", ins)
                ph.wait("act", U["t_z"])
                U["t_e"] = ph.done("act", ACT.activation(out=e_sb[b][:], in_=pf[:, 2 * b:2 * b + 2, :], func=AF.Exp))
                zfree[b] = U["t_e"]
                ph.wait("act", U["t_e"], spfree[b])
                U["t_sp"] = ph.done("act", ACT.activation(out=sp_sb[b][:], in_=e_sb[b][:], func=AF.Ln, bias=1.0))
                U["t_ss"] = None
                if not U["last"]:
                    m = U["m"]
                    dst = Ss[(k + 1) % 3]
                    ph.wait("dve", U["t_sp"], Ssfree[(k + 1) % 3])
                    if m == 0:
                        U["t_ss"] = ph.done("dve", DVE.tensor_tensor(out=dst[:], in0=sp_sb[b][:, 0, :], in1=sp_sb[b][:, 1, :], op=ALU.add))
                    else:
                        ph.wait("dve", U["t_ssprev"], Stmp_free[0])
                        t = ph.done("dve", DVE.tensor_tensor(out=Stmp[:], in0=Ss[k % 3][:], in1=sp_sb[b][:, 0, :], op=ALU.add))
                        ph.wait("dve", t)
                        U["t_ss"] = ph.done("dve", DVE.tensor_tensor(out=dst[:], in0=Stmp[:], in1=sp_sb[b][:, 1, :], op=ALU.add))
                        Stmp_free[0] = U["t_ss"]

            def stage1(U, k):
                b = k % 2
                m = U["m"]
                ph.wait("pe", U["t_sp"], Afree[0], U.get("t_ssprev"))
                for j in range(2):
                    lk, rq = operands(U, j)
                    mms = [(lk, rq), (negTri[:], sp_sb[b][:, j, :])]
                    if j == 1:
                        mms.append((negOne[:], sp_sb[b][:, 0, :]))
                    if m > 0:
                        mms.append((negOne[:], Ss[k % 3][:]))
                    if U["diag"]:
                        mms.append((identb[:], negmask[U["i"] - j][:]))
                    for jj, (l_, r_) in enumerate(mms):
                        ins = PE.matmul(pf[:, 4 + j, :], lhsT=l_, rhs=r_, start=(jj == 0), stop=(jj == len(mms) - 1))
                U["t_A"] = ph.done("pe", ins)
                if m > 0:
                    Ssfree[k % 3] = U["t_A"]
                spfree[b] = [U["t_A"], U["t_ss"]]
                ph.wait("act", U["t_A"], aTfree[b])
                U["t_a"] = ph.done("act", ACT.activation(out=aT_sb[b][:], in_=pf[:, 4:6, :], func=AF.Exp))
                Afree[0] = U["t_a"]

            def stage2(U, k):
                b = k % 2
                g2 = U["grp"] % 2
                h = U["h"]
                ph.wait("pe", U["t_a"], ofree[g2] if U["m"] == 0 else None)
                for j in range(2):
                    ins = PE.matmul(pf[0:64, 6 + g2, :], lhsT=vv[:, U["kb"] - j, h * 64:(h + 1) * 64], rhs=aT_sb[b][:, j, :],
                                    start=(U["m"] == 0 and j == 0), stop=(U["last"] and j == 1))
                U["t_av"] = ph.done("pe", ins)
                aTfree[b] = U["t_av"]
                if U["last"]:
                    ph.wait("dve", U["t_av"], ybfree[g2])
                    t_ev = ph.done("dve", DVE.tensor_copy(out=yb_st[g2][:], in_=pf[0:64, 6 + g2, :]))
                    ofree[g2] = t_ev
                    r0 = c0 + U["qt"] * T
                    p0 = (h % 2) * 64
                    ph.wait("sp", t_ev)
                    ybfree[g2] = ph.dma("sp", scr["ybT"][h // 2, p0:p0 + 64, r0:r0 + T], yb_st[g2][:], s_yb[g2])
                    U["t_ev"] = t_ev

            for step in range(NU + 2):
                if step < NU:
                    U = units[step]
                    if U["m"] > 0:
                        U["t_ssprev"] = units[step - 1]["t_ss"]
                    stage0(U, k0 + step)
                if 0 <= step - 1 < NU:
                    stage1(units[step - 1], k0 + step - 1)
                if 0 <= step - 2 < NU:
                    stage2(units[step - 2], k0 + step - 2)
            gk["k"] = k0 + NU
            lastU = units[-1]
            prev_done = [lastU["t_av"], lastU["t_a"], lastU["t_ev"]]
        ph.final = [x for x in ybfree if x is not None]


PHASES["p3"] = lambda nc, cfg, L, xin, W, tens, xout: phase3(nc, cfg, L, tens)


def phase4(nc, cfg, L, x_in, W, scr):
    with Phase(nc, f"p4l{L}") as ph:
        PE, ACT, DVE, POOL, SP = (ph.engs[k] for k in ("pe", "act", "dve", "pool", "sp"))
        Wa = ph.sb("wa", [128, 4, D], BF16)
        Wb = ph.sb("wb", [128, 4, D], BF16)
        Wo = ph.sb("wo", [128, 8, D], BF16)
        ya = [ph.sb(f"ya{i}", [128, 4, T], BF16) for i in range(2)]
        yb = [ph.sb(f"yb{i}", [128, 4, T], BF16) for i in range(2)]
        gT = [ph.sb(f"gT{i}", [128, 16, T], BF16) for i in range(2)]
        xs = [ph.sb(f"xs{i}", [128, 4, D], F32) for i in range(2)]
        mixT = ph.sb("mixT", [128, 8, T], BF16)
        tmpa = [ph.sb(f"tmpa{i}", [128, T], F32) for i in range(2)]
        tmpb = [ph.sb(f"tmpb{i}", [128, T], F32) for i in range(2)]
        pf = ph.ps("pf", [128, 8, 512], F32)
        s_w = ph.sem("w")
        ph.dma("pool", Wa[:], W["w_br_a"][L].rearrange("(k p) n -> p k n", p=128), s_w)
        ph.dma("pool", Wb[:], W["w_br_b"][L].rearrange("(k p) n -> p k n", p=128), s_w)
        wo_v = W["w_out"][L].rearrange("(k p) n -> p k n", p=128)
        ph.dma("pool", Wo[:, 0:4, :], wo_v[:, 0:4, :], s_w)
        t_w = ph.dma("pool", Wo[:, 4:8, :], wo_v[:, 4:8, :], s_w)
        s_ld = [ph.sem("ld0"), ph.sem("ld1")]
        s_st = [ph.sem("st0"), ph.sem("st1")]
        slot_free = [[], []]
        st_tok = [None, None]
        ld_toks = {}

        def emit_loads(i):
            p = i % 2
            r0 = i * T
            ph.wait("sp", slot_free[p], st_tok[p])
            ph.dma("sp", ya[p][:], scr["yaT"][:, :, r0:r0 + T].rearrange("c p t -> p c t"), s_ld[p])
            ph.dma("sp", yb[p][:], scr["ybT"][:, :, r0:r0 + T].rearrange("c p t -> p c t"), s_ld[p])
            ph.dma("sp", gT[p][:, 0:8, :], scr["gT"][0:8, :, r0:r0 + T].rearrange("c p t -> p c t"), s_ld[p])
            ph.dma("sp", gT[p][:, 8:16, :], scr["gT"][8:16, :, r0:r0 + T].rearrange("c p t -> p c t"), s_ld[p])
            ld_toks[i] = ph.dma("sp", xs[p][:], x_in[r0:r0 + T, :].rearrange("(b p) d -> p b d", p=128), s_ld[p])

        bank_free = [None] * 8
        tmpa_free = [None, None]
        tmpb_free = [None, None]
        mix_free = None
        emit_loads(0)
        cnt = 0
        for i in range(cfg.ntile):
            p = i % 2
            if i + 1 < cfg.ntile:
                emit_loads(i + 1)
            ld = ld_toks[i]
            mix_toks = []
            for c in range(8):
                ba, bb = (2 * c) % 4, (2 * c + 1) % 4
                ph.wait("pe", ld, t_w, bank_free[ba])
                for k in range(4):
                    ins = PE.matmul(pf[:, ba, :], lhsT=Wa[:, k, c * 128:(c + 1) * 128], rhs=ya[p][:, k, :], start=(k == 0), stop=(k == 3))
                t_pa = ph.done("pe", ins)
                ph.wait("pe", bank_free[bb])
                for k in range(4):
                    ins = PE.matmul(pf[:, bb, :], lhsT=Wb[:, k, c * 128:(c + 1) * 128], rhs=yb[p][:, k, :], start=(k == 0), stop=(k == 3))
                t_pb = ph.done("pe", ins)
                ti = c % 2
                ph.wait("dve", t_pa, ld, tmpa_free[ti])
                t_a = ph.done("dve", DVE.tensor_tensor(out=tmpa[ti][:], in0=pf[:, ba, :], in1=gT[p][:, c, :], op=ALU.mult))
                bank_free[ba] = t_a
                ph.wait("dve", t_pb, tmpb_free[ti])
                t_b = ph.done("dve", DVE.tensor_tensor(out=tmpb[ti][:], in0=pf[:, bb, :], in1=gT[p][:, 8 + c, :], op=ALU.mult))
                bank_free[bb] = t_b
                ph.wait("pool", t_a, t_b, mix_free if c == 0 else None)
                t_m = ph.done("pool", POOL.tensor_tensor(out=mixT[:, c, :], in0=tmpa[ti][:], in1=tmpb[ti][:], op=ALU.add))
                tmpa_free[ti] = t_m
                tmpb_free[ti] = t_m
                mix_toks.append(t_m)
            for b in range(4):
                for hf in range(2):
                    bk = 4 + (cnt % 4)
                    cnt += 1
                    ph.wait("pe", mix_toks, bank_free[bk])
                    for k in range(8):
                        ins = PE.matmul(pf[:, bk, :], lhsT=mixT[:, k, b * 128:(b + 1) * 128], rhs=Wo[:, k, hf * 512:(hf + 1) * 512],
                                        start=(k == 0), stop=(k == 7))
                    t_po = ph.done("pe", ins)
                    ph.wait("dve", t_po, ld)
                    t_x = ph.done("dve", DVE.tensor_tensor(out=xs[p][:, b, hf * 512:(hf + 1) * 512], in0=pf[:, bk, :],
                                                           in1=xs[p][:, b, hf * 512:(hf + 1) * 512], op=ALU.add))
                    bank_free[bk] = t_x
            mix_free = t_po
            r0 = i * T
            ph.wait("act", t_x)
            st_tok[p] = ph.dma("act", scr["x1"][r0:r0 + T, :].rearrange("(b p) d -> p b d", p=128), xs[p][:], s_st[p])
            slot_free[p] = [t_po, t_x, t_m]
        ph.final = [x for x in st_tok if x is not None]


def phase5(nc, cfg, L, W, scr, x_out):
    with Phase(nc, f"p5l{L}") as ph:
        PE, ACT, DVE, POOL, SP = (ph.engs[k] for k in ("pe", "act", "dve", "pool", "sp"))
        NC_FF = D_FF // 128
        Wgu = ph.sb("wgu", [128, 8, 2 * D_FF], BF16)
        Wd = ph.sb("wd", [128, NC_FF, D], BF16)
        gf = ph.sb("gf", [128, D], F32)
        xs = [ph.sb(f"xs{i}", [128, 4, D], F32) for i in range(2)]
        stat = [ph.sb(f"stat{i}", [128, 16], F32) for i in range(2)]
        hbf = [ph.sb(f"hbf{i}", [128, D], BF16) for i in range(4)]
        hT = ph.sb("hT", [128, 8, T], BF16)
        act = ph.sb("act", [128, NC_FF, T], BF16)
        pf = ph.ps("pf", [128, 6, 512], F32)
        pb = ph.ps("pb", [128, 2, 1024], BF16)
        cst = make_consts(ph)
        s_w = ph.sem("w")
        s_p = ph.sem("par")
        t_par = ph.dma("sp", gf[:], W["g_ffn"][L:L + 1, :].partition_broadcast(128), s_p)
        wg_v = W["w_gu"][L].rearrange("(k p) n -> p k n", p=128)
        for k in range(8):
            ph.dma("pool", Wgu[:, k, :], wg_v[:, k, :], s_w)
        wd_v = W["w_down"][L].rearrange("(k p) n -> p k n", p=128)
        for k0 in range(0, NC_FF, 6):
            k1 = min(NC_FF, k0 + 6)
            t_w = ph.dma("pool", Wd[:, k0:k1, :], wd_v[:, k0:k1, :], s_w)
        s_ld = [ph.sem("ld0"), ph.sem("ld1")]
        s_st = [ph.sem("st0"), ph.sem("st1")]
        slot_free = [[], []]
        st_tok = [None, None]
        ld_toks = {}

        def emit_loads(i):
            p = i % 2
            r0 = i * T
            ph.wait("sp", slot_free[p], st_tok[p])
            ld_toks[i] = ph.dma("sp", xs[p][:], scr["x1"][r0:r0 + T, :].rearrange("(b p) d -> p b d", p=128), s_ld[p])

        hbf_free = [None] * 4
        stat_free = [None, None]
        hT_free = None
        tp_free = [None, None]
        h_toks = {}

        def emit_norm(i):
            p = i % 2
            st = stat[p]
            toks = []
            for b in range(4):
                ph.wait("act", ld_toks[i], stat_free[p] if b == 0 else None, hbf_free[b])
                t = ph.done("act", ACT.activation(out=hbf[b][:], in_=xs[p][:, b, :], func=AF.Square, accum_out=st[:, b:b + 1]))
                ph.wait("act", t)
                t = ph.done("act", ACT.activation(out=st[:, 4 + b:5 + b], in_=st[:, b:b + 1], func=AF.Ln, bias=EPS, scale=1.0 / D))
                ph.wait("act", t)
                t_r = ph.done("act", ACT.activation(out=st[:, 8 + b:9 + b], in_=st[:, 4 + b:5 + b], func=AF.Exp, scale=-0.5))
                ph.wait("dve", t_r, t_par, hbf_free[b])
                t_h = ph.done("dve", DVE.scalar_tensor_tensor(out=hbf[b][:], in0=xs[p][:, b, :], scalar=st[:, 8 + b:9 + b], in1=gf[:],
                                                              op0=ALU.mult, op1=ALU.mult))
                toks.append(t_h)
            stat_free[p] = toks[-1]
            h_toks[i] = toks

        def emit_transposes(i):
            ready = []
            for b in range(4):
                tb = b % 2
                ph.wait("pe", h_toks[i][b], cst["t_identb"], tp_free[tb])
                for c in range(8):
                    ins = PE.transpose(out=pb[:, tb, c * 128:(c + 1) * 128], in_=hbf[b][:, c * 128:(c + 1) * 128], identity=cst["identb"][:])
                t_tp = ph.done("pe", ins)
                hbf_free[b] = t_tp
                ph.wait("act", t_tp, hT_free if b == 0 else None)
                t_e = ph.done("act", ACT.activation(out=hT[:, :, b * 128:(b + 1) * 128], in_=pb[:, tb, :].rearrange("p (c t) -> p c t", c=8),
                                                    func=AF.Copy))
                tp_free[tb] = t_e
                ready.append(t_e)
            return ready

        bank_free = [None] * 6
        sg_free = [None, None]
        act_free = None
        emit_loads(0)
        emit_norm(0)
        hT_ready = emit_transposes(0)
        cnt = 0
        for i in range(cfg.ntile):
            p = i % 2
            if i + 1 < cfg.ntile:
                emit_loads(i + 1)
            a_toks = []
            for c in range(NC_FF):
                bg_, bu_ = (2 * c) % 4, (2 * c + 1) % 4
                ph.wait("pe", hT_ready, t_w, bank_free[bg_])
                for k in range(8):
                    ins = PE.matmul(pf[:, bg_, :], lhsT=Wgu[:, k, c * 128:(c + 1) * 128], rhs=hT[:, k, :], start=(k == 0), stop=(k == 7))
                t_g = ph.done("pe", ins)
                ph.wait("pe", bank_free[bu_])
                for k in range(8):
                    ins = PE.matmul(pf[:, bu_, :], lhsT=Wgu[:, k, D_FF + c * 128:D_FF + (c + 1) * 128], rhs=hT[:, k, :],
                                    start=(k == 0), stop=(k == 7))
                t_u = ph.done("pe", ins)
                ph.wait("act", t_g, act_free if c == 0 else None)
                t_s = ph.done("act", ACT.activation(out=act[:, c, :], in_=pf[:, bg_, :], func=AF.Silu))
                bank_free[bg_] = t_s
                ph.wait("dve", t_s, t_u)
                t_a = ph.done("dve", DVE.tensor_tensor(out=act[:, c, :], in0=pf[:, bu_, :], in1=act[:, c, :], op=ALU.mult))
                bank_free[bu_] = t_a
                a_toks.append(t_a)
            hT_free = t_u
            if i + 1 < cfg.ntile:
                emit_norm(i + 1)
            for b in range(4):
                for hf in range(2):
                    bk = 4 + (cnt % 2)
                    cnt += 1
                    ph.wait("pe", a_toks, bank_free[bk])
                    for k in range(NC_FF):
                        ins = PE.matmul(pf[:, bk, :], lhsT=act[:, k, b * 128:(b + 1) * 128], rhs=Wd[:, k, hf * 512:(hf + 1) * 512],
                                        start=(k == 0), stop=(k == NC_FF - 1))
                    t_pd = ph.done("pe", ins)
                    ph.wait("dve", t_pd)
                    t_x = ph.done("dve", DVE.tensor_tensor(out=xs[p][:, b, hf * 512:(hf + 1) * 512], in0=pf[:, bk, :],
                                                           in1=xs[p][:, b, hf * 512:(hf + 1) * 512], op=ALU.add))
                    bank_free[bk] = t_x
            act_free = t_pd
            r0 = i * T
            ph.wait("act", t_x)
            st_tok[p] = ph.dma("act", x_out[r0:r0 + T, :].rearrange("(b p) d -> p b d", p=128), xs[p][:], s_st[p])
            slot_free[p] = [t_x]
            if i + 1 < cfg.ntile:
                hT_ready = emit_transposes(i + 1)
        ph.final = [x for x in st_tok if x is not None]


PHASES["p4"] = lambda nc, cfg, L, xin, W, tens, xout: phase4(nc, cfg, L, xin, W, tens)
PHASES["p5"] = lambda nc, cfg, L, xin, W, tens, xout: phase5(nc, cfg, L, W, tens, xout)


def full_plan():
    plan = []
    for L in range(DEPTH):
        xin = "x" if L == 0 else "xmid"
        xout = "xmid" if L == 0 else "out"
        plan += [("p1", L, xin, None), ("p2", L, None, None), ("p3", L, None, None), ("p4", L, xin, None), ("p5", L, None, xout)]
    return plan


_CACHE = {}


def kernel(x, g_mix, w_in, conv_w, conv_b, b_gates, g_q, g_k, w_br_a, w_br_b, w_out, g_ffn, w_gu, w_down):
    x = np.asarray(x, dtype=np.float32)
    B, S, _ = x.shape
    nseq = B // NCORES
    cfg = Cfg(nseq=nseq, S=S)
    key = (nseq, S)
    if key not in _CACHE:
        _CACHE[key] = build_program(cfg, full_plan())
    nc = _CACHE[key]
    Wd = host_layout_weights(dict(conv_w=conv_w, conv_b=conv_b, g_q=g_q, g_k=g_k, g_mix=g_mix, w_in=w_in, b_gates=b_gates,
                                  w_br_a=w_br_a, w_br_b=w_br_b, w_out=w_out, g_ffn=g_ffn, w_gu=w_gu, w_down=w_down))
    in_maps = []
    for c in range(NCORES):
        m = dict(Wd)
        m["x"] = np.ascontiguousarray(x[c * nseq:(c + 1) * nseq].reshape(nseq * S, D))
        in_maps.append(m)
    res = run_bass_kernel_spmd(nc, in_maps, core_ids=list(range(NCORES)))
    out = np.concatenate([np.asarray(r["out"], dtype=np.float32).reshape(nseq, S, D) for r in res.results], axis=0)
    return out
```
